# Optimizing a Trainium2 kernel written in Bass

```python
import math
import jax
import jax.numpy as jnp
from jax import lax
import numpy as np

D_MODEL = 1024
BATCH = 8
SEQ = 4096
DEPTH = 2

CHUNK = 64
CONV_W = 4
GLA_H = 6
GLA_DK = 32
GLA_DV = 64
GLA_RANK = 16
GLA_GATE_NORM = 16.0
LRU_W = 256
LRU_BLOCKS = 4
LRU_BW = LRU_W // LRU_BLOCKS
LRU_C = 8.0
DN_H = 6
DN_DK = 64
DN_DV = 64
D_FF = 2816
N_EXPERTS = 8
TOP_K = 2
D_FF_EXPERT = 3584
N_DENSE = (DEPTH + 1) // 2
N_MOE = DEPTH // 2
EPS = 1e-6

GLA_QK = GLA_H * GLA_DK
GLA_V = GLA_H * GLA_DV
DN_QKV = 2 * DN_H * DN_DK + DN_H * DN_DV
DN_V = DN_H * DN_DV
IN_SPLITS = (GLA_QK, GLA_QK, GLA_V, GLA_V, GLA_RANK, LRU_W, LRU_W, DN_QKV, DN_V, DN_H, DN_H)
D_IN = sum(IN_SPLITS)
D_MIX = GLA_V + LRU_W + DN_V

kernel_name = "hybrid_gla_rglru_gdn_moe_adaln"


def rms_norm(x, w):
    xf = x.astype(jnp.float32)
    y = xf * lax.rsqrt(jnp.mean(xf * xf, axis=-1, keepdims=True) + EPS)
    return (y * w.astype(jnp.float32)).astype(x.dtype)


def l2_norm(x):
    xf = x.astype(jnp.float32)
    return (xf * lax.rsqrt(jnp.sum(xf * xf, axis=-1, keepdims=True) + EPS)).astype(x.dtype)


def split_cols(t, sizes):
    idx = np.cumsum(np.array(sizes))[:-1].tolist()
    return jnp.split(t, idx, axis=-1)


def causal_depthwise_conv(x, w):
    k, ch = w.shape
    return lax.conv_general_dilated(
        x, w[:, None, :].astype(x.dtype), window_strides=(1,), padding=[(k - 1, 0)],
        dimension_numbers=("NWC", "WIO", "NWC"), feature_group_count=ch)


def to_chunks(t):
    b, s = t.shape[:2]
    t = t.reshape((b, s // CHUNK, CHUNK) + t.shape[2:])
    return jnp.swapaxes(jnp.swapaxes(t, 0, 1), 2, 3)


def from_chunks(t):
    t = jnp.swapaxes(jnp.swapaxes(t, 2, 3), 0, 1)
    b, n, c, h, d = t.shape
    return t.reshape(b, n * c, h, d)


def gla_chunked(q, k, v, log_a):
    dtype = v.dtype
    b_, s_, h_, dk = q.shape
    dv = v.shape[-1]
    q, k, v, log_a = (to_chunks(t.astype(jnp.float32)) for t in (q, k, v, log_a))
    bcum = jnp.cumsum(log_a, axis=-2)
    b_last = bcum[..., -1:, :]
    q_dec = q * jnp.exp(bcum)
    k_dec = k * jnp.exp(-bcum)
    k_end = k * jnp.exp(b_last - bcum)
    causal = jnp.tril(jnp.ones((CHUNK, CHUNK), dtype=bool))
    scores = jnp.where(causal, jnp.einsum("nbhck,nbhsk->nbhcs", q_dec, k_dec), 0.0)
    o_intra = jnp.einsum("nbhcs,nbhsv->nbhcv", scores, v)
    chunk_kv = jnp.einsum("nbhck,nbhcv->nbhkv", k_end, v)
    chunk_decay = jnp.exp(b_last[..., 0, :])

    def step(state, inp):
        kv, dec = inp
        return state * dec[..., None] + kv, state

    init = jnp.zeros((b_, h_, dk, dv), jnp.float32)
    _, states = lax.scan(step, init, (chunk_kv, chunk_decay))
    o = o_intra + jnp.einsum("nbhck,nbhkv->nbhcv", q_dec, states)
    return from_chunks(o).astype(dtype)


def gated_delta_chunked(q, k, v, g, beta):
    dtype = v.dtype
    b_, s_, h_, dk = q.shape
    dv = v.shape[-1]
    q, k, v, g, beta = (to_chunks(t.astype(jnp.float32)) for t in (q, k, v, g, beta))
    gc = jnp.cumsum(g, axis=-1)
    incl = jnp.tril(jnp.ones((CHUNK, CHUNK), dtype=bool))
    strict = jnp.tril(jnp.ones((CHUNK, CHUNK), dtype=bool), k=-1)
    diff = gc[..., :, None] - gc[..., None, :]
    decay = jnp.exp(jnp.where(incl, diff, -jnp.inf))
    k_beta = k * beta[..., None]
    v_beta = v * beta[..., None]
    kk = jnp.einsum("nbhck,nbhsk->nbhcs", k_beta, k)
    a_mat = jnp.where(strict, kk * decay, 0.0) + jnp.eye(CHUNK, dtype=jnp.float32)
    u = lax.linalg.triangular_solve(a_mat, v_beta, left_side=True, lower=True, unit_diagonal=True)
    w = lax.linalg.triangular_solve(a_mat, k_beta * jnp.exp(gc)[..., None],
                                    left_side=True, lower=True, unit_diagonal=True)
    qk = jnp.where(incl, jnp.einsum("nbhck,nbhsk->nbhcs", q, k) * decay, 0.0)
    q_dec = q * jnp.exp(gc)[..., None]
    g_last = gc[..., -1:]
    k_end = k * jnp.exp(g_last - gc)[..., None]
    chunk_decay = jnp.exp(g_last)

    def step(state, inp):
        u_n, w_n, q_n, qk_n, k_n, dec_n = inp
        v_new = u_n - jnp.einsum("bhck,bhkv->bhcv", w_n, state)
        o_n = (jnp.einsum("bhck,bhkv->bhcv", q_n, state)
               + jnp.einsum("bhcs,bhsv->bhcv", qk_n, v_new))
        state = state * dec_n[..., None] + jnp.einsum("bhck,bhcv->bhkv", k_n, v_new)
        return state, o_n

    init = jnp.zeros((b_, h_, dk, dv), jnp.float32)
    _, o = lax.scan(step, init, (u, w, q_dec, qk, k_end, chunk_decay))
    return from_chunks(o).astype(dtype)


def _lin_combine(left, right):
    a_l, u_l = left
    a_r, u_r = right
    return a_l * a_r, a_r * u_l + u_r


def rg_lru(x, w_a, b_a, w_x, b_x, lam):
    dtype = x.dtype
    b_, s_, _ = x.shape
    xf = x.astype(jnp.float32)
    xb = xf.reshape(b_, s_, LRU_BLOCKS, LRU_BW)
    r = jax.nn.sigmoid(jnp.einsum("bsnj,njk->bsnk", xb, w_a.astype(jnp.float32)).reshape(b_, s_, LRU_W) + b_a)
    i = jax.nn.sigmoid(jnp.einsum("bsnj,njk->bsnk", xb, w_x.astype(jnp.float32)).reshape(b_, s_, LRU_W) + b_x)
    log_a = -LRU_C * r * jax.nn.softplus(-lam.astype(jnp.float32))
    a = jnp.exp(log_a)
    u = jnp.sqrt(-jnp.expm1(2.0 * log_a)) * (i * xf)
    _, h = lax.associative_scan(_lin_combine, (a, u), axis=1)
    return h.astype(dtype)


def hybrid_mixer(h, w_in, gla_w_lr2, gla_b_lr2, gla_norm_w, lru_conv_w, lru_conv_b,
                 lru_w_a, lru_b_a, lru_w_x, lru_b_x, lru_lambda,
                 dn_conv_w, dn_a_log, dn_dt_bias, dn_norm_w, w_out):
    b_, s_, _ = h.shape
    proj = h @ w_in
    (gq, gk, gv, gz, glr, lx, lgate, dqkv, dz, dbeta, da) = split_cols(proj, IN_SPLITS)

    gq = gq.reshape(b_, s_, GLA_H, GLA_DK) * (GLA_DK ** -0.5)
    gk = gk.reshape(b_, s_, GLA_H, GLA_DK)
    gv = gv.reshape(b_, s_, GLA_H, GLA_DV)
    glog = jax.nn.log_sigmoid((glr @ gla_w_lr2 + gla_b_lr2).astype(jnp.float32)) / GLA_GATE_NORM
    o_gla = gla_chunked(gq, gk, gv, glog.reshape(b_, s_, GLA_H, GLA_DK))
    o_gla = rms_norm(o_gla, gla_norm_w) * jax.nn.silu(gz.reshape(b_, s_, GLA_H, GLA_DV))

    xr = causal_depthwise_conv(lx, lru_conv_w) + lru_conv_b
    o_lru = rg_lru(xr, lru_w_a, lru_b_a, lru_w_x, lru_b_x, lru_lambda) * jax.nn.gelu(lgate)

    dqkv = jax.nn.silu(causal_depthwise_conv(dqkv, dn_conv_w))
    dq, dk_, dv = split_cols(dqkv, (DN_H * DN_DK, DN_H * DN_DK, DN_V))
    dq = l2_norm(dq.reshape(b_, s_, DN_H, DN_DK)) * (DN_DK ** -0.5)
    dk_ = l2_norm(dk_.reshape(b_, s_, DN_H, DN_DK))
    dv = dv.reshape(b_, s_, DN_H, DN_DV)
    beta = jax.nn.sigmoid(dbeta)
    g = -jnp.exp(dn_a_log.astype(jnp.float32)) * jax.nn.softplus((da + dn_dt_bias).astype(jnp.float32))
    o_dn = gated_delta_chunked(dq, dk_, dv, g, beta)
    o_dn = rms_norm(o_dn, dn_norm_w) * jax.nn.silu(dz.reshape(b_, s_, DN_H, DN_DV))

    merged = jnp.concatenate([o_gla.reshape(b_, s_, GLA_V), o_lru, o_dn.reshape(b_, s_, DN_V)], axis=-1)
    return merged @ w_out


def swiglu(t, w_gate, w_up, w_down):
    return (jax.nn.silu(t @ w_gate) * (t @ w_up)) @ w_down


def moe_swiglu(h, router_w, w_gate, w_up, w_down):
    b_, s_, d = h.shape
    t = h.reshape(b_ * s_, d)
    logits = (t @ router_w).astype(jnp.float32)
    top_vals, top_idx = lax.top_k(logits, TOP_K)
    weights = jax.nn.softmax(top_vals, axis=-1)
    gates = jnp.sum(jax.nn.one_hot(top_idx, N_EXPERTS, dtype=jnp.float32) * weights[..., None], axis=1)
    y = jnp.zeros_like(t)
    for e in range(N_EXPERTS):
        y = y + gates[:, e:e + 1].astype(t.dtype) * swiglu(t, w_gate[e], w_up[e], w_down[e])
    return y.reshape(b_, s_, d)


def setup_inputs(seed: int = 0) -> dict:
    key = jax.random.key(seed)
    ks = jax.random.split(key, 40)
    f32 = jnp.float32

    def nrm(k, shape, scale):
        return jax.random.normal(k, shape, f32) * scale

    a0 = jax.random.uniform(ks[12], (DEPTH, LRU_W), f32, minval=0.9, maxval=0.999)
    dt = jnp.exp(jax.random.uniform(ks[15], (DEPTH, DN_H), f32, minval=math.log(1e-3), maxval=math.log(1e-1)))
    return {
        "x": nrm(ks[0], (BATCH, SEQ, D_MODEL), 1.0),
        "c": nrm(ks[1], (BATCH, D_MODEL), 1.0),
        "w_in": nrm(ks[2], (DEPTH, D_MODEL, D_IN), D_MODEL ** -0.5),
        "gla_w_lr2": nrm(ks[3], (DEPTH, GLA_RANK, GLA_QK), GLA_RANK ** -0.5),
        "gla_b_lr2": nrm(ks[4], (DEPTH, GLA_QK), 0.1),
        "gla_norm_w": 1.0 + nrm(ks[5], (DEPTH, GLA_DV), 0.02),
        "lru_conv_w": nrm(ks[6], (DEPTH, CONV_W, LRU_W), CONV_W ** -0.5),
        "lru_conv_b": nrm(ks[7], (DEPTH, LRU_W), 0.02),
        "lru_w_a": nrm(ks[8], (DEPTH, LRU_BLOCKS, LRU_BW, LRU_BW), LRU_BW ** -0.5),
        "lru_b_a": nrm(ks[9], (DEPTH, LRU_W), 0.1),
        "lru_w_x": nrm(ks[10], (DEPTH, LRU_BLOCKS, LRU_BW, LRU_BW), LRU_BW ** -0.5),
        "lru_b_x": nrm(ks[11], (DEPTH, LRU_W), 0.1),
        "lru_lambda": jnp.log(a0) - jnp.log1p(-a0),
        "dn_conv_w": nrm(ks[13], (DEPTH, CONV_W, DN_QKV), CONV_W ** -0.5),
        "dn_a_log": jnp.log(jax.random.uniform(ks[14], (DEPTH, DN_H), f32, minval=1.0, maxval=16.0)),
        "dn_dt_bias": dt + jnp.log(-jnp.expm1(-dt)),
        "dn_norm_w": 1.0 + nrm(ks[16], (DEPTH, DN_DV), 0.02),
        "w_out": nrm(ks[17], (DEPTH, D_MIX, D_MODEL), D_MIX ** -0.5),
        "norm1_w": 1.0 + nrm(ks[18], (DEPTH, D_MODEL), 0.02),
        "norm2_w": 1.0 + nrm(ks[19], (DEPTH, D_MODEL), 0.02),
        "ada_w": nrm(ks[20], (DEPTH, D_MODEL, 6 * D_MODEL), 0.5 * D_MODEL ** -0.5),
        "ada_b": nrm(ks[21], (DEPTH, 6 * D_MODEL), 0.02),
        "ffn_w_gate": nrm(ks[22], (N_DENSE, D_MODEL, D_FF), D_MODEL ** -0.5),
        "ffn_w_up": nrm(ks[23], (N_DENSE, D_MODEL, D_FF), D_MODEL ** -0.5),
        "ffn_w_down": nrm(ks[24], (N_DENSE, D_FF, D_MODEL), D_FF ** -0.5),
        "router_w": nrm(ks[25], (N_MOE, D_MODEL, N_EXPERTS), D_MODEL ** -0.5),
        "moe_w_gate": nrm(ks[26], (N_MOE, N_EXPERTS, D_MODEL, D_FF_EXPERT), D_MODEL ** -0.5),
        "moe_w_up": nrm(ks[27], (N_MOE, N_EXPERTS, D_MODEL, D_FF_EXPERT), D_MODEL ** -0.5),
        "moe_w_down": nrm(ks[28], (N_MOE, N_EXPERTS, D_FF_EXPERT, D_MODEL), D_FF_EXPERT ** -0.5),
        "final_norm_w": 1.0 + nrm(ks[29], (D_MODEL,), 0.02),
    }


def reference(x, c, w_in, gla_w_lr2, gla_b_lr2, gla_norm_w, lru_conv_w, lru_conv_b,
              lru_w_a, lru_b_a, lru_w_x, lru_b_x, lru_lambda, dn_conv_w, dn_a_log, dn_dt_bias,
              dn_norm_w, w_out, norm1_w, norm2_w, ada_w, ada_b, ffn_w_gate, ffn_w_up, ffn_w_down,
              router_w, moe_w_gate, moe_w_up, moe_w_down, final_norm_w):
    c_act = jax.nn.silu(c)
    for i in range(DEPTH):
        mod = c_act @ ada_w[i] + ada_b[i]
        sh1, sc1, g1, sh2, sc2, g2 = (m[:, None, :] for m in jnp.split(mod, 6, axis=-1))

        h = rms_norm(x, norm1_w[i]) * (1.0 + sc1) + sh1
        mix = hybrid_mixer(h, w_in[i], gla_w_lr2[i], gla_b_lr2[i], gla_norm_w[i],
                           lru_conv_w[i], lru_conv_b[i], lru_w_a[i], lru_b_a[i], lru_w_x[i],
                           lru_b_x[i], lru_lambda[i], dn_conv_w[i], dn_a_log[i], dn_dt_bias[i],
                           dn_norm_w[i], w_out[i])
        x = x + g1 * mix

        h = rms_norm(x, norm2_w[i]) * (1.0 + sc2) + sh2
        j = i // 2
        if i % 2 == 0:
            ff = swiglu(h, ffn_w_gate[j], ffn_w_up[j], ffn_w_down[j])
        else:
            ff = moe_swiglu(h, router_w[j], moe_w_gate[j], moe_w_up[j], moe_w_down[j])
        x = x + g2 * ff
    return rms_norm(x, final_norm_w)
```

```python
import os
import numpy as np
import concourse.bass as bass
import concourse.mybir as mybir
from concourse.bass_utils import run_bass_kernel_spmd
from contextlib import ExitStack

F32 = mybir.dt.float32
BF16 = mybir.dt.bfloat16
AF = mybir.ActivationFunctionType
ALU = mybir.AluOpType
AX = mybir.AxisListType

D = 1024
NK = 8
D_IN = 3228
D_FF = 2816
NE = 8
D_FFE = 3584
EPS = 1e-6
O_GQ, O_GK, O_GV, O_GZ, O_GLR, O_LX, O_LG, O_DQ, O_DK, O_DV, O_DZ, O_DB, O_DA = (
    0, 192, 384, 768, 1152, 1168, 1424, 1680, 2064, 2448, 2832, 3216, 3222)
TB = 512
TBA = 256
CH = 64


class Buf:
    _n = 0

    def __init__(self, h, name, init_evs=()):
        self.h = h
        self.name = name
        Buf._n += 1
        self.id = Buf._n
        self.init_evs = list(init_evs)

    def __getitem__(self, idx):
        return self.h[idx]


class BufV(Buf):
    def __init__(self, parent, width):
        self.h = parent.h
        self.name = parent.name
        self.id = parent.id
        self.width = width
        self.init_evs = parent.init_evs

    def __getitem__(self, idx):
        full = slice(None, None, None)
        if idx == full:
            return self.h[:, 0:self.width]
        if isinstance(idx, tuple) and len(idx) == 2 and idx[1] == full:
            return self.h[idx[0], 0:self.width]
        return self.h[idx]


class _Unit:
    __slots__ = ("w", "rs")

    def __init__(self):
        self.w = None
        self.rs = []


class _Rec:
    def __init__(self):
        self.call = None

    def __getattr__(self, name):
        def f(*a, **k):
            self.call = (name, a, k)
            return self
        return f


class _Ev:
    __slots__ = ("sem", "val", "eng", "seen")

    def __init__(self, sem, val, eng, seen):
        self.sem = sem
        self.val = val
        self.eng = eng
        self.seen = seen


class Prog:
    ENGS = ("pe", "act", "dve", "pool", "sp")

    def __init__(self, nc, n_dma_sems=16, same_eng_sync=True):
        self.nc = nc
        self.es = ExitStack()
        self.scopes = []
        self.same_eng_sync = same_eng_sync
        self.sems = {}
        self.cnt = {}
        self.seen = {}
        self.streams = {e: [] for e in self.ENGS}
        for e in self.ENGS:
            self.sems[e] = self.es.enter_context(nc.semaphore("s_" + e))
            self.cnt[e] = 0
            self.seen[e] = {}
        self.dma_sems = {}
        self.dma_uses = {}
        self.dma_rr = {}
        for q in ("sp", "act", "pool"):
            self.dma_sems[q] = [self.es.enter_context(nc.semaphore("d_%s%d" % (q, i)))
                                for i in range(n_dma_sems)]
            self.dma_uses[q] = [0] * n_dma_sems
            self.dma_rr[q] = 0
        self.units = {}
        self.ninst = 0
        self.last_ev = {}
        self.freed = {}
        self.scope_bufs = []
        self.buf_units = {}

    def _stack(self):
        return self.scopes[-1] if self.scopes else self.es

    def sb(self, name, shape, dtype):
        self._uid = getattr(self, "_uid", 0) + 1
        name = "%s_u%d" % (name, self._uid)
        h = self._stack().enter_context(self.nc.sbuf_tensor(name, list(shape), dtype))
        b = Buf(h, name, init_evs=self.freed.values())
        if self.scope_bufs:
            self.scope_bufs[-1].append(b)
        return b

    def ps(self, name, shape, dtype=F32):
        h = self._stack().enter_context(self.nc.psum_tensor(name, list(shape), dtype))
        return Buf(h, name)

    def dram(self, name, shape, dtype, kind="Internal"):
        h = self.nc.dram_tensor(name, list(shape), dtype, kind=kind)
        return Buf(h, name)

    def push_scope(self):
        self.scopes.append(ExitStack())
        self.scope_bufs.append([])

    def pop_scope(self, barrier=True):
        bufs = self.scope_bufs.pop()
        if barrier:
            self.barrier()
        else:
            for b in bufs:
                for key in self.buf_units.get(b.id, ()):
                    un = self.units[key]
                    for ev in ([un.w] if un.w is not None else []) + un.rs:
                        cur = self.freed.get(ev.sem)
                        if cur is None or cur.val < ev.val:
                            self.freed[ev.sem] = ev
                for ev in b.init_evs:
                    cur = self.freed.get(ev.sem)
                    if cur is None or cur.val < ev.val:
                        self.freed[ev.sem] = ev
        self.scopes.pop().close()

    def _unit(self, u):
        buf = u if isinstance(u, Buf) else u[0]
        key = (buf.id, None) if isinstance(u, Buf) else (buf.id, u[1])
        un = self.units.get(key)
        if un is None:
            un = self.units[key] = _Unit()
            un.rs = list(buf.init_evs)
            self.buf_units.setdefault(buf.id, []).append(key)
        return un

    def _collect(self, eng, r, w):
        need = {}

        def add(ev):
            if ev is None:
                return
            if ev.eng == eng and (eng in ("pe", "sp") or not self.same_eng_sync):
                return
            cur = need.get(ev.sem)
            if cur is None or cur[0] < ev.val:
                need[ev.sem] = (ev.val, ev)

        for u in r:
            add(self._unit(u).w)
        for u in w:
            un = self._unit(u)
            add(un.w)
            for ev in un.rs:
                add(ev)
        seen = self.seen[eng]
        waits = []
        for sem, (val, ev) in need.items():
            if seen.get(sem, 0) >= val:
                continue
            waits.append((sem, val))
            seen[sem] = val
            for s2, v2 in ev.seen.items():
                if seen.get(s2, 0) < v2:
                    seen[s2] = v2
        return waits

    def _record(self, ev, r, w):
        for u in r:
            self._unit(u).rs.append(ev)
        for u in w:
            un = self._unit(u)
            un.w = ev
            un.rs = []

    def op(self, eng, fn, r=(), w=()):
        waits = self._collect(eng, r, w)
        sem = self.sems[eng]
        self.cnt[eng] += 1
        val = self.cnt[eng]
        evseen = dict(self.seen[eng])
        evseen[sem] = val
        ev = _Ev(sem, val, eng, evseen)
        rec = _Rec()
        fn(rec)
        name, a, k = rec.call

        def fn2(e, name=name, a=a, k=k):
            return getattr(e, name)(*a, **k)
        self.streams[eng].append((waits, fn2, sem, 1))
        self._record(ev, r, w)
        self.ninst += 1
        self.last_ev[eng] = ev
        return ev

    def dma(self, q, out, in_, r=(), w=(), **kw):
        waits = self._collect(q, r, w)
        pool = self.dma_sems[q]
        i = self.dma_rr[q]
        self.dma_rr[q] = (i + 1) % len(pool)
        sem = pool[i]
        m = self.dma_uses[q][i]
        seen = self.seen[q]
        if m > 0 and seen.get(sem, 0) < 16 * m:
            waits.append((sem, 16 * m))
            seen[sem] = 16 * m
        self.dma_uses[q][i] = m + 1
        val = 16 * (m + 1)
        evseen = dict(seen)
        evseen[sem] = val
        ev = _Ev(sem, val, "dma_" + q, evseen)

        def fn(e, out=out, in_=in_, kw=kw):
            return e.dma_start(out=out, in_=in_, **kw)
        self.streams[q].append((waits, fn, sem, 16))
        self._record(ev, r, w)
        self.ninst += 1
        self.last_ev[("dma", q, i)] = ev
        return ev

    def wait_event(self, eng, ev):
        seen = self.seen[eng]
        if seen.get(ev.sem, 0) >= ev.val:
            return
        seen[ev.sem] = ev.val
        for s2, v2 in ev.seen.items():
            if seen.get(s2, 0) < v2:
                seen[s2] = v2
        self.streams[eng].append(([(ev.sem, ev.val)], None, None, 0))

    def barrier(self):
        evs = list(self.last_ev.values())
        for e in self.ENGS:
            for ev in evs:
                if ev.eng == e and e in ("pe", "sp"):
                    continue
                self.wait_event(e, ev)
        self.freed = {}

    def emit(self):
        nc = self.nc
        streams = self.streams
        with nc.Block() as block:
            def run(e, lst):
                for waits, fn, sem, inc in lst:
                    for (s, v) in waits:
                        e.wait_ge(s, v)
                    if fn is not None:
                        fn(e).then_inc(sem, inc)

            @block.tensor
            def _(e):
                run(e, streams["pe"])

            @block.scalar
            def _(e):
                run(e, streams["act"])

            @block.vector
            def _(e):
                run(e, streams["dve"])

            @block.gpsimd
            def _(e):
                run(e, streams["pool"])

            @block.sync
            def _(e):
                run(e, streams["sp"])


def _fm(v, nchunk):
    return np.ascontiguousarray(np.asarray(v, np.float32).reshape(nchunk, 128).T)


class Cols:
    def __init__(self):
        self.n = 0
        self.off = {}
        self.parts = []

    def add(self, name, arr):
        arr = np.asarray(arr, np.float32)
        if arr.ndim == 1:
            arr = arr[:, None]
        if arr.shape[0] < 128:
            arr = np.concatenate([arr, np.zeros((128 - arr.shape[0], arr.shape[1]), np.float32)], 0)
        self.off[name] = (self.n, arr.shape[1])
        self.n += arr.shape[1]
        self.parts.append(arr)

    def build(self):
        return np.ascontiguousarray(np.concatenate(self.parts, axis=1))


def build_consts():
    c = Cols()
    p = np.arange(128)
    c.add("ident", np.eye(128, dtype=np.float32))
    c.add("ones", np.ones((128, 128), np.float32))
    c.add("blk64", (p[:, None] // 64 == p[None, :] // 64).astype(np.float32))
    t = np.arange(TB)
    c.add("rst", np.tile((t % CH != 0).astype(np.float32)[None, :], (128, 1)))
    s = p % 64
    cc = np.arange(64)
    m = (cc[None, :] >= s[:, None]).astype(np.float32)
    c.add("ut6", np.tile(m, (1, 6)))
    ms = (cc[None, :] > s[:, None]).astype(np.float32)
    c.add("sut6", np.tile(ms, (1, 6)))
    c.add("su2", ((p[:, None] // 64 == p[None, :] // 64) & (p[:, None] > p[None, :])).astype(np.float32))
    for j in range(3):
        e = np.zeros((128, 128), np.float32)
        for h in range(6):
            e[h, :] = (2 * j + p // 64 == h)
        c.add("e6_%d" % j, e)
    nm = np.zeros((128, 64), np.float32)
    nm[:64, :] = np.where(cc[None, :] >= cc[:, None], 0.0, -30000.0)
    c.add("negm", nm)
    i6 = np.zeros((128, 384), np.float32)
    i6[:64, :] = np.tile(np.eye(64, dtype=np.float32), (1, 6))
    c.add("id6", i6)
    sh = np.zeros((128, 64), np.float32)
    sh[64 + np.arange(64), np.arange(64)] = 1.0
    c.add("selhi", sh)
    for h in range(6):
        a = np.zeros((128, 64), np.float32)
        a[h, :] = 1.0
        c.add("selh_%d" % h, a)
    oh = np.zeros((128, 6), np.float32)
    for h in range(6):
        oh[h, h] = 1.0
    c.add("oh6", oh)
    c.add("noh6", -oh)
    return c


def build_consts_b():
    c = Cols()
    for e_ in range(8):
        s8 = np.zeros((8, 128), np.float32)
        s8[e_, :] = 1.0
        c.add("sel8_%d" % e_, s8)
    return c


def build_params(inp):
    c = Cols()
    for l in range(2):
        c.add("ada_b%d" % l, _fm(inp["ada_b"][l], 48))
        c.add("n1w%d" % l, _fm(inp["norm1_w"][l], 8))
        c.add("n2w%d" % l, _fm(inp["norm2_w"][l], 8))
        b = np.asarray(inp["gla_b_lr2"][l], np.float32)
        c.add("glab%d" % l, np.stack([b[0:96], b[96:192]], 1))
        c.add("glanw%d" % l, np.tile(np.asarray(inp["gla_norm_w"][l], np.float32), 2))
        cw = np.asarray(inp["lru_conv_w"][l], np.float32)
        c.add("lrucw%d" % l, np.stack([cw[j, ch * 128:(ch + 1) * 128] for ch in range(2) for j in range(4)], 1))
        c.add("lrucb%d" % l, _fm(inp["lru_conv_b"][l], 2))
        c.add("lruba%d" % l, _fm(inp["lru_b_a"][l], 2))
        c.add("lrubx%d" % l, _fm(inp["lru_b_x"][l], 2))
        c.add("lrulam%d" % l, _fm(inp["lru_lambda"][l], 2))
        dw = np.asarray(inp["dn_conv_w"][l], np.float32)
        c.add("dncw%d" % l, np.stack([dw[j, ch * 128:(ch + 1) * 128] for ch in range(9) for j in range(4)], 1))
        c.add("dnalog%d" % l, np.asarray(inp["dn_a_log"][l], np.float32))
        c.add("dndtb%d" % l, np.asarray(inp["dn_dt_bias"][l], np.float32))
        c.add("dnnw%d" % l, np.tile(np.asarray(inp["dn_norm_w"][l], np.float32), 2))
    c.add("fnw", _fm(inp["final_norm_w"], 8))
    return c


def build_mats(inp):
    out = {}
    lr = np.zeros((2, 32, 192), np.float32)
    for l in range(2):
        lr[l, :16] = inp["gla_w_lr2"][l]
        lr[l, 16] = inp["gla_b_lr2"][l]
    out["wlr2"] = lr
    bd = np.zeros((2, 2, 2, 128, 128), np.float32)
    for l in range(2):
        for t, nm in enumerate(("lru_w_a", "lru_w_x")):
            w = np.asarray(inp[nm][l], np.float32)
            for ch in range(2):
                for q in range(2):
                    n = ch * 2 + q
                    bd[l, t, ch, q * 64:(q + 1) * 64, q * 64:(q + 1) * 64] = w[n]
    out["lrubd"] = bd
    return out


class Builder:
    def __init__(self, S, dbg=False, use=("gla", "lru", "dn"), nlayers=2, sbt=1024):
        self.S = S
        self.dbg = dbg
        self.use = use
        self.nlayers = nlayers
        self.NB = S // TB
        self.SBT = min(sbt, S)
        self.cc = build_consts()
        nc = bass.Bass("TRN2", target_bir_lowering=False)
        self.nc = nc
        P = Prog(nc, same_eng_sync=(os.environ.get('SES', '1') == '1'))
        self.P = P
        self.dbg_outs = []
        self._declare_io()
        self._globals()
        self.pass0()
        for l in range(nlayers):
            self.adaln(l)
            self.passA(l)
            self.passB(l, last=(l == nlayers - 1))
        P.barrier()
        for ev in self.out_events:
            P.wait_event("sp", ev)
        P.emit()

    def _declare_io(self):
        P, S = self.P, self.S
        di = lambda n, s: P.dram(n, s, F32, kind="ExternalInput")
        self.x = di("x", [S, D])
        self.cfm = di("cfm", [128, NK])
        self.w_in = di("w_in", [2, D, D_IN])
        self.w_out = di("w_out", [2, D, D])
        self.ada_w = di("ada_w", [2, D, 6 * D])
        self.ffn_wg = di("ffn_w_gate", [1, D, D_FF])
        self.ffn_wu = di("ffn_w_up", [1, D, D_FF])
        self.ffn_wd = di("ffn_w_down", [1, D_FF, D])
        self.router_w = di("router_w", [1, D, NE])
        self.moe_wg = di("moe_w_gate", [1, NE, D, D_FFE])
        self.moe_wu = di("moe_w_up", [1, NE, D, D_FFE])
        self.moe_wd = di("moe_w_down", [1, NE, D_FFE, D])
        self.cst_d = di("cst", [128, self.cc.n])
        self.ccb = build_consts_b()
        self.cstb_d = di("cstb", [128, self.ccb.n])
        self.pk_n = build_params_ncols()
        self.pk_d = di("pk", [128, self.pk_n])
        self.wlr2_d = di("wlr2", [2, 32, 192])
        self.lrubd_d = di("lrubd", [2, 2, 2, 128, 128])
        self.out = P.dram("out", [S, D], F32, kind="ExternalOutput")
        self.xT = P.dram("xT_scr", [D, S], F32)
        self.out_events = []

    def dbg_dump(self, name):
        if not self.dbg:
            return
        P = self.P
        o = P.dram(name, [D, self.S], F32, kind="ExternalOutput")
        P.push_scope()
        t = P.sb("dbgt", [128, NK, TB], F32)
        for b in range(self.NB):
            P.dma("sp", t[:], self.xT[:, b * TB:(b + 1) * TB].rearrange("(k p) n -> p k n", p=128),
                  r=[(self.xT, b)], w=[t])
            ev = P.dma("sp", o[:, b * TB:(b + 1) * TB].rearrange("(k p) n -> p k n", p=128), t[:],
                       r=[t], w=[o])
            self.out_events.append(ev)
        P.pop_scope()
        self.dbg_outs.append(name)

    def C(self, name, rows=128):
        o, n = self.cc.off[name]
        return self.cst[0:rows, o:o + n]

    def PK(self, name, rows=128, col=None, ncol=None):
        o, n = self.pko[name]
        if col is not None:
            o = o + col
            n = 1 if ncol is None else ncol
        return self.pk[0:rows, o:o + n]

    def _globals(self):
        P = self.P
        self.cst = P.sb("cst_sb", [128, self.cc.n], F32)
        P.dma("sp", self.cst[:], self.cst_d[:], w=[self.cst])
        self.pk = P.sb("pk_sb", [128, self.pk_n], F32)
        P.dma("sp", self.pk[:], self.pk_d[:], w=[self.pk])
        self.pko = build_params_offsets()
        self.ones_bf = P.sb("ones_bf", [128, 128], BF16)
        P.op("dve", lambda e: e.tensor_copy(out=self.ones_bf[:], in_=self.C("ones")), r=[self.cst], w=[self.ones_bf])
        self.blk_bf = P.sb("blk_bf", [128, 128], BF16)
        P.op("dve", lambda e: e.tensor_copy(out=self.blk_bf[:], in_=self.C("blk64")), r=[self.cst], w=[self.blk_bf])
        self.PSB = [P.ps("psb%d" % i, [128, 512], F32) for i in range(8)]
        self.epsc = P.sb("epsc", [128, 1], F32)
        P.op("dve", lambda e: e.memset(self.epsc[:], EPS), w=[self.epsc])
        self.csil = P.sb("csil", [128, NK], F32)
        ct = P.sb("ctmp", [128, NK], F32)
        P.dma("sp", ct[:], self.cfm[:], w=[ct])
        P.op("act", lambda e: e.activation(out=self.csil[:], in_=ct[:], func=AF.Silu), r=[ct], w=[self.csil])
        self.mod = [P.sb("mod%d" % l, [128, 48], F32) for l in range(2)]
        self.gv1 = [P.sb("gv1_%d" % l, [128, NK], F32) for l in range(2)]
        self.gv2 = [P.sb("gv2_%d" % l, [128, NK], F32) for l in range(2)]

    def pass0(self):
        P = self.P
        P.push_scope()
        xin = [P.sb("p0_in%d" % i, [128, D], F32) for i in range(2)]
        xt = [P.sb("p0_xt%d" % i, [128, NK, TB], F32) for i in range(2)]
        n = 0
        for b in range(self.NB):
            xo = xt[b % 2]
            for tt in range(TB // 128):
                xi = xin[n % 2]
                r0 = b * TB + tt * 128
                P.dma("sp", xi[:], self.x[r0:r0 + 128, :], r=[self.x], w=[xi])
                for half in range(2):
                    pb = self.PSB[(n * 2 + half) % 4]
                    for q in range(4):
                        k = half * 4 + q
                        P.op("pe", lambda e, pb=pb, q=q, xi=xi, k=k: e.transpose(
                            out=pb[:, q * 128:(q + 1) * 128], in_=xi[:, k * 128:(k + 1) * 128],
                            identity=self.C("ident")), r=[xi, self.cst], w=[pb])
                    eng = "act" if half == 0 else "dve"
                    if eng == "act":
                        P.op("act", lambda e, pb=pb, xo=xo, half=half, tt=tt: e.activation(
                            out=xo[:, half * 4:half * 4 + 4, tt * 128:(tt + 1) * 128],
                            in_=pb[:].rearrange("p (q n) -> p q n", q=4), func=AF.Copy), r=[pb], w=[xo])
                    else:
                        P.op("dve", lambda e, pb=pb, xo=xo, half=half, tt=tt: e.tensor_copy(
                            out=xo[:, half * 4:half * 4 + 4, tt * 128:(tt + 1) * 128],
                            in_=pb[:].rearrange("p (q n) -> p q n", q=4)), r=[pb], w=[xo])
                n += 1
            P.dma("sp", self.xT[:, b * TB:(b + 1) * TB].rearrange("(k p) n -> p k n", p=128), xo[:],
                  r=[xo], w=[(self.xT, b)])
        P.pop_scope()
        self.dbg_dump("dbg_x0")

    def adaln(self, l):
        P = self.P
        P.push_scope()
        wt = [P.sb("ada_wt%d" % i, [128, NK, 1024], F32) for i in range(2)]
        pb = self.PSB[7]
        for g in range(6):
            t = wt[g % 2]
            P.dma("sp" if g % 2 == 0 else "act", t[:],
                  self.ada_w[l, :, g * 1024:(g + 1) * 1024].rearrange("(k p) n -> p k n", p=128),
                  r=[self.ada_w], w=[t])
            for oc in range(8):
                col = g * 8 + oc
                for k in range(NK):
                    P.op("pe", lambda e, t=t, k=k, oc=oc, col=col: e.matmul(
                        pb[:, col:col + 1], lhsT=t[:, k, oc * 128:(oc + 1) * 128], rhs=self.csil[:, k:k + 1],
                        start=(k == 0), stop=(k == NK - 1)), r=[t, self.csil], w=[pb])
        mod = self.mod[l]
        P.op("dve", lambda e: e.tensor_tensor(out=mod[:], in0=pb[:, 0:48], in1=self.PK("ada_b%d" % l), op=ALU.add),
             r=[pb, self.pk], w=[mod])
        for (gv, nw, c0) in ((self.gv1[l], "n1w%d" % l, 8), (self.gv2[l], "n2w%d" % l, 32)):
            P.op("dve", lambda e, gv=gv, nw=nw, c0=c0: e.scalar_tensor_tensor(
                out=gv[:], in0=mod[:, c0:c0 + 8], scalar=1.0, in1=self.PK(nw), op0=ALU.add, op1=ALU.mult),
                r=[mod, self.pk], w=[gv])
        P.pop_scope()

    def load_norm(self, XT, b, gv, shc, Hap, Hdep, SQ, tmp, rstd, hf_cb=None, ps=None, tb=TB):
        P = self.P
        ps = ps if ps is not None else self.PSB[7]
        P.dma("sp", XT[:], self.xT[:, b * tb:(b + 1) * tb].rearrange("(k p) n -> p k n", p=128),
              r=[(self.xT, b)], w=[XT])
        P.op("act", lambda e: e.activation(out=SQ[:], in_=XT[:], func=AF.Square), r=[XT], w=[SQ])
        for k in range(NK):
            P.op("pe", lambda e, k=k: e.matmul(ps[:, 0:tb], lhsT=self.ones_bf[:], rhs=SQ[:, k, :],
                                               start=(k == 0), stop=(k == NK - 1)),
                 r=[self.ones_bf, SQ], w=[ps])
        P.op("act", lambda e: e.activation(out=rstd[:], in_=ps[:, 0:tb], func=AF.Ln, bias=self.epsc[:, 0:1], scale=1.0 / D),
             r=[ps, self.epsc], w=[rstd])
        P.op("act", lambda e: e.activation(out=rstd[:], in_=rstd[:], func=AF.Exp, scale=-0.5), r=[rstd], w=[rstd])
        for k in range(NK):
            t = tmp[k % len(tmp)]
            P.op("dve", lambda e, k=k, t=t: e.scalar_tensor_tensor(
                out=t[:], in0=XT[:, k, :], scalar=gv[:, k:k + 1], in1=rstd[:], op0=ALU.mult, op1=ALU.mult),
                r=[XT, gv, rstd], w=[t])
            if hf_cb is None:
                P.op("act", lambda e, k=k, t=t: e.activation(
                    out=Hap(k), in_=t[:], func=AF.Identity, bias=shc(k), scale=1.0),
                    r=[t, self.mod[0], self.mod[1]], w=[Hdep])
            else:
                P.op("act", lambda e, k=k, t=t: e.activation(
                    out=t[:], in_=t[:], func=AF.Identity, bias=shc(k), scale=1.0),
                    r=[t, self.mod[0], self.mod[1]], w=[t])
                P.op("dve", lambda e, k=k, t=t: e.tensor_copy(out=Hap(k), in_=t[:]), r=[t], w=[Hdep])
                hf_cb(k, t)

    def passA(self, l):
        P = self.P
        P.push_scope()
        self.A_l = l
        self.PSA = [BufV(b_, TBA) for b_ in self.PSB]
        WIN = P.sb("WIN", [128, NK, D_IN], BF16)
        self.WIN = WIN
        WOUT = P.sb("WOUT", [128, NK, D], BF16)
        for k in range(NK):
            P.dma("pool", WOUT[:, k, :], self.w_out[l, k * 128:(k + 1) * 128, :], r=[self.w_out], w=[(WOUT, k)])
        for k in range(NK):
            P.dma("pool", WIN[:, k, :], self.w_in[l, k * 128:(k + 1) * 128, :], r=[self.w_in], w=[(WIN, k)],
                  max_dma_last_dim=4096)
        H = P.sb("A_H", [128, NK, TBA], BF16)
        MT = P.sb("A_MT", [128, NK, TBA], BF16)
        self.H, self.MT = H, MT
        self.tpool = [P.sb("A_t%d" % i, [128, TBA], F32) for i in range(self.NTP)]
        self.tfree = list(self.tpool)
        self.mixer_setup(l)
        mod = self.mod[l]
        for b in range(self.S // TBA):
            P.push_scope()
            XT = P.sb("A_XT", [128, NK, TBA], F32)
            SQ = P.sb("A_SQ", [128, NK, TBA], BF16)
            rstd = P.sb("A_rstd", [128, TBA], F32)
            tmp = [P.sb("A_tmp%d" % i, [128, TBA], F32) for i in range(2)]
            self.load_norm(XT, b, self.gv1[l], lambda k: mod[:, 0 + k:0 + k + 1], lambda k: H[:, k, :], H, SQ, tmp, rstd, tb=TBA)
            P.pop_scope(barrier=False)
            if "lru" in self.use:
                self.lru_block(l, b)
            else:
                self.zero_mt((3, 4))
            if "gla" in self.use:
                self.gla_block(l, b)
            else:
                self.zero_mt((0, 1, 2))
            if "dn" in self.use:
                self.dn_block(l, b)
            else:
                self.zero_mt((5, 6, 7))
            P.push_scope()
            XT = P.sb("A_XT2", [128, NK, TBA], F32)
            P.dma("sp", XT[:], self.xT[:, b * TBA:(b + 1) * TBA].rearrange("(k p) n -> p k n", p=128),
                  r=[(self.xT, b)], w=[XT])
            for dc in range(NK):
                pb = self.PSA[dc % 2]
                for j in range(NK):
                    P.op("pe", lambda e, pb=pb, j=j, dc=dc: e.matmul(
                        pb[:], lhsT=WOUT[:, j, dc * 128:(dc + 1) * 128], rhs=MT[:, j, :],
                        start=(j == 0), stop=(j == NK - 1)),
                        r=[(WOUT, j), (MT, j)], w=[pb])
                P.op("dve", lambda e, pb=pb, dc=dc: e.scalar_tensor_tensor(
                    out=XT[:, dc, :], in0=pb[:], scalar=mod[:, 16 + dc:16 + dc + 1], in1=XT[:, dc, :],
                    op0=ALU.mult, op1=ALU.add), r=[pb, mod, XT], w=[XT])
            P.dma("sp", self.xT[:, b * TBA:(b + 1) * TBA].rearrange("(k p) n -> p k n", p=128), XT[:],
                  r=[XT], w=[(self.xT, b)])
            P.pop_scope(barrier=False)
        P.pop_scope()
        self.dbg_dump("dbg_xA%d" % l)

    def zero_mt(self, js):
        P = self.P
        for j in js:
            P.op("pool", lambda e, j=j: e.memset(self.MT[:, j, :], 0.0), w=[(self.MT, j)])

    NTP = 6

    def tget(self):
        return self.tfree.pop()

    def tput(self, *ts):
        for t in ts:
            self.tfree.append(t)

    def proj_fm(self, pb, col0, ncols, prow0=0):
        P = self.P
        for k in range(NK):
            P.op("pe", lambda e, k=k: e.matmul(
                pb[prow0:prow0 + ncols, :], lhsT=self.WIN[:, k, col0:col0 + ncols], rhs=self.H[:, k, :],
                start=(k == 0), stop=(k == NK - 1)), r=[(self.WIN, k), self.H], w=[pb])

    def mixer_setup(self, l):
        P = self.P
        self.lru_bd = P.sb("lru_bd", [128, 2, 2, 128], F32)
        for t in range(2):
            for ch in range(2):
                P.dma("sp", self.lru_bd[:, t, ch, :], self.lrubd_d[l, t, ch], r=[self.lrubd_d], w=[self.lru_bd])
        self.lru_c1 = P.sb("lru_c1", [128, 2], F32)
        self.lru_c2 = P.sb("lru_c2", [128, 2], F32)
        ta = P.sb("lru_ta", [128, 2], F32)
        tb = P.sb("lru_tb", [128, 2], F32)
        lam = self.PK("lrulam%d" % l)
        P.op("act", lambda e: e.activation(out=ta[:], in_=lam, func=AF.Abs), r=[self.pk], w=[ta])
        P.op("act", lambda e: e.activation(out=ta[:], in_=ta[:], func=AF.Exp, scale=-1.0), r=[ta], w=[ta])
        P.op("act", lambda e: e.activation(out=ta[:], in_=ta[:], func=AF.Ln, bias=1.0, scale=1.0), r=[ta], w=[ta])
        P.op("dve", lambda e: e.tensor_scalar(out=tb[:], in0=lam, scalar1=-1.0, scalar2=0.0,
                                              op0=ALU.mult, op1=ALU.max), r=[self.pk], w=[tb])
        P.op("dve", lambda e: e.tensor_tensor(out=ta[:], in0=ta[:], in1=tb[:], op=ALU.add), r=[ta, tb], w=[ta])
        P.op("dve", lambda e: e.tensor_scalar(out=self.lru_c1[:], in0=ta[:], scalar1=-8.0, scalar2=None,
                                              op0=ALU.mult), r=[ta], w=[self.lru_c1])
        P.op("dve", lambda e: e.tensor_scalar(out=self.lru_c2[:], in0=ta[:], scalar1=-16.0, scalar2=None,
                                              op0=ALU.mult), r=[ta], w=[self.lru_c2])
        self.lru_x = [P.sb("lru_x%d" % ch, [128, 3 + TBA], F32) for ch in range(2)]
        self.lru_h = P.sb("lru_h", [128, 2], F32)
        for ch in range(2):
            P.op("pool", lambda e, ch=ch: e.memset(self.lru_x[ch][:], 0.0), w=[self.lru_x[ch]])
        P.op("pool", lambda e: e.memset(self.lru_h[:], 0.0), w=[self.lru_h])
        if "gla" in self.use:
            self.gla_setup(l)
        if "dn" in self.use:
            self.dn_setup(l)

    def conv4(self, out, xin, wname, wcol0, bias_ap=None, rdeps=(), wdeps=()):
        P = self.P
        w = lambda j: self.PK(wname, col=wcol0 + j)
        if bias_ap is not None:
            P.op("dve", lambda e: e.tensor_scalar(out=out, in0=xin[:, 0:TBA], scalar1=w(0), scalar2=bias_ap,
                                                  op0=ALU.mult, op1=ALU.add), r=list(rdeps) + [self.pk], w=list(wdeps))
        else:
            P.op("dve", lambda e: e.tensor_scalar(out=out, in0=xin[:, 0:TBA], scalar1=w(0), scalar2=None,
                                                  op0=ALU.mult), r=list(rdeps) + [self.pk], w=list(wdeps))
        for j in range(1, 4):
            P.op("dve", lambda e, j=j: e.scalar_tensor_tensor(out=out, in0=xin[:, j:j + TBA], scalar=w(j), in1=out,
                                                              op0=ALU.mult, op1=ALU.add),
                 r=list(rdeps) + [self.pk] + list(wdeps), w=list(wdeps))

    def lru_block(self, l, b):
        P = self.P
        PS = self.PSA
        P.push_scope()
        CH2 = range(2)
        T = {nm: [P.sb("lru_%s%d" % (nm, ch), [128, TBA], F32) for ch in CH2] for nm in ("xr", "rg", "ig", "a", "a2", "g", "t2", "hs")}
        X = self.lru_x
        for ch in CH2:
            if b > 0:
                P.op("dve", lambda e, ch=ch: e.tensor_copy(out=X[ch][:, 0:3], in_=X[ch][:, TBA:TBA + 3]), r=[X[ch]], w=[X[ch]])
            self.proj_fm(PS[ch], O_LX + ch * 128, 128)
            P.op("act", lambda e, ch=ch: e.activation(out=X[ch][:, 3:3 + TBA], in_=PS[ch][:], func=AF.Copy), r=[PS[ch]], w=[X[ch]])
            self.proj_fm(PS[6 + ch], O_LG + ch * 128, 128)
            P.op("act", lambda e, ch=ch: e.activation(out=T["g"][ch][:], in_=PS[6 + ch][:], func=AF.Copy), r=[PS[6 + ch]], w=[T["g"][ch]])
        for ch in CH2:
            xr = T["xr"][ch]
            self.conv4(xr[:], X[ch], "lrucw%d" % l, ch * 4, bias_ap=self.PK("lrucb%d" % l, col=ch), rdeps=[X[ch]], wdeps=[xr])
        for ch in CH2:
            g, t2 = T["g"][ch], T["t2"][ch]
            P.op("dve", lambda e, g=g, t2=t2: e.tensor_tensor(out=t2[:], in0=g[:], in1=g[:], op=ALU.mult), r=[g], w=[t2])
        for ch in CH2:
            t2 = T["t2"][ch]
            P.op("dve", lambda e, t2=t2: e.tensor_scalar(out=t2[:], in0=t2[:], scalar1=0.044715, scalar2=1.0,
                                                         op0=ALU.mult, op1=ALU.add), r=[t2], w=[t2])
        for ch in CH2:
            g, t2 = T["g"][ch], T["t2"][ch]
            P.op("dve", lambda e, g=g, t2=t2: e.tensor_tensor(out=t2[:], in0=t2[:], in1=g[:], op=ALU.mult), r=[t2, g], w=[t2])
        for ch in CH2:
            xr = T["xr"][ch]
            P.op("pe", lambda e, ch=ch, xr=xr: e.matmul(PS[2 + ch][:], lhsT=self.lru_bd[:, 0, ch, :], rhs=xr[:],
                                                        start=True, stop=True), r=[self.lru_bd, xr], w=[PS[2 + ch]])
            P.op("pe", lambda e, ch=ch, xr=xr: e.matmul(PS[4 + ch][:], lhsT=self.lru_bd[:, 1, ch, :], rhs=xr[:],
                                                        start=True, stop=True), r=[self.lru_bd, xr], w=[PS[4 + ch]])
        for ch in CH2:
            t2 = T["t2"][ch]
            P.op("act", lambda e, t2=t2: e.activation(out=t2[:], in_=t2[:], func=AF.Sigmoid, scale=1.5957691216), r=[t2], w=[t2])
        for ch in CH2:
            rg, ig = T["rg"][ch], T["ig"][ch]
            P.op("act", lambda e, rg=rg, ch=ch: e.activation(
                out=rg[:], in_=PS[2 + ch][:], func=AF.Sigmoid, bias=self.PK("lruba%d" % l, col=ch), scale=1.0),
                r=[PS[2 + ch], self.pk], w=[rg])
            P.op("act", lambda e, ig=ig, ch=ch: e.activation(
                out=ig[:], in_=PS[4 + ch][:], func=AF.Sigmoid, bias=self.PK("lrubx%d" % l, col=ch), scale=1.0),
                r=[PS[4 + ch], self.pk], w=[ig])
        for ch in CH2:
            g, t2 = T["g"][ch], T["t2"][ch]
            P.op("dve", lambda e, g=g, t2=t2: e.tensor_tensor(out=t2[:], in0=t2[:], in1=g[:], op=ALU.mult), r=[t2, g], w=[t2])
        for ch in CH2:
            rg, a, a2 = T["rg"][ch], T["a"][ch], T["a2"][ch]
            P.op("act", lambda e, a=a, rg=rg, ch=ch: e.activation(
                out=a[:], in_=rg[:], func=AF.Exp, scale=self.lru_c1[:, ch:ch + 1]), r=[rg, self.lru_c1], w=[a])
            P.op("act", lambda e, a2=a2, rg=rg, ch=ch: e.activation(
                out=a2[:], in_=rg[:], func=AF.Exp, scale=self.lru_c2[:, ch:ch + 1]), r=[rg, self.lru_c2], w=[a2])
        for ch in CH2:
            a2 = T["a2"][ch]
            P.op("dve", lambda e, a2=a2: e.tensor_scalar(out=a2[:], in0=a2[:], scalar1=-1.0, scalar2=1.0,
                                                         op0=ALU.mult, op1=ALU.add), r=[a2], w=[a2])
        for ch in CH2:
            a2 = T["a2"][ch]
            P.op("dve", lambda e, a2=a2: e.tensor_scalar(out=a2[:], in0=a2[:], scalar1=1e-30, scalar2=None,
                                                         op0=ALU.max), r=[a2], w=[a2])
        for ch in CH2:
            a2 = T["a2"][ch]
            P.op("act", lambda e, a2=a2: e.activation(out=a2[:], in_=a2[:], func=AF.Ln), r=[a2], w=[a2])
        for ch in CH2:
            a2 = T["a2"][ch]
            P.op("act", lambda e, a2=a2: e.activation(out=a2[:], in_=a2[:], func=AF.Exp, scale=0.5), r=[a2], w=[a2])
        for ch in CH2:
            ig, xr = T["ig"][ch], T["xr"][ch]
            P.op("dve", lambda e, ig=ig, xr=xr: e.tensor_tensor(out=ig[:], in0=ig[:], in1=xr[:], op=ALU.mult), r=[ig, xr], w=[ig])
        for ch in CH2:
            ig, a2 = T["ig"][ch], T["a2"][ch]
            P.op("dve", lambda e, ig=ig, a2=a2: e.tensor_tensor(out=ig[:], in0=ig[:], in1=a2[:], op=ALU.mult), r=[ig, a2], w=[ig])
        for ch in CH2:
            hs, a, ig = T["hs"][ch], T["a"][ch], T["ig"][ch]
            P.op("dve", lambda e, hs=hs, a=a, ig=ig, ch=ch: e.tensor_tensor_scan(
                out=hs[:], data0=a[:], data1=ig[:], initial=self.lru_h[:, ch:ch + 1], op0=ALU.mult, op1=ALU.add),
                r=[a, ig, self.lru_h], w=[hs])
        for ch in CH2:
            hs = T["hs"][ch]
            P.op("dve", lambda e, hs=hs, ch=ch: e.tensor_copy(out=self.lru_h[:, ch:ch + 1], in_=hs[:, TBA - 1:TBA]),
                 r=[hs], w=[self.lru_h])
        for ch in CH2:
            hs, t2 = T["hs"][ch], T["t2"][ch]
            P.op("dve", lambda e, t2=t2, hs=hs, ch=ch: e.tensor_tensor(out=self.MT[:, 3 + ch, :], in0=t2[:], in1=hs[:],
                                                                       op=ALU.mult), r=[t2, hs], w=[(self.MT, 3 + ch)])
        P.pop_scope(barrier=False)

    def gla_setup(self, l):
        P = self.P
        W6 = 6 * TBA
        self.WLR = P.sb("gla_wlr", [32, 192], F32)
        P.dma("sp", self.WLR[:], self.wlr2_d[l], r=[self.wlr2_d], w=[self.WLR])
        self.gS = P.sb("gla_S", [64, 384], F32)
        P.op("pool", lambda e: e.memset(self.gS[:], 0.0), w=[self.gS])

    def gla_block(self, l, b):
        P = self.P
        PS = self.PSA
        NEG = -1.0 / 16.0
        W6 = 6 * TBA
        P.push_scope()
        self.GL = P.sb("gla_gl", [32, TBA], F32)
        P.op("pool", lambda e: e.memset(self.GL[:], 1.0), w=[self.GL])
        GQ = P.sb("gla_q", [64, W6], F32)
        GK = P.sb("gla_k", [64, W6], F32)
        GEB = P.sb("gla_eb", [32, W6], F32)
        GL1 = P.sb("gla_l1", [32, W6], F32)
        GL2 = P.sb("gla_l2", [32, W6], F32)
        gS = self.gS
        for t in (GQ, GK):
            P.op("pool", lambda e, t=t: e.memset(t[:], 0.0), w=[t])
        self.gTM = [P.sb("gla_tm%d" % t, [64, 1152], F32) for t in range(TBA // CH)]
        self.proj_fm(PS[0], O_GLR, 16)
        P.op("act", lambda e: e.activation(out=self.GL[0:16, :], in_=PS[0][0:16, :], func=AF.Copy), r=[PS[0]], w=[self.GL])
        for h in range(6):
            pz = PS[1 + h % 2]
            P.op("pe", lambda e, h=h, pz=pz: e.matmul(pz[0:32, :], lhsT=self.WLR[0:17, h * 32:(h + 1) * 32],
                                                      rhs=self.GL[0:17, :], start=True, stop=True),
                 r=[self.WLR, self.GL], w=[pz])
            P.op("dve", lambda e, h=h, pz=pz: e.tensor_scalar(out=GL1[:, h * TBA:(h + 1) * TBA], in0=pz[0:32, :], scalar1=-80.0,
                                                              scalar2=None, op0=ALU.max), r=[pz], w=[GL1])
        P.op("act", lambda e: e.activation(out=GL1[:], in_=GL1[:], func=AF.Exp, scale=-1.0), r=[GL1], w=[GL1])
        P.op("act", lambda e: e.activation(out=GL1[:], in_=GL1[:], func=AF.Ln, bias=1.0, scale=1.0), r=[GL1], w=[GL1])
        for i in range(3):
            cs = slice(i * 2 * TBA, (i + 1) * 2 * TBA)
            P.op("dve", lambda e, cs=cs: e.tensor_tensor_scan(
                out=GL2[:, cs], data0=self.C("rst", rows=32)[:, 0:2 * TBA], data1=GL1[:, cs], initial=0.0,
                op0=ALU.mult, op1=ALU.add), r=[GL1, self.cst], w=[GL2])
        P.op("act", lambda e: e.activation(out=GEB[:], in_=GL2[:], func=AF.Exp, scale=NEG), r=[GL2], w=[GEB])
        P.op("act", lambda e: e.activation(out=GL1[:], in_=GL2[:], func=AF.Exp, scale=-NEG), r=[GL2], w=[GL1])
        for h in range(6):
            hs = slice(h * TBA, (h + 1) * TBA)
            pq = PS[1 + h % 2]
            self.proj_fm(pq, O_GQ + h * 32, 32)
            P.op("dve", lambda e, hs=hs, pq=pq: e.scalar_tensor_tensor(
                out=GQ[0:32, hs], in0=pq[0:32, :], scalar=0.17677669529663687, in1=GEB[:, hs], op0=ALU.mult, op1=ALU.mult),
                r=[pq, GEB], w=[GQ])
            pk_ = PS[3 + h % 2]
            self.proj_fm(pk_, O_GK + h * 32, 32)
            P.op("dve", lambda e, hs=hs, pk_=pk_: e.tensor_tensor(
                out=GK[0:32, hs], in0=pk_[0:32, :], in1=GL1[:, hs], op=ALU.mult), r=[pk_, GL1], w=[GK])
        NCH = TBA // CH
        for n in range(NCH):
            tm = self.gTM[n]
            ctok = slice(n * 64, (n + 1) * 64)
            LT = tm[:, 0:192]
            KE = tm[:, 192:384]
            VT = tm[:, 384:768]
            pz = PS[5]
            P.op("pe", lambda e, ctok=ctok, pz=pz: e.matmul(pz[0:64, 0:192], lhsT=self.GL[0:17, ctok], rhs=self.WLR[0:17, :],
                                                            start=True, stop=True), r=[self.GL, self.WLR], w=[pz])
            P.op("dve", lambda e, LT=LT, pz=pz: e.tensor_scalar(out=LT, in0=pz[0:64, 0:192], scalar1=-80.0, scalar2=None,
                                                                op0=ALU.max), r=[pz], w=[(tm, "L")])
            P.op("act", lambda e, LT=LT: e.activation(out=LT, in_=LT, func=AF.Exp, scale=-1.0), r=[(tm, "L")], w=[(tm, "L")])
            P.op("act", lambda e, LT=LT: e.activation(out=LT, in_=LT, func=AF.Ln, bias=1.0, scale=1.0),
                 r=[(tm, "L")], w=[(tm, "L")])
            P.op("pe", lambda e, LT=LT, pz=pz: e.matmul(pz[0:64, 192:384], lhsT=self.C("su2", rows=64)[:, 0:64], rhs=LT,
                                                        start=True, stop=True), r=[self.cst, (tm, "L")], w=[pz])
            P.op("act", lambda e, KE=KE, pz=pz: e.activation(out=KE, in_=pz[0:64, 192:384], func=AF.Exp, scale=NEG),
                 r=[pz], w=[(tm, "KE")])
            pk_ = PS[6]
            pv = PS[7]
            tok = slice(n * 64, (n + 1) * 64)
            for kc in range(NK):
                P.op("pe", lambda e, kc=kc, tok=tok, pk_=pk_: e.matmul(
                    pk_[0:64, 0:192], lhsT=self.H[:, kc, tok], rhs=self.WIN[:, kc, O_GK:O_GK + 192],
                    start=(kc == 0), stop=(kc == NK - 1)), r=[self.H, (self.WIN, kc)], w=[pk_])
            for kc in range(NK):
                P.op("pe", lambda e, kc=kc, tok=tok, pv=pv: e.matmul(
                    pv[0:64, 0:384], lhsT=self.H[:, kc, tok], rhs=self.WIN[:, kc, O_GV:O_GV + 384],
                    start=(kc == 0), stop=(kc == NK - 1)), r=[self.H, (self.WIN, kc)], w=[pv])
            P.op("dve", lambda e, KE=KE, pk_=pk_: e.tensor_tensor(out=KE, in0=pk_[0:64, 0:192], in1=KE, op=ALU.mult),
                 r=[pk_, (tm, "KE")], w=[(tm, "KE")])
            P.op("act", lambda e, VT=VT, pv=pv: e.activation(out=VT, in_=pv[0:64, 0:384], func=AF.Copy), r=[pv], w=[(tm, "V")])
            psc = PS[5]
            for h in range(6):
                hc = slice(h * TBA + n * 64, h * TBA + n * 64 + 64)
                P.op("pe", lambda e, h=h, hc=hc, psc=psc: e.matmul(
                    psc[0:64, h * 64:(h + 1) * 64], lhsT=GK[:, hc], rhs=GQ[:, hc], start=True, stop=True),
                    r=[GK, GQ], w=[psc])
            P.op("dve", lambda e, tm=tm, psc=psc: e.tensor_tensor(out=tm[:, 768:1152], in0=psc[0:64, 0:384],
                                                                  in1=self.C("ut6", rows=64), op=ALU.mult),
                 r=[psc, self.cst], w=[(tm, "SC")])
        OT = [PS[0], PS[1], PS[2]]
        pkv = PS[3]
        for n in range(NCH):
            tm = self.gTM[n]
            ctok = slice(n * 64, (n + 1) * 64)
            for h in range(6):
                jj, hp = h // 2, h % 2
                hc = slice(h * TBA + n * 64, h * TBA + n * 64 + 64)
                oap = OT[jj][hp * 64:(hp + 1) * 64, ctok]
                P.op("pe", lambda e, oap=oap, tm=tm, h=h: e.matmul(
                    oap, lhsT=tm[:, 384 + h * 64:384 + (h + 1) * 64], rhs=tm[:, 768 + h * 64:768 + (h + 1) * 64],
                    start=True, stop=False), r=[(tm, "V"), (tm, "SC")], w=[OT[jj]])
                P.op("pe", lambda e, oap=oap, h=h, hc=hc: e.matmul(
                    oap, lhsT=gS[:, h * 64:(h + 1) * 64], rhs=GQ[:, hc], start=False, stop=True),
                    r=[gS, GQ], w=[OT[jj]])
            for h in range(6):
                P.op("pe", lambda e, tm=tm, h=h: e.matmul(
                    pkv[0:32, h * 64:(h + 1) * 64], lhsT=tm[:, 192 + h * 32:192 + (h + 1) * 32],
                    rhs=tm[:, 384 + h * 64:384 + (h + 1) * 64], start=True, stop=True),
                    r=[(tm, "KE"), (tm, "V")], w=[pkv])
            col = n * 64 + 63
            decB = GEB[:].rearrange("p (h t) -> p h t", h=6)[:, :, col:col + 1].broadcast_to([32, 6, 64])
            S3 = gS[0:32, :].rearrange("p (h v) -> p h v", h=6)
            P.op("dve", lambda e, S3=S3, decB=decB: e.tensor_tensor(out=S3, in0=S3, in1=decB, op=ALU.mult),
                 r=[gS, GEB], w=[gS])
            P.op("dve", lambda e: e.tensor_tensor(out=gS[0:32, :], in0=gS[0:32, :], in1=pkv[0:32, 0:384], op=ALU.add),
                 r=[gS, pkv], w=[gS])
        NPS = [PS[3], PS[4], PS[5]]
        NT = [(self.tget(), self.tget()) for _ in range(3)]
        for jj in range(3):
            osb, sq = NT[jj]
            P.op("act", lambda e, osb=osb, jj=jj: e.activation(out=osb[:], in_=OT[jj][:], func=AF.Copy), r=[OT[jj]], w=[osb])
            P.op("act", lambda e, sq=sq, jj=jj: e.activation(out=sq[:], in_=OT[jj][:], func=AF.Square), r=[OT[jj]], w=[sq])
        for jj in range(3):
            osb, sq = NT[jj]
            pss = NPS[jj]
            P.op("pe", lambda e, sq=sq, pss=pss: e.matmul(pss[:], lhsT=self.C("blk64"), rhs=sq[:], start=True, stop=True),
                 r=[self.cst, sq], w=[pss])
        for jj in range(3):
            osb, sq = NT[jj]
            pss = NPS[jj]
            P.op("act", lambda e, sq=sq, pss=pss: e.activation(out=sq[:], in_=pss[:], func=AF.Ln, bias=self.epsc[:, 0:1],
                                                               scale=1.0 / 64), r=[pss, self.epsc], w=[sq])
        for jj in range(3):
            osb, sq = NT[jj]
            P.op("act", lambda e, sq=sq: e.activation(out=sq[:], in_=sq[:], func=AF.Exp, scale=-0.5), r=[sq], w=[sq])
        for jj in range(3):
            osb, sq = NT[jj]
            P.op("dve", lambda e, osb=osb, sq=sq: e.scalar_tensor_tensor(
                out=osb[:], in0=osb[:], scalar=self.PK("glanw%d" % l), in1=sq[:], op0=ALU.mult, op1=ALU.mult),
                r=[osb, sq, self.pk], w=[osb])
        for jj in range(3):
            osb, sq = NT[jj]
            pgz = NPS[jj]
            self.proj_fm(pgz, O_GZ + jj * 128, 128)
            P.op("act", lambda e, sq=sq, pgz=pgz: e.activation(out=sq[:], in_=pgz[:], func=AF.Silu), r=[pgz], w=[sq])
        for jj in range(3):
            osb, sq = NT[jj]
            P.op("dve", lambda e, osb=osb, sq=sq, jj=jj: e.tensor_tensor(out=self.MT[:, 0 + jj, :], in0=osb[:], in1=sq[:],
                                                                         op=ALU.mult), r=[osb, sq], w=[(self.MT, 0 + jj)])
        for a_, b_ in NT:
            self.tput(a_, b_)
        P.pop_scope(barrier=False)

    def dn_setup(self, l):
        P = self.P
        self.dS = P.sb("dn_S", [64, 384], F32)
        P.op("pool", lambda e: e.memset(self.dS[:], 0.0), w=[self.dS])
        self.dHist = P.sb("dn_hist", [128, 9, 3], F32)
        P.op("pool", lambda e: e.memset(self.dHist[:], 0.0), w=[self.dHist])
        self.dnA = P.sb("dn_nA", [6, 1], F32)
        P.op("act", lambda e: e.activation(out=self.dnA[:], in_=self.PK("dnalog%d" % l, rows=6), func=AF.Exp),
             r=[self.pk], w=[self.dnA])
        P.op("dve", lambda e: e.tensor_scalar(out=self.dnA[:], in0=self.dnA[:], scalar1=-1.0, scalar2=None, op0=ALU.mult),
             r=[self.dnA], w=[self.dnA])

    def dn_block(self, l, b):
        P = self.P
        PS = self.PSA
        NCH = TBA // CH
        dS, dHist = self.dS, self.dHist
        P.push_scope()
        C = [P.sb("dn_c%d" % j, [128, TBA], F32) for j in range(9)]
        P.push_scope()
        DX = P.sb("dn_x", [128, 9, 3 + TBA], F32)
        P.op("dve", lambda e: e.tensor_copy(out=DX[:, :, 0:3], in_=dHist[:]), r=[dHist], w=[DX])
        for j in range(9):
            pb = PS[3 + j % 4]
            self.proj_fm(pb, O_DQ + j * 128, 128)
            P.op("act", lambda e, j=j, pb=pb: e.activation(out=DX[:, j, 3:3 + TBA], in_=pb[:], func=AF.Copy), r=[pb], w=[DX])
        P.op("dve", lambda e: e.tensor_copy(out=dHist[:], in_=DX[:, :, TBA:TBA + 3]), r=[DX], w=[dHist])
        wv = lambda j, t: self.PK("dncw%d" % l, col=j * 4 + t)
        for j in range(9):
            P.op("dve", lambda e, j=j: e.tensor_scalar(out=C[j][:], in0=DX[:, j, 0:TBA], scalar1=wv(j, 0), scalar2=None,
                                                       op0=ALU.mult), r=[DX, self.pk], w=[C[j]])
        for t in range(1, 4):
            for j in range(9):
                P.op("dve", lambda e, j=j, t=t: e.scalar_tensor_tensor(out=C[j][:], in0=DX[:, j, t:t + TBA], scalar=wv(j, t),
                                                                       in1=C[j][:], op0=ALU.mult, op1=ALU.add),
                     r=[DX, self.pk, C[j]], w=[C[j]])
        for j in range(9):
            P.op("act", lambda e, j=j: e.activation(out=C[j][:], in_=C[j][:], func=AF.Silu), r=[C[j]], w=[C[j]])
        P.pop_scope(barrier=False)
        P.push_scope()
        SQs = [P.sb("dn_sq%d" % j, [128, TBA], F32) for j in range(6)]
        pss = [PS[j] for j in range(6)]
        for j in range(6):
            P.op("act", lambda e, j=j: e.activation(out=SQs[j][:], in_=C[j][:], func=AF.Square), r=[C[j]], w=[SQs[j]])
        for j in range(6):
            P.op("pe", lambda e, j=j: e.matmul(pss[j][:], lhsT=self.C("blk64"), rhs=SQs[j][:], start=True, stop=True),
                 r=[self.cst, SQs[j]], w=[pss[j]])
        for j in range(6):
            P.op("act", lambda e, j=j: e.activation(out=SQs[j][:], in_=pss[j][:], func=AF.Ln, bias=self.epsc[:, 0:1], scale=1.0),
                 r=[pss[j], self.epsc], w=[SQs[j]])
        for j in range(6):
            P.op("act", lambda e, j=j: e.activation(out=SQs[j][:], in_=SQs[j][:], func=AF.Exp, scale=-0.5), r=[SQs[j]], w=[SQs[j]])
        for j in range(6):
            P.op("dve", lambda e, j=j: e.scalar_tensor_tensor(
                out=C[j][:], in0=C[j][:], scalar=(0.125 if j < 3 else 1.0), in1=SQs[j][:], op0=ALU.mult, op1=ALU.mult),
                r=[C[j], SQs[j]], w=[C[j]])
        P.pop_scope(barrier=False)
        QN, KN = C[0:3], C[3:6]
        BT = P.sb("dn_bt", [6, TBA], F32)
        GC = P.sb("dn_gc", [6, TBA], F32)
        EG = P.sb("dn_eg", [6, TBA], F32)
        BG = P.sb("dn_bg", [6, TBA], F32)
        ER = P.sb("dn_er", [6, TBA], F32)
        T1 = P.sb("dn_t1", [6, TBA], F32)
        T2 = P.sb("dn_t2", [6, TBA], F32)
        NG = P.sb("dn_ng", [6, 6, TBA], F32)
        pb = PS[3]
        self.proj_fm(pb, O_DB, 6)
        P.op("act", lambda e: e.activation(out=BT[:], in_=pb[0:6, :], func=AF.Sigmoid), r=[pb], w=[BT])
        pa = PS[4]
        self.proj_fm(pa, O_DA, 6)
        P.op("act", lambda e: e.activation(out=T2[:], in_=pa[0:6, :], func=AF.Identity,
                                           bias=self.PK("dndtb%d" % l, rows=6), scale=1.0), r=[pa, self.pk], w=[T2])
        P.op("act", lambda e: e.activation(out=T1[:], in_=T2[:], func=AF.Abs), r=[T2], w=[T1])
        P.op("act", lambda e: e.activation(out=T1[:], in_=T1[:], func=AF.Exp, scale=-1.0), r=[T1], w=[T1])
        P.op("act", lambda e: e.activation(out=T1[:], in_=T1[:], func=AF.Ln, bias=1.0, scale=1.0), r=[T1], w=[T1])
        P.op("dve", lambda e: e.tensor_scalar(out=T2[:], in0=T2[:], scalar1=0.0, scalar2=None, op0=ALU.max), r=[T2], w=[T2])
        P.op("dve", lambda e: e.tensor_tensor(out=T1[:], in0=T1[:], in1=T2[:], op=ALU.add), r=[T1, T2], w=[T1])
        P.op("dve", lambda e: e.tensor_scalar(out=T2[:], in0=T1[:], scalar1=self.dnA[:, 0:1], scalar2=None, op0=ALU.mult),
             r=[T1, self.dnA], w=[T2])
        P.op("dve", lambda e: e.tensor_tensor_scan(out=GC[:], data0=self.C("rst", rows=6)[:, 0:TBA], data1=T2[:], initial=0.0,
                                                   op0=ALU.mult, op1=ALU.add), r=[T2, self.cst], w=[GC])
        P.op("act", lambda e: e.activation(out=EG[:], in_=GC[:], func=AF.Exp), r=[GC], w=[EG])
        P.op("dve", lambda e: e.tensor_tensor(out=BG[:], in0=BT[:], in1=EG[:], op=ALU.mult), r=[BT, EG], w=[BG])
        for n in range(NCH):
            col = n * 64 + 63
            P.op("dve", lambda e, n=n, col=col: e.tensor_scalar(
                out=ER[:, n * 64:(n + 1) * 64], in0=GC[:, n * 64:(n + 1) * 64], scalar1=-1.0, scalar2=GC[:, col:col + 1],
                op0=ALU.mult, op1=ALU.add), r=[GC], w=[ER])
        P.op("act", lambda e: e.activation(out=ER[:], in_=ER[:], func=AF.Exp), r=[ER], w=[ER])
        for h in range(6):
            P.op("dve", lambda e, h=h: e.tensor_scalar(out=NG[:, h, :], in0=GC[:], scalar1=self.C("noh6", rows=6)[:, h:h + 1],
                                                       scalar2=None, op0=ALU.mult), r=[GC, self.cst], w=[NG])
        EGd = P.sb("dn_egd", [6, NCH * 6], F32)
        DEC = P.sb("dn_dec", [64, NCH * 6], F32)
        for n in range(NCH):
            col = n * 64 + 63
            P.op("dve", lambda e, n=n, col=col: e.tensor_scalar(
                out=EGd[:, n * 6:(n + 1) * 6], in0=self.C("oh6", rows=6), scalar1=EG[:, col:col + 1], scalar2=None,
                op0=ALU.mult), r=[EG, self.cst], w=[EGd])
        pdx = PS[5]
        P.op("pe", lambda e: e.matmul(pdx[0:64, 0:NCH * 6], lhsT=self.C("ones", rows=6)[:, 0:64], rhs=EGd[:],
                                      start=True, stop=True), r=[self.cst, EGd], w=[pdx])
        P.op("act", lambda e: e.activation(out=DEC[:], in_=pdx[0:64, 0:NCH * 6], func=AF.Copy), r=[pdx], w=[DEC])
        KB = [P.sb("dn_kb%d" % j, [128, TBA], F32) for j in range(3)]
        KG = [P.sb("dn_kg%d" % j, [128, TBA], F32) for j in range(3)]
        QG = [P.sb("dn_qg%d" % j, [128, TBA], F32) for j in range(3)]
        KR = [P.sb("dn_kr%d" % j, [128, TBA], F32) for j in range(3)]
        VB = C[6:9]
        nb = [0]

        def bprod(fld, dst, src, j):
            pbx = PS[3 + nb[0] % 4]
            nb[0] += 1
            P.op("pe", lambda e: e.matmul(pbx[:], lhsT=self.C("e6_%d" % j, rows=6), rhs=fld[:], start=True, stop=True),
                 r=[self.cst, fld], w=[pbx])
            P.op("dve", lambda e: e.tensor_tensor(out=dst[:], in0=src[:], in1=pbx[:], op=ALU.mult), r=[src, pbx], w=[dst])

        for j in range(3):
            bprod(BT, KB[j], KN[j], j)
            bprod(BG, KG[j], KN[j], j)
            bprod(BT, VB[j], VB[j], j)
            bprod(EG, QG[j], QN[j], j)
            bprod(ER, KR[j], KN[j], j)
        XO = {}
        for nm, X in (("kn", KN), ("kb", KB), ("qn", QN), ("qg", QG)):
            xo = P.sb("dn_xo_" + nm, [64, 3 * TBA], F32)
            XO[nm] = xo
            for j in range(3):
                px = PS[3 + nb[0] % 4]
                nb[0] += 1
                P.op("pe", lambda e, px=px, X=X, j=j: e.matmul(px[0:64, :], lhsT=self.C("selhi"), rhs=X[j][:],
                                                               start=True, stop=True), r=[self.cst, X[j]], w=[px])
                P.op("act", lambda e, px=px, xo=xo, j=j: e.activation(out=xo[:, j * TBA:(j + 1) * TBA], in_=px[0:64, :],
                                                                      func=AF.Copy), r=[px], w=[xo])
        XE = {"kn": KN, "kb": KB, "qn": QN, "qg": QG}

        def hd(nm, h, ctok):
            j = h // 2
            if h % 2 == 0:
                return XE[nm][j][0:64, ctok], XE[nm][j]
            return XO[nm][:, j * TBA + ctok.start:j * TBA + ctok.stop], XO[nm]

        NIL = int(os.environ.get("DN_NIL", "2"))
        TMa = [P.sb("dn_tma%d" % p, [64, 768], F32) for p in range(NIL)]
        KRt = [P.sb("dn_krt%d" % n, [64, 384], F32) for n in range(NCH)]
        QK = [P.sb("dn_qk%d" % n, [64, 384], F32) for n in range(NCH)]
        U = [P.sb("dn_u%d" % n, [64, 384], F32) for n in range(NCH)]
        WT = [P.sb("dn_wt%d" % n, [64, 384], F32) for n in range(NCH)]
        DT = [P.sb("dn_dt%d" % p, [64, 384], F32) for p in range(NIL)]
        Mt = [[P.sb("dn_m%d_%d" % (p, i), [64, 384], F32) for i in range(2)] for p in range(NIL)]
        Nt = [[P.sb("dn_n%d_%d" % (p, i), [64, 384], F32) for i in range(2)] for p in range(NIL)]
        W = [P.sb("dn_w%d" % p, [64, 384], F32) for p in range(NIL)]
        VNEW = P.sb("dn_vnew", [64, 384], F32)
        id64 = self.C("ident", rows=64)[:, 0:64]
        hcs = [slice(h * 64, (h + 1) * 64) for h in range(6)]
        bk = [0]

        def nbank():
            bk[0] += 1
            return PS[3 + bk[0] % 5]

        def st_transpose(n, p):
            ctok = slice(n * 64, (n + 1) * 64)
            for qi, (X, dst, dcol, dep) in enumerate(((KG, TMa[p], 0, (TMa[p], 0)), (VB, TMa[p], 384, (TMa[p], 1)),
                                                       (KR, KRt[n], 0, KRt[n]))):
                pt = nbank()
                for j in range(3):
                    P.op("pe", lambda e, pt=pt, X=X, j=j: e.transpose(out=pt[0:64, j * 128:(j + 1) * 128], in_=X[j][:, ctok],
                                                                      identity=self.C("ident")), r=[X[j], self.cst], w=[pt])
                P.op("act", lambda e, pt=pt, dst=dst, dcol=dcol: e.activation(out=dst[:, dcol:dcol + 384], in_=pt[0:64, 0:384],
                                                                              func=AF.Copy), r=[pt], w=[dep])

        def st_decay(n, p):
            ctok = slice(n * 64, (n + 1) * 64)
            pd = nbank()
            for h in range(6):
                oh_ = pd[0:64, hcs[h]]
                P.op("pe", lambda e, oh_=oh_: e.matmul(oh_, lhsT=id64, rhs=self.C("negm", rows=64), start=True, stop=False),
                     r=[self.cst], w=[pd])
                P.op("pe", lambda e, oh_=oh_, h=h: e.matmul(oh_, lhsT=self.C("selh_%d" % h, rows=6), rhs=GC[:, ctok],
                                                            start=False, stop=False), r=[self.cst, GC], w=[pd])
                P.op("pe", lambda e, oh_=oh_, h=h: e.matmul(oh_, lhsT=NG[:, h, ctok], rhs=self.C("ones", rows=6)[:, 0:64],
                                                            start=False, stop=True), r=[self.cst, NG], w=[pd])
            P.op("act", lambda e: e.activation(out=DT[p][:], in_=pd[0:64, 0:384], func=AF.Exp), r=[pd], w=[DT[p]])

        def st_kk(n, p):
            ctok = slice(n * 64, (n + 1) * 64)
            pk_ = nbank()
            pq = nbank()
            for h in range(6):
                kn_ap, kn_d = hd("kn", h, ctok)
                kb_ap, kb_d = hd("kb", h, ctok)
                qn_ap, qn_d = hd("qn", h, ctok)
                P.op("pe", lambda e, h=h, kn_ap=kn_ap, kb_ap=kb_ap: e.matmul(pk_[0:64, hcs[h]], lhsT=kn_ap, rhs=kb_ap,
                                                                             start=True, stop=True), r=[kn_d, kb_d], w=[pk_])
                P.op("pe", lambda e, h=h, kn_ap=kn_ap, qn_ap=qn_ap: e.matmul(pq[0:64, hcs[h]], lhsT=kn_ap, rhs=qn_ap,
                                                                             start=True, stop=True), r=[kn_d, qn_d], w=[pq])
            M = Mt[p][0]
            P.op("dve", lambda e: e.tensor_tensor(out=M[:], in0=pk_[0:64, 0:384], in1=DT[p][:], op=ALU.mult),
                 r=[pk_, DT[p]], w=[M])
            P.op("pool", lambda e: e.tensor_tensor(out=M[:], in0=M[:], in1=self.C("sut6", rows=64), op=ALU.mult),
                 r=[M, self.cst], w=[M])
            P.op("dve", lambda e: e.tensor_tensor(out=QK[n][:], in0=pq[0:64, 0:384], in1=DT[p][:], op=ALU.mult),
                 r=[pq, DT[p]], w=[QK[n]])

        def st_n0(n, p):
            M, N = Mt[p][0], Nt[p][0]
            pn = nbank()
            for h in range(6):
                P.op("pe", lambda e, h=h: e.transpose(out=pn[0:64, hcs[h]], in_=M[:, hcs[h]], identity=id64),
                     r=[M, self.cst], w=[pn])
            P.op("act", lambda e: e.activation(out=N[:], in_=pn[0:64, 0:384], func=AF.Copy), r=[pn], w=[N])
            P.op("dve", lambda e: e.tensor_tensor(out=W[p][:], in0=self.C("id6", rows=64), in1=M[:], op=ALU.subtract),
                 r=[M, self.cst], w=[W[p]])

        def st_level(i):
            def f(n, p):
                M, N = Mt[p][i % 2], Nt[p][i % 2]
                Mn, Nn = Mt[p][(i + 1) % 2], Nt[p][(i + 1) % 2]
                pn = nbank()
                for h in range(6):
                    P.op("pe", lambda e, h=h: e.matmul(pn[0:64, hcs[h]], lhsT=M[:, hcs[h]], rhs=N[:, hcs[h]],
                                                       start=True, stop=True), r=[M, N], w=[pn])
                P.op("act", lambda e: e.activation(out=Nn[:], in_=pn[0:64, 0:384], func=AF.Copy), r=[pn], w=[Nn])
                if i < 4:
                    pm = nbank()
                    for h in range(6):
                        P.op("pe", lambda e, h=h: e.matmul(pm[0:64, hcs[h]], lhsT=N[:, hcs[h]], rhs=M[:, hcs[h]],
                                                           start=True, stop=True), r=[M, N], w=[pm])
                    P.op("dve", lambda e: e.tensor_copy(out=Mn[:], in_=pm[0:64, 0:384]), r=[pm], w=[Mn])
            return f

        def st_wupd(i):
            def f(n, p):
                Nn = Nt[p][(i + 1) % 2]
                pw = nbank()
                for h in range(6):
                    P.op("pe", lambda e, h=h: e.matmul(pw[0:64, hcs[h]], lhsT=Nn[:, hcs[h]], rhs=W[p][:, hcs[h]],
                                                       start=True, stop=True), r=[Nn, W[p]], w=[pw])
                P.op("dve", lambda e: e.tensor_tensor(out=W[p][:], in0=pw[0:64, 0:384], in1=W[p][:], op=ALU.add),
                     r=[pw, W[p]], w=[W[p]])
            return f

        def st_uw(n, p):
            pu = nbank()
            pw_ = nbank()
            for h in range(6):
                P.op("pe", lambda e, h=h: e.matmul(pu[0:64, hcs[h]], lhsT=W[p][:, hcs[h]],
                                                   rhs=TMa[p][:, 384 + h * 64:384 + (h + 1) * 64],
                                                   start=True, stop=True), r=[W[p], (TMa[p], 1)], w=[pu])
                P.op("pe", lambda e, h=h: e.matmul(pw_[0:64, hcs[h]], lhsT=TMa[p][:, h * 64:(h + 1) * 64], rhs=W[p][:, hcs[h]],
                                                   start=True, stop=True), r=[W[p], (TMa[p], 0)], w=[pw_])
            P.op("act", lambda e: e.activation(out=U[n][:], in_=pu[0:64, 0:384], func=AF.Copy), r=[pu], w=[U[n]])
            P.op("dve", lambda e: e.tensor_copy(out=WT[n][:], in_=pw_[0:64, 0:384]), r=[pw_], w=[WT[n]])

        stages = [st_transpose, st_decay, st_kk, st_n0]
        for i in range(int(os.environ.get("DN_LEVELS", "5"))):
            stages += [st_level(i), st_wupd(i)]
        stages.append(st_uw)
        for g0 in range(0, NCH, NIL):
            for st in stages:
                for p in range(NIL):
                    st(g0 + p, p)
        OT = [PS[0], PS[1], PS[2]]
        pv = PS[3]
        pkv = PS[4]
        for n in range(NCH):
            ctok = slice(n * 64, (n + 1) * 64)
            for h in range(6):
                P.op("pe", lambda e, h=h, n=n: e.matmul(pv[0:64, hcs[h]], lhsT=WT[n][:, hcs[h]], rhs=dS[:, hcs[h]],
                                                        start=True, stop=True), r=[WT[n], dS], w=[pv])
            P.op("dve", lambda e, n=n: e.tensor_tensor(out=VNEW[:], in0=U[n][:], in1=pv[0:64, 0:384], op=ALU.subtract),
                 r=[U[n], pv], w=[VNEW])
            for h in range(6):
                oap = OT[h // 2][(h % 2) * 64:(h % 2 + 1) * 64, ctok]
                qg_ap, qg_d = hd("qg", h, ctok)
                P.op("pe", lambda e, h=h, oap=oap, qg_ap=qg_ap: e.matmul(oap, lhsT=dS[:, hcs[h]], rhs=qg_ap,
                                                                         start=True, stop=False), r=[dS, qg_d], w=[OT[h // 2]])
                P.op("pe", lambda e, h=h, oap=oap, n=n: e.matmul(oap, lhsT=VNEW[:, hcs[h]], rhs=QK[n][:, hcs[h]],
                                                                 start=False, stop=True), r=[VNEW, QK[n]], w=[OT[h // 2]])
            for h in range(6):
                P.op("pe", lambda e, h=h, n=n: e.matmul(pkv[0:64, hcs[h]], lhsT=KRt[n][:, hcs[h]], rhs=VNEW[:, hcs[h]],
                                                        start=True, stop=True), r=[KRt[n], VNEW], w=[pkv])
            S3 = dS[:].rearrange("p (h v) -> p h v", h=6)
            decB = DEC[:, n * 6:(n + 1) * 6].rearrange("p (h o) -> p h o", o=1).broadcast_to([64, 6, 64])
            P.op("dve", lambda e, S3=S3, decB=decB: e.tensor_tensor(out=S3, in0=S3, in1=decB, op=ALU.mult), r=[dS, DEC], w=[dS])
            P.op("dve", lambda e: e.tensor_tensor(out=dS[:], in0=dS[:], in1=pkv[0:64, 0:384], op=ALU.add), r=[dS, pkv], w=[dS])
        NPS = [PS[5], PS[6], PS[7]]
        NT = [(C[jj], C[3 + jj]) for jj in range(3)]
        for jj in range(3):
            osb, sq = NT[jj]
            P.op("act", lambda e, osb=osb, jj=jj: e.activation(out=osb[:], in_=OT[jj][:], func=AF.Copy), r=[OT[jj]], w=[osb])
            P.op("act", lambda e, sq=sq, jj=jj: e.activation(out=sq[:], in_=OT[jj][:], func=AF.Square), r=[OT[jj]], w=[sq])
        for jj in range(3):
            osb, sq = NT[jj]
            pss = NPS[jj]
            P.op("pe", lambda e, sq=sq, pss=pss: e.matmul(pss[:], lhsT=self.C("blk64"), rhs=sq[:], start=True, stop=True),
                 r=[self.cst, sq], w=[pss])
        for jj in range(3):
            osb, sq = NT[jj]
            pss = NPS[jj]
            P.op("act", lambda e, sq=sq, pss=pss: e.activation(out=sq[:], in_=pss[:], func=AF.Ln, bias=self.epsc[:, 0:1],
                                                               scale=1.0 / 64), r=[pss, self.epsc], w=[sq])
        for jj in range(3):
            osb, sq = NT[jj]
            P.op("act", lambda e, sq=sq: e.activation(out=sq[:], in_=sq[:], func=AF.Exp, scale=-0.5), r=[sq], w=[sq])
        for jj in range(3):
            osb, sq = NT[jj]
            P.op("dve", lambda e, osb=osb, sq=sq: e.scalar_tensor_tensor(
                out=osb[:], in0=osb[:], scalar=self.PK("dnnw%d" % l), in1=sq[:], op0=ALU.mult, op1=ALU.mult),
                r=[osb, sq, self.pk], w=[osb])
        for jj in range(3):
            osb, sq = NT[jj]
            pgz = NPS[jj]
            self.proj_fm(pgz, O_DZ + jj * 128, 128)
            P.op("act", lambda e, sq=sq, pgz=pgz: e.activation(out=sq[:], in_=pgz[:], func=AF.Silu), r=[pgz], w=[sq])
        for jj in range(3):
            osb, sq = NT[jj]
            P.op("dve", lambda e, osb=osb, sq=sq, jj=jj: e.tensor_tensor(out=self.MT[:, 5 + jj, :], in0=osb[:], in1=sq[:],
                                                                         op=ALU.mult), r=[osb, sq], w=[(self.MT, 5 + jj)])
        P.pop_scope(barrier=False)

    def passB(self, l, last):
        P = self.P
        S, SBT = self.S, self.SBT
        moe = (l % 2 == 1)
        P.push_scope()
        nbs = SBT // TB
        H2 = P.sb("B_H2", [128, NK, SBT], BF16)
        YACC = P.sb("B_YACC", [128, NK, SBT], F32)
        XT = P.sb("B_XT", [128, NK, TB], F32)
        SQ = P.sb("B_SQ", [128, NK, TB], BF16)
        rstd = P.sb("B_rstd", [128, TB], F32)
        tmp = [P.sb("B_tmp%d" % i, [128, TB], F32) for i in range(2)]
        WG = [P.sb("B_WG%d" % i, [128, NK, 512], BF16) for i in range(2)]
        WU = [P.sb("B_WU%d" % i, [128, NK, 512], BF16) for i in range(2)]
        WD = [P.sb("B_WD%d" % i, [128, 4, D], BF16) for i in range(2)]
        AT = [P.sb("B_AT%d" % i, [128, 4, TB], BF16) for i in range(2)]
        SG = [P.sb("B_SG%d" % i, [128, TB], F32) for i in range(2)]
        mod = self.mod[l]
        if moe:
            CB = P.sb("B_cstb", [8, self.ccb.n], F32)
            P.dma("sp", CB[:], self.cstb_d[0:8, :], r=[self.cstb_d], w=[CB])
            HE = [P.sb("B_HE%d" % i, [128, NK, TB], BF16) for i in range(2)]
            RW = P.sb("B_RW", [128, NK, NE], F32)
            P.dma("sp", RW[:], self.router_w[0].rearrange("(k p) n -> p k n", p=128), r=[self.router_w], w=[RW])
            GT = P.sb("B_GT", [128, SBT // 128, NE], F32)
            GF = P.sb("B_GF", [NE, SBT], F32)
            gs = [P.sb("B_gs%d" % i, [128, NE], F32) for i in range(4)]
            gm = [P.sb("B_gm%d" % i, [128, 1], F32) for i in range(4)]
            experts = list(range(NE))
            dff = D_FFE
        else:
            experts = [0]
            dff = D_FF
        fgs = []
        f0 = 0
        while f0 < dff:
            fs = min(512, dff - f0)
            fgs.append((f0, fs))
            f0 += fs
        nsb = S // SBT
        for sb in range(nsb):
            for bi in range(nbs):
                b = sb * nbs + bi
                if moe:
                    lgp = self.PSB[6]
                    first = [True]

                    def hf_cb(k, t, bi=bi, lgp=lgp, first=first):
                        for tt in range(4):
                            st = first[0]
                            first[0] = False
                            P.op("pe", lambda e, k=k, t=t, tt=tt, st=st: e.matmul(
                                lgp[:, tt * NE:(tt + 1) * NE], lhsT=t[:, tt * 128:(tt + 1) * 128], rhs=RW[:, k, :],
                                start=st, stop=(k == NK - 1), skip_group_check=True), r=[t, RW], w=[lgp])
                else:
                    hf_cb = None
                self.load_norm(XT, b, self.gv2[l], lambda k: mod[:, 24 + k:24 + k + 1],
                               lambda k, bi=bi: H2[:, k, bi * TB:(bi + 1) * TB], (H2, bi), SQ, tmp, rstd, hf_cb=hf_cb)
                if moe:
                    for tt in range(4):
                        ti = bi * 4 + tt
                        lg = lgp[:, tt * NE:(tt + 1) * NE]
                        g0, g1_, g2_, g3_ = gs
                        m1, m2, sm, _ = gm
                        P.op("dve", lambda e, lg=lg: e.tensor_copy(out=g0[:], in_=lg), r=[lgp], w=[g0])
                        P.op("dve", lambda e: e.tensor_reduce(out=m1[:], in_=g0[:], axis=AX.X, op=ALU.max), r=[g0], w=[m1])
                        P.op("dve", lambda e: e.tensor_scalar(out=g1_[:], in0=g0[:], scalar1=m1[:, 0:1], scalar2=-1e30,
                                                              op0=ALU.is_ge, op1=ALU.mult), r=[g0, m1], w=[g1_])
                        P.op("dve", lambda e: e.tensor_tensor(out=g2_[:], in0=g0[:], in1=g1_[:], op=ALU.add), r=[g0, g1_], w=[g2_])
                        P.op("dve", lambda e: e.tensor_reduce(out=m2[:], in_=g2_[:], axis=AX.X, op=ALU.max), r=[g2_], w=[m2])
                        P.op("dve", lambda e: e.tensor_scalar(out=g1_[:], in0=g0[:], scalar1=m2[:, 0:1], scalar2=None,
                                                              op0=ALU.is_ge), r=[g0, m2], w=[g1_])
                        P.op("dve", lambda e: e.tensor_scalar(out=g2_[:], in0=g0[:], scalar1=m1[:, 0:1], scalar2=None,
                                                              op0=ALU.subtract), r=[g0, m1], w=[g2_])
                        P.op("act", lambda e: e.activation(out=g2_[:], in_=g2_[:], func=AF.Exp), r=[g2_], w=[g2_])
                        P.op("dve", lambda e: e.tensor_tensor(out=g2_[:], in0=g2_[:], in1=g1_[:], op=ALU.mult), r=[g2_, g1_], w=[g2_])
                        P.op("dve", lambda e: e.tensor_reduce(out=sm[:], in_=g2_[:], axis=AX.X, op=ALU.add), r=[g2_], w=[sm])
                        P.op("dve", lambda e: e.reciprocal(out=sm[:], in_=sm[:]), r=[sm], w=[sm])
                        P.op("dve", lambda e, ti=ti: e.tensor_scalar(out=GT[:, ti, :], in0=g2_[:], scalar1=sm[:, 0:1],
                                                                     scalar2=None, op0=ALU.mult), r=[g2_, sm], w=[GT])
                        tp = self.PSB[7]
                        P.op("pe", lambda e, ti=ti, tp=tp: e.transpose(out=tp[0:NE, 0:128], in_=GT[:, ti, :],
                                                                       identity=self.C("ident")), r=[GT, self.cst], w=[tp])
                        P.op("act", lambda e, ti=ti, tp=tp: e.activation(out=GF[:, ti * 128:(ti + 1) * 128],
                                                                         in_=tp[0:NE, 0:128], func=AF.Copy), r=[tp], w=[GF])
            units = []
            for ei, ex in enumerate(experts):
                for fi, (f0, fs) in enumerate(fgs):
                    for bi in range(nbs):
                        units.append((ei, ex, fi, f0, fs, bi))
            wslot = {}
            nload = [0]

            def load_w(ex, fi, f0, fs):
                s = nload[0] % 2
                nload[0] += 1
                wslot[(ex, fi)] = s
                nfc = fs // 128
                if moe:
                    wg, wu, wd = self.moe_wg[0, ex], self.moe_wu[0, ex], self.moe_wd[0, ex]
                else:
                    wg, wu, wd = self.ffn_wg[0], self.ffn_wu[0], self.ffn_wd[0]
                P.dma("pool", WG[s][:, :, 0:fs], wg[:, f0:f0 + fs].rearrange("(k p) n -> p k n", p=128),
                      r=[self.moe_wg], w=[WG[s]])
                P.dma("pool", WU[s][:, :, 0:fs], wu[:, f0:f0 + fs].rearrange("(k p) n -> p k n", p=128),
                      r=[self.moe_wg], w=[WU[s]])
                P.dma("pool", WD[s][:, 0:nfc, :], wd[f0:f0 + fs, :].rearrange("(k p) n -> p k n", p=128),
                      r=[self.moe_wg], w=[WD[s]])

            efs = [(ex, fi, f0, fs) for ex in experts for fi, (f0, fs) in enumerate(fgs)]
            load_w(*efs[0])
            he_slot = {}
            nhe = [0]
            gcount = [0]
            ycount = [0]

            def stage1(u, ui):
                ei, ex, fi, f0, fs, bi = u
                s = wslot[(ex, fi)]
                nfc = fs // 128
                tok = slice(bi * TB, (bi + 1) * TB)
                if moe:
                    if fi == 0:
                        hs_ = nhe[0] % 2
                        nhe[0] += 1
                        he_slot[(ex, bi)] = hs_
                        gb = self.PSB[7]
                        P.op("pe", lambda e, ex=ex, tok=tok, gb=gb: e.matmul(
                            gb[:], lhsT=CB[:, self.ccb.off["sel8_%d" % ex][0]:self.ccb.off["sel8_%d" % ex][0] + 128],
                            rhs=GF[:, tok], start=True, stop=True), r=[CB, GF], w=[gb])
                        P.op("act", lambda e, gb=gb: e.activation(out=rstd[:], in_=gb[:], func=AF.Copy), r=[gb], w=[rstd])
                        for k in range(NK):
                            P.op("dve", lambda e, k=k, hs_=hs_, tok=tok: e.tensor_tensor(
                                out=HE[hs_][:, k, :], in0=H2[:, k, tok], in1=rstd[:], op=ALU.mult),
                                r=[(H2, bi), rstd], w=[HE[hs_]])
                    hu = HE[he_slot[(ex, bi)]]
                at = AT[ui % 2]
                for fc in range(nfc):
                    pg = self.PSB[gcount[0] % 2]
                    pu = self.PSB[2 + gcount[0] % 2]
                    sg = SG[gcount[0] % 2]
                    gcount[0] += 1
                    for k in range(NK):
                        P.op("pe", lambda e, k=k, fc=fc, pg=pg, s=s, tok=tok: e.matmul(
                            pg[:], lhsT=WG[s][:, k, fc * 128:(fc + 1) * 128], rhs=H2[:, k, tok],
                            start=(k == 0), stop=(k == NK - 1)), r=[WG[s], (H2, bi)], w=[pg])
                    for k in range(NK):
                        if moe:
                            P.op("pe", lambda e, k=k, fc=fc, pu=pu, s=s, hu=hu: e.matmul(
                                pu[:], lhsT=WU[s][:, k, fc * 128:(fc + 1) * 128], rhs=hu[:, k, :],
                                start=(k == 0), stop=(k == NK - 1)), r=[WU[s], hu], w=[pu])
                        else:
                            P.op("pe", lambda e, k=k, fc=fc, pu=pu, s=s, tok=tok: e.matmul(
                                pu[:], lhsT=WU[s][:, k, fc * 128:(fc + 1) * 128], rhs=H2[:, k, tok],
                                start=(k == 0), stop=(k == NK - 1)), r=[WU[s], (H2, bi)], w=[pu])
                    P.op("act", lambda e, sg=sg, pg=pg: e.activation(out=sg[:], in_=pg[:], func=AF.Silu), r=[pg], w=[sg])
                    P.op("dve", lambda e, sg=sg, pu=pu, at=at, fc=fc: e.tensor_tensor(
                        out=at[:, fc, :], in0=pu[:], in1=sg[:], op=ALU.mult), r=[pu, sg], w=[at])

            def stage2(u, ui):
                ei, ex, fi, f0, fs, bi = u
                s = wslot[(ex, fi)]
                nfc = fs // 128
                at = AT[ui % 2]
                tok = slice(bi * TB, (bi + 1) * TB)
                firstacc = (ei == 0 and fi == 0)
                for dc in range(NK):
                    py = self.PSB[4 + ycount[0] % 4]
                    ycount[0] += 1
                    for fc in range(nfc):
                        P.op("pe", lambda e, fc=fc, dc=dc, py=py, s=s, at=at: e.matmul(
                            py[:], lhsT=WD[s][:, fc, dc * 128:(dc + 1) * 128], rhs=at[:, fc, :],
                            start=(fc == 0), stop=(fc == nfc - 1)), r=[WD[s], at], w=[py])
                    if firstacc:
                        P.op("act", lambda e, dc=dc, py=py, tok=tok: e.activation(
                            out=YACC[:, dc, tok], in_=py[:], func=AF.Copy), r=[py], w=[(YACC, (bi, dc))])
                    else:
                        P.op("dve", lambda e, dc=dc, py=py, tok=tok: e.tensor_tensor(
                            out=YACC[:, dc, tok], in0=py[:], in1=YACC[:, dc, tok], op=ALU.add),
                            r=[py, (YACC, (bi, dc))], w=[(YACC, (bi, dc))])

            for ui, u in enumerate(units):
                stage1(u, ui)
                if ui > 0:
                    stage2(units[ui - 1], ui - 1)
                if u[5] == 0:
                    idx = efs.index((u[1], u[2], u[3], u[4]))
                    if idx + 1 < len(efs):
                        load_w(*efs[idx + 1])
            stage2(units[-1], len(units) - 1)
            for bi in range(nbs):
                b = sb * nbs + bi
                tok = slice(bi * TB, (bi + 1) * TB)
                P.dma("sp", XT[:], self.xT[:, b * TB:(b + 1) * TB].rearrange("(k p) n -> p k n", p=128),
                      r=[(self.xT, b)], w=[XT])
                for k in range(NK):
                    P.op("dve", lambda e, k=k, tok=tok: e.scalar_tensor_tensor(
                        out=XT[:, k, :], in0=YACC[:, k, tok], scalar=mod[:, 40 + k:40 + k + 1], in1=XT[:, k, :],
                        op0=ALU.mult, op1=ALU.add), r=[(YACC, (bi, k)), mod, XT], w=[XT])
                if not last or self.dbg:
                    P.dma("sp", self.xT[:, b * TB:(b + 1) * TB].rearrange("(k p) n -> p k n", p=128), XT[:],
                          r=[XT], w=[(self.xT, b)])
                if last:
                    self.final_block(XT, SQ, rstd, tmp, b)
        P.pop_scope()
        self.dbg_dump("dbg_xB%d" % l)

    def final_block(self, XT, SQ, rstd, tmp, b):
        P = self.P
        ps = self.PSB[7]
        P.op("act", lambda e: e.activation(out=SQ[:], in_=XT[:], func=AF.Square), r=[XT], w=[SQ])
        for k in range(NK):
            P.op("pe", lambda e, k=k: e.matmul(ps[:], lhsT=self.ones_bf[:], rhs=SQ[:, k, :],
                                               start=(k == 0), stop=(k == NK - 1)), r=[self.ones_bf, SQ], w=[ps])
        P.op("act", lambda e: e.activation(out=rstd[:], in_=ps[:], func=AF.Ln, bias=self.epsc[:, 0:1], scale=1.0 / D),
             r=[ps, self.epsc], w=[rstd])
        P.op("act", lambda e: e.activation(out=rstd[:], in_=rstd[:], func=AF.Exp, scale=-0.5), r=[rstd], w=[rstd])
        for k in range(NK):
            P.op("dve", lambda e, k=k: e.scalar_tensor_tensor(
                out=XT[:, k, :], in0=XT[:, k, :], scalar=self.PK("fnw", col=k), in1=rstd[:],
                op0=ALU.mult, op1=ALU.mult), r=[XT, self.pk, rstd], w=[XT])
        for tt in range(TB // 128):
            ot = tmp[tt % 2]
            for half in range(2):
                pb = self.PSB[(tt * 2 + half) % 4]
                for q in range(4):
                    k = half * 4 + q
                    P.op("pe", lambda e, pb=pb, q=q, k=k, tt=tt: e.transpose(
                        out=pb[:, q * 128:(q + 1) * 128], in_=XT[:, k, tt * 128:(tt + 1) * 128],
                        identity=self.C("ident")), r=[XT, self.cst], w=[pb])
                o = tmp[half]
                if half == 0:
                    P.op("act", lambda e, pb=pb, o=o: e.activation(out=o[:], in_=pb[:], func=AF.Copy), r=[pb], w=[o])
                else:
                    P.op("dve", lambda e, pb=pb, o=o: e.tensor_copy(out=o[:], in_=pb[:]), r=[pb], w=[o])
                r0 = b * TB + tt * 128
                ev = P.dma("sp", self.out[r0:r0 + 128, half * 512:(half + 1) * 512], o[:], r=[o], w=[(self.out, (r0, half))])
                self.out_events.append(ev)


class _View:
    def __init__(self, buf, bi):
        self.buf = buf
        self.bi = bi
        self.id = buf.id

    def __getitem__(self, idx):
        p, k, n = idx
        assert n == slice(None, None, None)
        return self.buf[p, k, self.bi * TB:(self.bi + 1) * TB]


_PARAM_CACHE = {}


def _dummy_inputs():
    z = lambda *s: np.zeros(s, np.float32)
    return {"ada_b": z(2, 6144), "norm1_w": z(2, 1024), "norm2_w": z(2, 1024), "gla_b_lr2": z(2, 192),
            "gla_norm_w": z(2, 64), "lru_conv_w": z(2, 4, 256), "lru_conv_b": z(2, 256), "lru_b_a": z(2, 256),
            "lru_b_x": z(2, 256), "lru_lambda": z(2, 256), "dn_conv_w": z(2, 4, 1152), "dn_a_log": z(2, 6),
            "dn_dt_bias": z(2, 6), "dn_norm_w": z(2, 64), "final_norm_w": z(1024)}


def build_params_ncols():
    if "c" not in _PARAM_CACHE:
        _PARAM_CACHE["c"] = build_params(_dummy_inputs())
    return _PARAM_CACHE["c"].n


def build_params_offsets():
    build_params_ncols()
    return _PARAM_CACHE["c"].off


def make_in_maps(inputs, S, ncores):
    consts = build_consts().build()
    pk = build_params(inputs).build()
    mats = build_mats(inputs)
    f = lambda a: np.ascontiguousarray(np.asarray(a, np.float32))
    shared = {
        "w_in": f(inputs["w_in"]), "w_out": f(inputs["w_out"]), "ada_w": f(inputs["ada_w"]),
        "ffn_w_gate": f(inputs["ffn_w_gate"]), "ffn_w_up": f(inputs["ffn_w_up"]), "ffn_w_down": f(inputs["ffn_w_down"]),
        "router_w": f(inputs["router_w"]), "moe_w_gate": f(inputs["moe_w_gate"]), "moe_w_up": f(inputs["moe_w_up"]),
        "moe_w_down": f(inputs["moe_w_down"]), "cst": consts, "cstb": build_consts_b().build(), "pk": pk, "wlr2": mats["wlr2"], "lrubd": mats["lrubd"],
    }
    maps = []
    x = np.asarray(inputs["x"], np.float32)
    c = np.asarray(inputs["c"], np.float32)
    for i in range(ncores):
        m = dict(shared)
        m["x"] = np.ascontiguousarray(x[i, :S])
        m["cfm"] = _fm(c[i], 8)
        maps.append(m)
    return maps


_BUILD = {}


def kernel(**inputs):
    S = inputs["x"].shape[1]
    B = inputs["x"].shape[0]
    if "b" not in _BUILD:
        _BUILD["b"] = Builder(S)
    bld = _BUILD["b"]
    maps = make_in_maps(inputs, S, B)
    res = run_bass_kernel_spmd(bld.nc, maps, core_ids=list(range(B)))
    return np.stack([np.asarray(r["out"]) for r in res.results], 0).astype(np.float32)
```

```python
import os
import numpy as np
import concourse.bass as bass
import concourse.mybir as mybir
from concourse.bass_utils import run_bass_kernel_spmd
from contextlib import ExitStack

F32 = mybir.dt.float32
BF16 = mybir.dt.bfloat16
AF = mybir.ActivationFunctionType
ALU = mybir.AluOpType
AX = mybir.AxisListType

D = 1024
NK = 8
D_IN = 3228
D_FF = 2816
NE = 8
D_FFE = 3584
EPS = 1e-6
O_GQ, O_GK, O_GV, O_GZ, O_GLR, O_LX, O_LG, O_DQ, O_DK, O_DV, O_DZ, O_DB, O_DA = (
    0, 192, 384, 768, 1152, 1168, 1424, 1680, 2064, 2448, 2832, 3216, 3222)
TB = 512
TBA = 256
CH = 64


class Buf:
    _n = 0

    def __init__(self, h, name, init_evs=()):
        self.h = h
        self.name = name
        Buf._n += 1
        self.id = Buf._n
        self.init_evs = list(init_evs)

    def __getitem__(self, idx):
        return self.h[idx]


class BufV(Buf):
    def __init__(self, parent, width):
        self.h = parent.h
        self.name = parent.name
        self.id = parent.id
        self.width = width
        self.init_evs = parent.init_evs

    def __getitem__(self, idx):
        full = slice(None, None, None)
        if idx == full:
            return self.h[:, 0:self.width]
        if isinstance(idx, tuple) and len(idx) == 2 and idx[1] == full:
            return self.h[idx[0], 0:self.width]
        return self.h[idx]


class _Unit:
    __slots__ = ("w", "rs")

    def __init__(self):
        self.w = None
        self.rs = []


class _Rec:
    def __init__(self):
        self.call = None

    def __getattr__(self, name):
        def f(*a, **k):
            self.call = (name, a, k)
            return self
        return f


class _Ev:
    __slots__ = ("sem", "val", "eng", "seen")

    def __init__(self, sem, val, eng, seen):
        self.sem = sem
        self.val = val
        self.eng = eng
        self.seen = seen


class Prog:
    ENGS = ("pe", "act", "dve", "pool", "sp")

    def __init__(self, nc, n_dma_sems=16, same_eng_sync=True):
        self.nc = nc
        self.es = ExitStack()
        self.scopes = []
        self.same_eng_sync = same_eng_sync
        self.sems = {}
        self.cnt = {}
        self.seen = {}
        self.streams = {e: [] for e in self.ENGS}
        for e in self.ENGS:
            self.sems[e] = self.es.enter_context(nc.semaphore("s_" + e))
            self.cnt[e] = 0
            self.seen[e] = {}
        self.dma_sems = {}
        self.dma_uses = {}
        self.dma_rr = {}
        for q in ("sp", "act", "pool"):
            self.dma_sems[q] = [self.es.enter_context(nc.semaphore("d_%s%d" % (q, i)))
                                for i in range(n_dma_sems)]
            self.dma_uses[q] = [0] * n_dma_sems
            self.dma_rr[q] = 0
        self.units = {}
        self.ninst = 0
        self.last_ev = {}
        self.freed = {}
        self.scope_bufs = []
        self.buf_units = {}

    def _stack(self):
        return self.scopes[-1] if self.scopes else self.es

    def sb(self, name, shape, dtype):
        self._uid = getattr(self, "_uid", 0) + 1
        name = "%s_u%d" % (name, self._uid)
        h = self._stack().enter_context(self.nc.sbuf_tensor(name, list(shape), dtype))
        b = Buf(h, name, init_evs=self.freed.values())
        if self.scope_bufs:
            self.scope_bufs[-1].append(b)
        return b

    def ps(self, name, shape, dtype=F32):
        h = self._stack().enter_context(self.nc.psum_tensor(name, list(shape), dtype))
        return Buf(h, name)

    def dram(self, name, shape, dtype, kind="Internal"):
        h = self.nc.dram_tensor(name, list(shape), dtype, kind=kind)
        return Buf(h, name)

    def push_scope(self):
        self.scopes.append(ExitStack())
        self.scope_bufs.append([])

    def pop_scope(self, barrier=True):
        bufs = self.scope_bufs.pop()
        if barrier:
            self.barrier()
        else:
            for b in bufs:
                for key in self.buf_units.get(b.id, ()):
                    un = self.units[key]
                    for ev in ([un.w] if un.w is not None else []) + un.rs:
                        cur = self.freed.get(ev.sem)
                        if cur is None or cur.val < ev.val:
                            self.freed[ev.sem] = ev
                for ev in b.init_evs:
                    cur = self.freed.get(ev.sem)
                    if cur is None or cur.val < ev.val:
                        self.freed[ev.sem] = ev
        self.scopes.pop().close()

    def _unit(self, u):
        buf = u if isinstance(u, Buf) else u[0]
        key = (buf.id, None) if isinstance(u, Buf) else (buf.id, u[1])
        un = self.units.get(key)
        if un is None:
            un = self.units[key] = _Unit()
            un.rs = list(buf.init_evs)
            self.buf_units.setdefault(buf.id, []).append(key)
        return un

    def _collect(self, eng, r, w):
        need = {}

        def add(ev):
            if ev is None:
                return
            if ev.eng == eng and (eng in ("pe", "sp") or not self.same_eng_sync):
                return
            cur = need.get(ev.sem)
            if cur is None or cur[0] < ev.val:
                need[ev.sem] = (ev.val, ev)

        for u in r:
            add(self._unit(u).w)
        for u in w:
            un = self._unit(u)
            add(un.w)
            for ev in un.rs:
                add(ev)
        seen = self.seen[eng]
        waits = []
        for sem, (val, ev) in need.items():
            if seen.get(sem, 0) >= val:
                continue
            waits.append((sem, val))
            seen[sem] = val
            for s2, v2 in ev.seen.items():
                if seen.get(s2, 0) < v2:
                    seen[s2] = v2
        return waits

    def _record(self, ev, r, w):
        for u in r:
            self._unit(u).rs.append(ev)
        for u in w:
            un = self._unit(u)
            un.w = ev
            un.rs = []

    def op(self, eng, fn, r=(), w=()):
        waits = self._collect(eng, r, w)
        sem = self.sems[eng]
        self.cnt[eng] += 1
        val = self.cnt[eng]
        evseen = dict(self.seen[eng])
        evseen[sem] = val
        ev = _Ev(sem, val, eng, evseen)
        rec = _Rec()
        fn(rec)
        name, a, k = rec.call

        def fn2(e, name=name, a=a, k=k):
            return getattr(e, name)(*a, **k)
        self.streams[eng].append((waits, fn2, sem, 1))
        self._record(ev, r, w)
        self.ninst += 1
        self.last_ev[eng] = ev
        return ev

    def dma(self, q, out, in_, r=(), w=(), **kw):
        waits = self._collect(q, r, w)
        pool = self.dma_sems[q]
        i = self.dma_rr[q]
        self.dma_rr[q] = (i + 1) % len(pool)
        sem = pool[i]
        m = self.dma_uses[q][i]
        seen = self.seen[q]
        if m > 0 and seen.get(sem, 0) < 16 * m:
            waits.append((sem, 16 * m))
            seen[sem] = 16 * m
        self.dma_uses[q][i] = m + 1
        val = 16 * (m + 1)
        evseen = dict(seen)
        evseen[sem] = val
        ev = _Ev(sem, val, "dma_" + q, evseen)

        def fn(e, out=out, in_=in_, kw=kw):
            return e.dma_start(out=out, in_=in_, **kw)
        self.streams[q].append((waits, fn, sem, 16))
        self._record(ev, r, w)
        self.ninst += 1
        self.last_ev[("dma", q, i)] = ev
        return ev

    def wait_event(self, eng, ev):
        seen = self.seen[eng]
        if seen.get(ev.sem, 0) >= ev.val:
            return
        seen[ev.sem] = ev.val
        for s2, v2 in ev.seen.items():
            if seen.get(s2, 0) < v2:
                seen[s2] = v2
        self.streams[eng].append(([(ev.sem, ev.val)], None, None, 0))

    def barrier(self):
        evs = list(self.last_ev.values())
        for e in self.ENGS:
            for ev in evs:
                if ev.eng == e and e in ("pe", "sp"):
                    continue
                self.wait_event(e, ev)
        self.freed = {}

    def emit(self):
        nc = self.nc
        streams = self.streams
        with nc.Block() as block:
            def run(e, lst):
                for waits, fn, sem, inc in lst:
                    for (s, v) in waits:
                        e.wait_ge(s, v)
                    if fn is not None:
                        fn(e).then_inc(sem, inc)

            @block.tensor
            def _(e):
                run(e, streams["pe"])

            @block.scalar
            def _(e):
                run(e, streams["act"])

            @block.vector
            def _(e):
                run(e, streams["dve"])

            @block.gpsimd
            def _(e):
                run(e, streams["pool"])

            @block.sync
            def _(e):
                run(e, streams["sp"])


def _fm(v, nchunk):
    return np.ascontiguousarray(np.asarray(v, np.float32).reshape(nchunk, 128).T)


class Cols:
    def __init__(self):
        self.n = 0
        self.off = {}
        self.parts = []

    def add(self, name, arr):
        arr = np.asarray(arr, np.float32)
        if arr.ndim == 1:
            arr = arr[:, None]
        if arr.shape[0] < 128:
            arr = np.concatenate([arr, np.zeros((128 - arr.shape[0], arr.shape[1]), np.float32)], 0)
        self.off[name] = (self.n, arr.shape[1])
        self.n += arr.shape[1]
        self.parts.append(arr)

    def build(self):
        return np.ascontiguousarray(np.concatenate(self.parts, axis=1))


def build_consts():
    c = Cols()
    p = np.arange(128)
    c.add("ident", np.eye(128, dtype=np.float32))
    c.add("ones", np.ones((128, 128), np.float32))
    c.add("blk64", (p[:, None] // 64 == p[None, :] // 64).astype(np.float32))
    t = np.arange(TB)
    c.add("rst", np.tile((t % CH != 0).astype(np.float32)[None, :], (128, 1)))
    s = p % 64
    cc = np.arange(64)
    m = (cc[None, :] >= s[:, None]).astype(np.float32)
    c.add("ut6", np.tile(m, (1, 6)))
    ms = (cc[None, :] > s[:, None]).astype(np.float32)
    c.add("sut6", np.tile(ms, (1, 6)))
    c.add("su2", ((p[:, None] // 64 == p[None, :] // 64) & (p[:, None] > p[None, :])).astype(np.float32))
    for j in range(3):
        e = np.zeros((128, 128), np.float32)
        for h in range(6):
            e[h, :] = (2 * j + p // 64 == h)
        c.add("e6_%d" % j, e)
    nm = np.zeros((128, 64), np.float32)
    nm[:64, :] = np.where(cc[None, :] >= cc[:, None], 0.0, -30000.0)
    c.add("negm", nm)
    i6 = np.zeros((128, 384), np.float32)
    i6[:64, :] = np.tile(np.eye(64, dtype=np.float32), (1, 6))
    c.add("id6", i6)
    sh = np.zeros((128, 64), np.float32)
    sh[64 + np.arange(64), np.arange(64)] = 1.0
    c.add("selhi", sh)
    for h in range(6):
        a = np.zeros((128, 64), np.float32)
        a[h, :] = 1.0
        c.add("selh_%d" % h, a)
    oh = np.zeros((128, 6), np.float32)
    for h in range(6):
        oh[h, h] = 1.0
    c.add("oh6", oh)
    c.add("noh6", -oh)
    return c


def build_consts_b():
    c = Cols()
    for e_ in range(8):
        s8 = np.zeros((8, 128), np.float32)
        s8[e_, :] = 1.0
        c.add("sel8_%d" % e_, s8)
    return c


def build_params(inp):
    c = Cols()
    for l in range(2):
        c.add("ada_b%d" % l, _fm(inp["ada_b"][l], 48))
        c.add("n1w%d" % l, _fm(inp["norm1_w"][l], 8))
        c.add("n2w%d" % l, _fm(inp["norm2_w"][l], 8))
        b = np.asarray(inp["gla_b_lr2"][l], np.float32)
        c.add("glab%d" % l, np.stack([b[0:96], b[96:192]], 1))
        c.add("glanw%d" % l, np.tile(np.asarray(inp["gla_norm_w"][l], np.float32), 2))
        cw = np.asarray(inp["lru_conv_w"][l], np.float32)
        c.add("lrucw%d" % l, np.stack([cw[j, ch * 128:(ch + 1) * 128] for ch in range(2) for j in range(4)], 1))
        c.add("lrucb%d" % l, _fm(inp["lru_conv_b"][l], 2))
        c.add("lruba%d" % l, _fm(inp["lru_b_a"][l], 2))
        c.add("lrubx%d" % l, _fm(inp["lru_b_x"][l], 2))
        c.add("lrulam%d" % l, _fm(inp["lru_lambda"][l], 2))
        dw = np.asarray(inp["dn_conv_w"][l], np.float32)
        c.add("dncw%d" % l, np.stack([dw[j, ch * 128:(ch + 1) * 128] for ch in range(9) for j in range(4)], 1))
        c.add("dnalog%d" % l, np.asarray(inp["dn_a_log"][l], np.float32))
        c.add("dndtb%d" % l, np.asarray(inp["dn_dt_bias"][l], np.float32))
        c.add("dnnw%d" % l, np.tile(np.asarray(inp["dn_norm_w"][l], np.float32), 2))
    c.add("fnw", _fm(inp["final_norm_w"], 8))
    return c


def build_mats(inp):
    out = {}
    lr = np.zeros((2, 32, 192), np.float32)
    for l in range(2):
        lr[l, :16] = inp["gla_w_lr2"][l]
        lr[l, 16] = inp["gla_b_lr2"][l]
    out["wlr2"] = lr
    bd = np.zeros((2, 2, 2, 128, 128), np.float32)
    for l in range(2):
        for t, nm in enumerate(("lru_w_a", "lru_w_x")):
            w = np.asarray(inp[nm][l], np.float32)
            for ch in range(2):
                for q in range(2):
                    n = ch * 2 + q
                    bd[l, t, ch, q * 64:(q + 1) * 64, q * 64:(q + 1) * 64] = w[n]
    out["lrubd"] = bd
    return out


class Builder:
    def __init__(self, S, dbg=False, use=("gla", "lru", "dn"), nlayers=2, sbt=1024):
        self.S = S
        self.dbg = dbg
        self.use = use
        self.nlayers = nlayers
        self.NB = S // TB
        self.SBT = min(sbt, S)
        self.cc = build_consts()
        nc = bass.Bass("TRN2", target_bir_lowering=False)
        self.nc = nc
        P = Prog(nc, same_eng_sync=(os.environ.get('SES', '1') == '1'))
        self.P = P
        self.dbg_outs = []
        self._declare_io()
        self._globals()
        self.pass0()
        for l in range(nlayers):
            self.adaln(l)
            self.passA(l)
            self.passB(l, last=(l == nlayers - 1))
        P.barrier()
        for ev in self.out_events:
            P.wait_event("sp", ev)
        P.emit()

    def _declare_io(self):
        P, S = self.P, self.S
        di = lambda n, s: P.dram(n, s, F32, kind="ExternalInput")
        self.x = di("x", [S, D])
        self.cfm = di("cfm", [128, NK])
        self.w_in = di("w_in", [2, D, D_IN])
        self.w_out = di("w_out", [2, D, D])
        self.ada_w = di("ada_w", [2, D, 6 * D])
        self.ffn_wg = di("ffn_w_gate", [1, D, D_FF])
        self.ffn_wu = di("ffn_w_up", [1, D, D_FF])
        self.ffn_wd = di("ffn_w_down", [1, D_FF, D])
        self.router_w = di("router_w", [1, D, NE])
        self.moe_wg = di("moe_w_gate", [1, NE, D, D_FFE])
        self.moe_wu = di("moe_w_up", [1, NE, D, D_FFE])
        self.moe_wd = di("moe_w_down", [1, NE, D_FFE, D])
        self.cst_d = di("cst", [128, self.cc.n])
        self.ccb = build_consts_b()
        self.cstb_d = di("cstb", [128, self.ccb.n])
        self.pk_n = build_params_ncols()
        self.pk_d = di("pk", [128, self.pk_n])
        self.wlr2_d = di("wlr2", [2, 32, 192])
        self.lrubd_d = di("lrubd", [2, 2, 2, 128, 128])
        self.out = P.dram("out", [S, D], F32, kind="ExternalOutput")
        self.xT = P.dram("xT_scr", [D, S], F32)
        self.out_events = []

    def dbg_dump(self, name):
        if not self.dbg:
            return
        P = self.P
        o = P.dram(name, [D, self.S], F32, kind="ExternalOutput")
        P.push_scope()
        t = P.sb("dbgt", [128, NK, TB], F32)
        for b in range(self.NB):
            P.dma("sp", t[:], self.xT[:, b * TB:(b + 1) * TB].rearrange("(k p) n -> p k n", p=128),
                  r=[(self.xT, b)], w=[t])
            ev = P.dma("sp", o[:, b * TB:(b + 1) * TB].rearrange("(k p) n -> p k n", p=128), t[:],
                       r=[t], w=[o])
            self.out_events.append(ev)
        P.pop_scope()
        self.dbg_outs.append(name)

    def C(self, name, rows=128):
        o, n = self.cc.off[name]
        return self.cst[0:rows, o:o + n]

    def PK(self, name, rows=128, col=None, ncol=None):
        o, n = self.pko[name]
        if col is not None:
            o = o + col
            n = 1 if ncol is None else ncol
        return self.pk[0:rows, o:o + n]

    def _globals(self):
        P = self.P
        self.cst = P.sb("cst_sb", [128, self.cc.n], F32)
        P.dma("sp", self.cst[:], self.cst_d[:], w=[self.cst])
        self.pk = P.sb("pk_sb", [128, self.pk_n], F32)
        P.dma("sp", self.pk[:], self.pk_d[:], w=[self.pk])
        self.pko = build_params_offsets()
        self.ones_bf = P.sb("ones_bf", [128, 128], BF16)
        P.op("dve", lambda e: e.tensor_copy(out=self.ones_bf[:], in_=self.C("ones")), r=[self.cst], w=[self.ones_bf])
        self.blk_bf = P.sb("blk_bf", [128, 128], BF16)
        P.op("dve", lambda e: e.tensor_copy(out=self.blk_bf[:], in_=self.C("blk64")), r=[self.cst], w=[self.blk_bf])
        self.PSB = [P.ps("psb%d" % i, [128, 512], F32) for i in range(8)]
        self.epsc = P.sb("epsc", [128, 1], F32)
        P.op("dve", lambda e: e.memset(self.epsc[:], EPS), w=[self.epsc])
        self.csil = P.sb("csil", [128, NK], F32)
        ct = P.sb("ctmp", [128, NK], F32)
        P.dma("sp", ct[:], self.cfm[:], w=[ct])
        P.op("act", lambda e: e.activation(out=self.csil[:], in_=ct[:], func=AF.Silu), r=[ct], w=[self.csil])
        self.mod = [P.sb("mod%d" % l, [128, 48], F32) for l in range(2)]
        self.gv1 = [P.sb("gv1_%d" % l, [128, NK], F32) for l in range(2)]
        self.gv2 = [P.sb("gv2_%d" % l, [128, NK], F32) for l in range(2)]

    def pass0(self):
        P = self.P
        P.push_scope()
        xin = [P.sb("p0_in%d" % i, [128, D], F32) for i in range(2)]
        xt = [P.sb("p0_xt%d" % i, [128, NK, TB], F32) for i in range(2)]
        n = 0
        for b in range(self.NB):
            xo = xt[b % 2]
            for tt in range(TB // 128):
                xi = xin[n % 2]
                r0 = b * TB + tt * 128
                P.dma("sp", xi[:], self.x[r0:r0 + 128, :], r=[self.x], w=[xi])
                for half in range(2):
                    pb = self.PSB[(n * 2 + half) % 4]
                    for q in range(4):
                        k = half * 4 + q
                        P.op("pe", lambda e, pb=pb, q=q, xi=xi, k=k: e.transpose(
                            out=pb[:, q * 128:(q + 1) * 128], in_=xi[:, k * 128:(k + 1) * 128],
                            identity=self.C("ident")), r=[xi, self.cst], w=[pb])
                    eng = "act" if half == 0 else "dve"
                    if eng == "act":
                        P.op("act", lambda e, pb=pb, xo=xo, half=half, tt=tt: e.activation(
                            out=xo[:, half * 4:half * 4 + 4, tt * 128:(tt + 1) * 128],
                            in_=pb[:].rearrange("p (q n) -> p q n", q=4), func=AF.Copy), r=[pb], w=[xo])
                    else:
                        P.op("dve", lambda e, pb=pb, xo=xo, half=half, tt=tt: e.tensor_copy(
                            out=xo[:, half * 4:half * 4 + 4, tt * 128:(tt + 1) * 128],
                            in_=pb[:].rearrange("p (q n) -> p q n", q=4)), r=[pb], w=[xo])
                n += 1
            P.dma("sp", self.xT[:, b * TB:(b + 1) * TB].rearrange("(k p) n -> p k n", p=128), xo[:],
                  r=[xo], w=[(self.xT, b)])
        P.pop_scope()
        self.dbg_dump("dbg_x0")

    def adaln(self, l):
        P = self.P
        P.push_scope()
        wt = [P.sb("ada_wt%d" % i, [128, NK, 1024], F32) for i in range(2)]
        pb = self.PSB[7]
        for g in range(6):
            t = wt[g % 2]
            P.dma("sp" if g % 2 == 0 else "act", t[:],
                  self.ada_w[l, :, g * 1024:(g + 1) * 1024].rearrange("(k p) n -> p k n", p=128),
                  r=[self.ada_w], w=[t])
            for oc in range(8):
                col = g * 8 + oc
                for k in range(NK):
                    P.op("pe", lambda e, t=t, k=k, oc=oc, col=col: e.matmul(
                        pb[:, col:col + 1], lhsT=t[:, k, oc * 128:(oc + 1) * 128], rhs=self.csil[:, k:k + 1],
                        start=(k == 0), stop=(k == NK - 1)), r=[t, self.csil], w=[pb])
        mod = self.mod[l]
        P.op("dve", lambda e: e.tensor_tensor(out=mod[:], in0=pb[:, 0:48], in1=self.PK("ada_b%d" % l), op=ALU.add),
             r=[pb, self.pk], w=[mod])
        for (gv, nw, c0) in ((self.gv1[l], "n1w%d" % l, 8), (self.gv2[l], "n2w%d" % l, 32)):
            P.op("dve", lambda e, gv=gv, nw=nw, c0=c0: e.scalar_tensor_tensor(
                out=gv[:], in0=mod[:, c0:c0 + 8], scalar=1.0, in1=self.PK(nw), op0=ALU.add, op1=ALU.mult),
                r=[mod, self.pk], w=[gv])
        P.pop_scope()

    def load_norm(self, XT, b, gv, shc, Hap, Hdep, SQ, tmp, rstd, hf_cb=None, ps=None, tb=TB):
        P = self.P
        ps = ps if ps is not None else self.PSB[7]
        P.dma("sp", XT[:], self.xT[:, b * tb:(b + 1) * tb].rearrange("(k p) n -> p k n", p=128),
              r=[(self.xT, b)], w=[XT])
        P.op("act", lambda e: e.activation(out=SQ[:], in_=XT[:], func=AF.Square), r=[XT], w=[SQ])
        for k in range(NK):
            P.op("pe", lambda e, k=k: e.matmul(ps[:, 0:tb], lhsT=self.ones_bf[:], rhs=SQ[:, k, :],
                                               start=(k == 0), stop=(k == NK - 1)),
                 r=[self.ones_bf, SQ], w=[ps])
        P.op("act", lambda e: e.activation(out=rstd[:], in_=ps[:, 0:tb], func=AF.Ln, bias=self.epsc[:, 0:1], scale=1.0 / D),
             r=[ps, self.epsc], w=[rstd])
        P.op("act", lambda e: e.activation(out=rstd[:], in_=rstd[:], func=AF.Exp, scale=-0.5), r=[rstd], w=[rstd])
        for k in range(NK):
            t = tmp[k % len(tmp)]
            P.op("dve", lambda e, k=k, t=t: e.scalar_tensor_tensor(
                out=t[:], in0=XT[:, k, :], scalar=gv[:, k:k + 1], in1=rstd[:], op0=ALU.mult, op1=ALU.mult),
                r=[XT, gv, rstd], w=[t])
            if hf_cb is None:
                P.op("act", lambda e, k=k, t=t: e.activation(
                    out=Hap(k), in_=t[:], func=AF.Identity, bias=shc(k), scale=1.0),
                    r=[t, self.mod[0], self.mod[1]], w=[Hdep])
            else:
                P.op("act", lambda e, k=k, t=t: e.activation(
                    out=t[:], in_=t[:], func=AF.Identity, bias=shc(k), scale=1.0),
                    r=[t, self.mod[0], self.mod[1]], w=[t])
                P.op("dve", lambda e, k=k, t=t: e.tensor_copy(out=Hap(k), in_=t[:]), r=[t], w=[Hdep])
                hf_cb(k, t)

    def passA(self, l):
        P = self.P
        P.push_scope()
        self.A_l = l
        self.PSA = [BufV(b_, TBA) for b_ in self.PSB]
        WIN = P.sb("WIN", [128, NK, D_IN], BF16)
        self.WIN = WIN
        WOUT = P.sb("WOUT", [128, NK, D], BF16)
        for k in range(NK):
            P.dma("pool", WOUT[:, k, :], self.w_out[l, k * 128:(k + 1) * 128, :], r=[self.w_out], w=[(WOUT, k)])
        for k in range(NK):
            P.dma("pool", WIN[:, k, :], self.w_in[l, k * 128:(k + 1) * 128, :], r=[self.w_in], w=[(WIN, k)],
                  max_dma_last_dim=4096)
        H = P.sb("A_H", [128, NK, TBA], BF16)
        MT = P.sb("A_MT", [128, NK, TBA], BF16)
        self.H, self.MT = H, MT
        self.tpool = [P.sb("A_t%d" % i, [128, TBA], F32) for i in range(self.NTP)]
        self.tfree = list(self.tpool)
        self.mixer_setup(l)
        mod = self.mod[l]
        for b in range(self.S // TBA):
            P.push_scope()
            XT = P.sb("A_XT", [128, NK, TBA], F32)
            SQ = P.sb("A_SQ", [128, NK, TBA], BF16)
            rstd = P.sb("A_rstd", [128, TBA], F32)
            tmp = [P.sb("A_tmp%d" % i, [128, TBA], F32) for i in range(2)]
            self.load_norm(XT, b, self.gv1[l], lambda k: mod[:, 0 + k:0 + k + 1], lambda k: H[:, k, :], H, SQ, tmp, rstd, tb=TBA)
            P.pop_scope(barrier=False)
            if "lru" in self.use:
                self.lru_block(l, b)
            else:
                self.zero_mt((3, 4))
            if "gla" in self.use:
                self.gla_block(l, b)
            else:
                self.zero_mt((0, 1, 2))
            if "dn" in self.use:
                self.dn_block(l, b)
            else:
                self.zero_mt((5, 6, 7))
            P.push_scope()
            XT = P.sb("A_XT2", [128, NK, TBA], F32)
            P.dma("sp", XT[:], self.xT[:, b * TBA:(b + 1) * TBA].rearrange("(k p) n -> p k n", p=128),
                  r=[(self.xT, b)], w=[XT])
            for dc in range(NK):
                pb = self.PSA[dc % 2]
                for j in range(NK):
                    P.op("pe", lambda e, pb=pb, j=j, dc=dc: e.matmul(
                        pb[:], lhsT=WOUT[:, j, dc * 128:(dc + 1) * 128], rhs=MT[:, j, :],
                        start=(j == 0), stop=(j == NK - 1)),
                        r=[(WOUT, j), (MT, j)], w=[pb])
                P.op("dve", lambda e, pb=pb, dc=dc: e.scalar_tensor_tensor(
                    out=XT[:, dc, :], in0=pb[:], scalar=mod[:, 16 + dc:16 + dc + 1], in1=XT[:, dc, :],
                    op0=ALU.mult, op1=ALU.add), r=[pb, mod, XT], w=[XT])
            P.dma("sp", self.xT[:, b * TBA:(b + 1) * TBA].rearrange("(k p) n -> p k n", p=128), XT[:],
                  r=[XT], w=[(self.xT, b)])
            P.pop_scope(barrier=False)
        P.pop_scope()
        self.dbg_dump("dbg_xA%d" % l)

    def zero_mt(self, js):
        P = self.P
        for j in js:
            P.op("pool", lambda e, j=j: e.memset(self.MT[:, j, :], 0.0), w=[(self.MT, j)])

    NTP = 6

    def tget(self):
        return self.tfree.pop()

    def tput(self, *ts):
        for t in ts:
            self.tfree.append(t)

    def proj_fm(self, pb, col0, ncols, prow0=0):
        P = self.P
        for k in range(NK):
            P.op("pe", lambda e, k=k: e.matmul(
                pb[prow0:prow0 + ncols, :], lhsT=self.WIN[:, k, col0:col0 + ncols], rhs=self.H[:, k, :],
                start=(k == 0), stop=(k == NK - 1)), r=[(self.WIN, k), self.H], w=[pb])

    def mixer_setup(self, l):
        P = self.P
        self.lru_bd = P.sb("lru_bd", [128, 2, 2, 128], F32)
        for t in range(2):
            for ch in range(2):
                P.dma("sp", self.lru_bd[:, t, ch, :], self.lrubd_d[l, t, ch], r=[self.lrubd_d], w=[self.lru_bd])
        self.lru_c1 = P.sb("lru_c1", [128, 2], F32)
        self.lru_c2 = P.sb("lru_c2", [128, 2], F32)
        ta = P.sb("lru_ta", [128, 2], F32)
        tb = P.sb("lru_tb", [128, 2], F32)
        lam = self.PK("lrulam%d" % l)
        P.op("act", lambda e: e.activation(out=ta[:], in_=lam, func=AF.Abs), r=[self.pk], w=[ta])
        P.op("act", lambda e: e.activation(out=ta[:], in_=ta[:], func=AF.Exp, scale=-1.0), r=[ta], w=[ta])
        P.op("act", lambda e: e.activation(out=ta[:], in_=ta[:], func=AF.Ln, bias=1.0, scale=1.0), r=[ta], w=[ta])
        P.op("dve", lambda e: e.tensor_scalar(out=tb[:], in0=lam, scalar1=-1.0, scalar2=0.0,
                                              op0=ALU.mult, op1=ALU.max), r=[self.pk], w=[tb])
        P.op("dve", lambda e: e.tensor_tensor(out=ta[:], in0=ta[:], in1=tb[:], op=ALU.add), r=[ta, tb], w=[ta])
        P.op("dve", lambda e: e.tensor_scalar(out=self.lru_c1[:], in0=ta[:], scalar1=-8.0, scalar2=None,
                                              op0=ALU.mult), r=[ta], w=[self.lru_c1])
        P.op("dve", lambda e: e.tensor_scalar(out=self.lru_c2[:], in0=ta[:], scalar1=-16.0, scalar2=None,
                                              op0=ALU.mult), r=[ta], w=[self.lru_c2])
        self.lru_x = [P.sb("lru_x%d" % ch, [128, 3 + TBA], F32) for ch in range(2)]
        self.lru_h = P.sb("lru_h", [128, 2], F32)
        for ch in range(2):
            P.op("pool", lambda e, ch=ch: e.memset(self.lru_x[ch][:], 0.0), w=[self.lru_x[ch]])
        P.op("pool", lambda e: e.memset(self.lru_h[:], 0.0), w=[self.lru_h])
        if "gla" in self.use:
            self.gla_setup(l)
        if "dn" in self.use:
            self.dn_setup(l)

    def conv4(self, out, xin, wname, wcol0, bias_ap=None, rdeps=(), wdeps=()):
        P = self.P
        w = lambda j: self.PK(wname, col=wcol0 + j)
        if bias_ap is not None:
            P.op("dve", lambda e: e.tensor_scalar(out=out, in0=xin[:, 0:TBA], scalar1=w(0), scalar2=bias_ap,
                                                  op0=ALU.mult, op1=ALU.add), r=list(rdeps) + [self.pk], w=list(wdeps))
        else:
            P.op("dve", lambda e: e.tensor_scalar(out=out, in0=xin[:, 0:TBA], scalar1=w(0), scalar2=None,
                                                  op0=ALU.mult), r=list(rdeps) + [self.pk], w=list(wdeps))
        for j in range(1, 4):
            P.op("dve", lambda e, j=j: e.scalar_tensor_tensor(out=out, in0=xin[:, j:j + TBA], scalar=w(j), in1=out,
                                                              op0=ALU.mult, op1=ALU.add),
                 r=list(rdeps) + [self.pk] + list(wdeps), w=list(wdeps))

    def lru_block(self, l, b):
        P = self.P
        PS = self.PSA
        P.push_scope()
        CH2 = range(2)
        T = {nm: [P.sb("lru_%s%d" % (nm, ch), [128, TBA], F32) for ch in CH2] for nm in ("xr", "rg", "ig", "a", "a2", "g", "t2", "hs")}
        X = self.lru_x
        for ch in CH2:
            if b > 0:
                P.op("dve", lambda e, ch=ch: e.tensor_copy(out=X[ch][:, 0:3], in_=X[ch][:, TBA:TBA + 3]), r=[X[ch]], w=[X[ch]])
            self.proj_fm(PS[ch], O_LX + ch * 128, 128)
            P.op("act", lambda e, ch=ch: e.activation(out=X[ch][:, 3:3 + TBA], in_=PS[ch][:], func=AF.Copy), r=[PS[ch]], w=[X[ch]])
            self.proj_fm(PS[6 + ch], O_LG + ch * 128, 128)
            P.op("act", lambda e, ch=ch: e.activation(out=T["g"][ch][:], in_=PS[6 + ch][:], func=AF.Copy), r=[PS[6 + ch]], w=[T["g"][ch]])
        for ch in CH2:
            xr = T["xr"][ch]
            self.conv4(xr[:], X[ch], "lrucw%d" % l, ch * 4, bias_ap=self.PK("lrucb%d" % l, col=ch), rdeps=[X[ch]], wdeps=[xr])
        for ch in CH2:
            g, t2 = T["g"][ch], T["t2"][ch]
            P.op("dve", lambda e, g=g, t2=t2: e.tensor_tensor(out=t2[:], in0=g[:], in1=g[:], op=ALU.mult), r=[g], w=[t2])
        for ch in CH2:
            t2 = T["t2"][ch]
            P.op("dve", lambda e, t2=t2: e.tensor_scalar(out=t2[:], in0=t2[:], scalar1=0.044715, scalar2=1.0,
                                                         op0=ALU.mult, op1=ALU.add), r=[t2], w=[t2])
        for ch in CH2:
            g, t2 = T["g"][ch], T["t2"][ch]
            P.op("dve", lambda e, g=g, t2=t2: e.tensor_tensor(out=t2[:], in0=t2[:], in1=g[:], op=ALU.mult), r=[t2, g], w=[t2])
        for ch in CH2:
            xr = T["xr"][ch]
            P.op("pe", lambda e, ch=ch, xr=xr: e.matmul(PS[2 + ch][:], lhsT=self.lru_bd[:, 0, ch, :], rhs=xr[:],
                                                        start=True, stop=True), r=[self.lru_bd, xr], w=[PS[2 + ch]])
            P.op("pe", lambda e, ch=ch, xr=xr: e.matmul(PS[4 + ch][:], lhsT=self.lru_bd[:, 1, ch, :], rhs=xr[:],
                                                        start=True, stop=True), r=[self.lru_bd, xr], w=[PS[4 + ch]])
        for ch in CH2:
            t2 = T["t2"][ch]
            P.op("act", lambda e, t2=t2: e.activation(out=t2[:], in_=t2[:], func=AF.Sigmoid, scale=1.5957691216), r=[t2], w=[t2])
        for ch in CH2:
            rg, ig = T["rg"][ch], T["ig"][ch]
            P.op("act", lambda e, rg=rg, ch=ch: e.activation(
                out=rg[:], in_=PS[2 + ch][:], func=AF.Sigmoid, bias=self.PK("lruba%d" % l, col=ch), scale=1.0),
                r=[PS[2 + ch], self.pk], w=[rg])
            P.op("act", lambda e, ig=ig, ch=ch: e.activation(
                out=ig[:], in_=PS[4 + ch][:], func=AF.Sigmoid, bias=self.PK("lrubx%d" % l, col=ch), scale=1.0),
                r=[PS[4 + ch], self.pk], w=[ig])
        for ch in CH2:
            g, t2 = T["g"][ch], T["t2"][ch]
            P.op("dve", lambda e, g=g, t2=t2: e.tensor_tensor(out=t2[:], in0=t2[:], in1=g[:], op=ALU.mult), r=[t2, g], w=[t2])
        for ch in CH2:
            rg, a, a2 = T["rg"][ch], T["a"][ch], T["a2"][ch]
            P.op("act", lambda e, a=a, rg=rg, ch=ch: e.activation(
                out=a[:], in_=rg[:], func=AF.Exp, scale=self.lru_c1[:, ch:ch + 1]), r=[rg, self.lru_c1], w=[a])
            P.op("act", lambda e, a2=a2, rg=rg, ch=ch: e.activation(
                out=a2[:], in_=rg[:], func=AF.Exp, scale=self.lru_c2[:, ch:ch + 1]), r=[rg, self.lru_c2], w=[a2])
        for ch in CH2:
            a2 = T["a2"][ch]
            P.op("dve", lambda e, a2=a2: e.tensor_scalar(out=a2[:], in0=a2[:], scalar1=-1.0, scalar2=1.0,
                                                         op0=ALU.mult, op1=ALU.add), r=[a2], w=[a2])
        for ch in CH2:
            a2 = T["a2"][ch]
            P.op("dve", lambda e, a2=a2: e.tensor_scalar(out=a2[:], in0=a2[:], scalar1=1e-30, scalar2=None,
                                                         op0=ALU.max), r=[a2], w=[a2])
        for ch in CH2:
            a2 = T["a2"][ch]
            P.op("act", lambda e, a2=a2: e.activation(out=a2[:], in_=a2[:], func=AF.Ln), r=[a2], w=[a2])
        for ch in CH2:
            a2 = T["a2"][ch]
            P.op("act", lambda e, a2=a2: e.activation(out=a2[:], in_=a2[:], func=AF.Exp, scale=0.5), r=[a2], w=[a2])
        for ch in CH2:
            ig, xr = T["ig"][ch], T["xr"][ch]
            P.op("dve", lambda e, ig=ig, xr=xr: e.tensor_tensor(out=ig[:], in0=ig[:], in1=xr[:], op=ALU.mult), r=[ig, xr], w=[ig])
        for ch in CH2:
            ig, a2 = T["ig"][ch], T["a2"][ch]
            P.op("dve", lambda e, ig=ig, a2=a2: e.tensor_tensor(out=ig[:], in0=ig[:], in1=a2[:], op=ALU.mult), r=[ig, a2], w=[ig])
        for ch in CH2:
            hs, a, ig = T["hs"][ch], T["a"][ch], T["ig"][ch]
            P.op("dve", lambda e, hs=hs, a=a, ig=ig, ch=ch: e.tensor_tensor_scan(
                out=hs[:], data0=a[:], data1=ig[:], initial=self.lru_h[:, ch:ch + 1], op0=ALU.mult, op1=ALU.add),
                r=[a, ig, self.lru_h], w=[hs])
        for ch in CH2:
            hs = T["hs"][ch]
            P.op("dve", lambda e, hs=hs, ch=ch: e.tensor_copy(out=self.lru_h[:, ch:ch + 1], in_=hs[:, TBA - 1:TBA]),
                 r=[hs], w=[self.lru_h])
        for ch in CH2:
            hs, t2 = T["hs"][ch], T["t2"][ch]
            P.op("dve", lambda e, t2=t2, hs=hs, ch=ch: e.tensor_tensor(out=self.MT[:, 3 + ch, :], in0=t2[:], in1=hs[:],
                                                                       op=ALU.mult), r=[t2, hs], w=[(self.MT, 3 + ch)])
        P.pop_scope(barrier=False)

    def gla_setup(self, l):
        P = self.P
        W6 = 6 * TBA
        self.WLR = P.sb("gla_wlr", [32, 192], F32)
        P.dma("sp", self.WLR[:], self.wlr2_d[l], r=[self.wlr2_d], w=[self.WLR])
        self.gS = P.sb("gla_S", [64, 384], F32)
        P.op("pool", lambda e: e.memset(self.gS[:], 0.0), w=[self.gS])

    def gla_block(self, l, b):
        P = self.P
        PS = self.PSA
        NEG = -1.0 / 16.0
        W6 = 6 * TBA
        P.push_scope()
        self.GL = P.sb("gla_gl", [32, TBA], F32)
        P.op("pool", lambda e: e.memset(self.GL[:], 1.0), w=[self.GL])
        GQ = P.sb("gla_q", [64, W6], F32)
        GK = P.sb("gla_k", [64, W6], F32)
        GEB = P.sb("gla_eb", [32, W6], F32)
        GL1 = P.sb("gla_l1", [32, W6], F32)
        GL2 = P.sb("gla_l2", [32, W6], F32)
        gS = self.gS
        for t in (GQ, GK):
            P.op("pool", lambda e, t=t: e.memset(t[:], 0.0), w=[t])
        self.gTM = [P.sb("gla_tm%d" % t, [64, 1152], F32) for t in range(TBA // CH)]
        self.proj_fm(PS[0], O_GLR, 16)
        P.op("act", lambda e: e.activation(out=self.GL[0:16, :], in_=PS[0][0:16, :], func=AF.Copy), r=[PS[0]], w=[self.GL])
        for h in range(6):
            pz = PS[1 + h % 2]
            P.op("pe", lambda e, h=h, pz=pz: e.matmul(pz[0:32, :], lhsT=self.WLR[0:17, h * 32:(h + 1) * 32],
                                                      rhs=self.GL[0:17, :], start=True, stop=True),
                 r=[self.WLR, self.GL], w=[pz])
            P.op("dve", lambda e, h=h, pz=pz: e.tensor_scalar(out=GL1[:, h * TBA:(h + 1) * TBA], in0=pz[0:32, :], scalar1=-80.0,
                                                              scalar2=None, op0=ALU.max), r=[pz], w=[GL1])
        P.op("act", lambda e: e.activation(out=GL1[:], in_=GL1[:], func=AF.Exp, scale=-1.0), r=[GL1], w=[GL1])
        P.op("act", lambda e: e.activation(out=GL1[:], in_=GL1[:], func=AF.Ln, bias=1.0, scale=1.0), r=[GL1], w=[GL1])
        for i in range(3):
            cs = slice(i * 2 * TBA, (i + 1) * 2 * TBA)
            P.op("dve", lambda e, cs=cs: e.tensor_tensor_scan(
                out=GL2[:, cs], data0=self.C("rst", rows=32)[:, 0:2 * TBA], data1=GL1[:, cs], initial=0.0,
                op0=ALU.mult, op1=ALU.add), r=[GL1, self.cst], w=[GL2])
        P.op("act", lambda e: e.activation(out=GEB[:], in_=GL2[:], func=AF.Exp, scale=NEG), r=[GL2], w=[GEB])
        P.op("act", lambda e: e.activation(out=GL1[:], in_=GL2[:], func=AF.Exp, scale=-NEG), r=[GL2], w=[GL1])
        for h in range(6):
            hs = slice(h * TBA, (h + 1) * TBA)
            pq = PS[1 + h % 2]
            self.proj_fm(pq, O_GQ + h * 32, 32)
            P.op("dve", lambda e, hs=hs, pq=pq: e.scalar_tensor_tensor(
                out=GQ[0:32, hs], in0=pq[0:32, :], scalar=0.17677669529663687, in1=GEB[:, hs], op0=ALU.mult, op1=ALU.mult),
                r=[pq, GEB], w=[GQ])
            pk_ = PS[3 + h % 2]
            self.proj_fm(pk_, O_GK + h * 32, 32)
            P.op("dve", lambda e, hs=hs, pk_=pk_: e.tensor_tensor(
                out=GK[0:32, hs], in0=pk_[0:32, :], in1=GL1[:, hs], op=ALU.mult), r=[pk_, GL1], w=[GK])
        NCH = TBA // CH
        BSETS = [(PS[0], PS[1], PS[2]), (PS[5], PS[6], PS[7])]

        def tparts(n):
            tm = self.gTM[n]
            return tm, tm[:, 0:192], tm[:, 192:384], tm[:, 384:768]

        def t_s1(n, bs):
            tm, LT, KE, VT = tparts(n)
            ctok = slice(n * 64, (n + 1) * 64)
            pz = bs[0]
            P.op("pe", lambda e: e.matmul(pz[0:64, 0:192], lhsT=self.GL[0:17, ctok], rhs=self.WLR[0:17, :],
                                          start=True, stop=True), r=[self.GL, self.WLR], w=[pz])
            P.op("dve", lambda e: e.tensor_scalar(out=LT, in0=pz[0:64, 0:192], scalar1=-80.0, scalar2=None,
                                                  op0=ALU.max), r=[pz], w=[(tm, "L")])

        def t_s2(n, bs):
            tm, LT, KE, VT = tparts(n)
            P.op("act", lambda e: e.activation(out=LT, in_=LT, func=AF.Exp, scale=-1.0), r=[(tm, "L")], w=[(tm, "L")])
            P.op("act", lambda e: e.activation(out=LT, in_=LT, func=AF.Ln, bias=1.0, scale=1.0),
                 r=[(tm, "L")], w=[(tm, "L")])

        def t_s3(n, bs):
            tm, LT, KE, VT = tparts(n)
            pz = bs[0]
            P.op("pe", lambda e: e.matmul(pz[0:64, 192:384], lhsT=self.C("su2", rows=64)[:, 0:64], rhs=LT,
                                          start=True, stop=True), r=[self.cst, (tm, "L")], w=[pz])
            P.op("act", lambda e: e.activation(out=KE, in_=pz[0:64, 192:384], func=AF.Exp, scale=NEG),
                 r=[pz], w=[(tm, "KE")])

        def t_s4(n, bs):
            tok = slice(n * 64, (n + 1) * 64)
            pk_, pv = bs[1], bs[2]
            for kc in range(NK):
                P.op("pe", lambda e, kc=kc: e.matmul(
                    pk_[0:64, 0:192], lhsT=self.H[:, kc, tok], rhs=self.WIN[:, kc, O_GK:O_GK + 192],
                    start=(kc == 0), stop=(kc == NK - 1)), r=[self.H, (self.WIN, kc)], w=[pk_])
            for kc in range(NK):
                P.op("pe", lambda e, kc=kc: e.matmul(
                    pv[0:64, 0:384], lhsT=self.H[:, kc, tok], rhs=self.WIN[:, kc, O_GV:O_GV + 384],
                    start=(kc == 0), stop=(kc == NK - 1)), r=[self.H, (self.WIN, kc)], w=[pv])

        def t_s5(n, bs):
            tm, LT, KE, VT = tparts(n)
            pk_, pv = bs[1], bs[2]
            P.op("dve", lambda e: e.tensor_tensor(out=KE, in0=pk_[0:64, 0:192], in1=KE, op=ALU.mult),
                 r=[pk_, (tm, "KE")], w=[(tm, "KE")])
            P.op("act", lambda e: e.activation(out=VT, in_=pv[0:64, 0:384], func=AF.Copy), r=[pv], w=[(tm, "V")])

        def t_s6(n, bs):
            tm, LT, KE, VT = tparts(n)
            psc = bs[0]
            for h in range(6):
                hc = slice(h * TBA + n * 64, h * TBA + n * 64 + 64)
                P.op("pe", lambda e, h=h, hc=hc: e.matmul(
                    psc[0:64, h * 64:(h + 1) * 64], lhsT=GK[:, hc], rhs=GQ[:, hc], start=True, stop=True),
                    r=[GK, GQ], w=[psc])
            P.op("dve", lambda e: e.tensor_tensor(out=tm[:, 768:1152], in0=psc[0:64, 0:384],
                                                  in1=self.C("ut6", rows=64), op=ALU.mult),
                 r=[psc, self.cst], w=[(tm, "SC")])

        for g0 in range(0, NCH, 2):
            for st in (t_s1, t_s2, t_s3, t_s4, t_s5, t_s6):
                for p in range(2):
                    st(g0 + p, BSETS[p])
        OT = [PS[0], PS[1], PS[2]]
        pkv = PS[3]
        for n in range(NCH):
            tm = self.gTM[n]
            ctok = slice(n * 64, (n + 1) * 64)
            for h in range(6):
                jj, hp = h // 2, h % 2
                hc = slice(h * TBA + n * 64, h * TBA + n * 64 + 64)
                oap = OT[jj][hp * 64:(hp + 1) * 64, ctok]
                P.op("pe", lambda e, oap=oap, tm=tm, h=h: e.matmul(
                    oap, lhsT=tm[:, 384 + h * 64:384 + (h + 1) * 64], rhs=tm[:, 768 + h * 64:768 + (h + 1) * 64],
                    start=True, stop=False), r=[(tm, "V"), (tm, "SC")], w=[OT[jj]])
                P.op("pe", lambda e, oap=oap, h=h, hc=hc: e.matmul(
                    oap, lhsT=gS[:, h * 64:(h + 1) * 64], rhs=GQ[:, hc], start=False, stop=True),
                    r=[gS, GQ], w=[OT[jj]])
            for h in range(6):
                P.op("pe", lambda e, tm=tm, h=h: e.matmul(
                    pkv[0:32, h * 64:(h + 1) * 64], lhsT=tm[:, 192 + h * 32:192 + (h + 1) * 32],
                    rhs=tm[:, 384 + h * 64:384 + (h + 1) * 64], start=True, stop=True),
                    r=[(tm, "KE"), (tm, "V")], w=[pkv])
            col = n * 64 + 63
            decB = GEB[:].rearrange("p (h t) -> p h t", h=6)[:, :, col:col + 1].broadcast_to([32, 6, 64])
            S3 = gS[0:32, :].rearrange("p (h v) -> p h v", h=6)
            P.op("dve", lambda e, S3=S3, decB=decB: e.tensor_tensor(out=S3, in0=S3, in1=decB, op=ALU.mult),
                 r=[gS, GEB], w=[gS])
            P.op("dve", lambda e: e.tensor_tensor(out=gS[0:32, :], in0=gS[0:32, :], in1=pkv[0:32, 0:384], op=ALU.add),
                 r=[gS, pkv], w=[gS])
        NPS = [PS[3], PS[4], PS[5]]
        NT = [(self.tget(), self.tget()) for _ in range(3)]
        for jj in range(3):
            osb, sq = NT[jj]
            P.op("act", lambda e, osb=osb, jj=jj: e.activation(out=osb[:], in_=OT[jj][:], func=AF.Copy), r=[OT[jj]], w=[osb])
            P.op("act", lambda e, sq=sq, jj=jj: e.activation(out=sq[:], in_=OT[jj][:], func=AF.Square), r=[OT[jj]], w=[sq])
        for jj in range(3):
            osb, sq = NT[jj]
            pss = NPS[jj]
            P.op("pe", lambda e, sq=sq, pss=pss: e.matmul(pss[:], lhsT=self.C("blk64"), rhs=sq[:], start=True, stop=True),
                 r=[self.cst, sq], w=[pss])
        for jj in range(3):
            osb, sq = NT[jj]
            pss = NPS[jj]
            P.op("act", lambda e, sq=sq, pss=pss: e.activation(out=sq[:], in_=pss[:], func=AF.Ln, bias=self.epsc[:, 0:1],
                                                               scale=1.0 / 64), r=[pss, self.epsc], w=[sq])
        for jj in range(3):
            osb, sq = NT[jj]
            P.op("act", lambda e, sq=sq: e.activation(out=sq[:], in_=sq[:], func=AF.Exp, scale=-0.5), r=[sq], w=[sq])
        for jj in range(3):
            osb, sq = NT[jj]
            P.op("dve", lambda e, osb=osb, sq=sq: e.scalar_tensor_tensor(
                out=osb[:], in0=osb[:], scalar=self.PK("glanw%d" % l), in1=sq[:], op0=ALU.mult, op1=ALU.mult),
                r=[osb, sq, self.pk], w=[osb])
        for jj in range(3):
            osb, sq = NT[jj]
            pgz = NPS[jj]
            self.proj_fm(pgz, O_GZ + jj * 128, 128)
            P.op("act", lambda e, sq=sq, pgz=pgz: e.activation(out=sq[:], in_=pgz[:], func=AF.Silu), r=[pgz], w=[sq])
        for jj in range(3):
            osb, sq = NT[jj]
            P.op("dve", lambda e, osb=osb, sq=sq, jj=jj: e.tensor_tensor(out=self.MT[:, 0 + jj, :], in0=osb[:], in1=sq[:],
                                                                         op=ALU.mult), r=[osb, sq], w=[(self.MT, 0 + jj)])
        for a_, b_ in NT:
            self.tput(a_, b_)
        P.pop_scope(barrier=False)

    def dn_setup(self, l):
        P = self.P
        self.dS = P.sb("dn_S", [64, 384], F32)
        P.op("pool", lambda e: e.memset(self.dS[:], 0.0), w=[self.dS])
        self.dHist = P.sb("dn_hist", [128, 9, 3], F32)
        P.op("pool", lambda e: e.memset(self.dHist[:], 0.0), w=[self.dHist])
        self.dnA = P.sb("dn_nA", [6, 1], F32)
        P.op("act", lambda e: e.activation(out=self.dnA[:], in_=self.PK("dnalog%d" % l, rows=6), func=AF.Exp),
             r=[self.pk], w=[self.dnA])
        P.op("dve", lambda e: e.tensor_scalar(out=self.dnA[:], in0=self.dnA[:], scalar1=-1.0, scalar2=None, op0=ALU.mult),
             r=[self.dnA], w=[self.dnA])

    def dn_block(self, l, b):
        P = self.P
        PS = self.PSA
        NCH = TBA // CH
        dS, dHist = self.dS, self.dHist
        P.push_scope()
        C = [P.sb("dn_c%d" % j, [128, TBA], F32) for j in range(9)]
        P.push_scope()
        DX = P.sb("dn_x", [128, 9, 3 + TBA], F32)
        P.op("dve", lambda e: e.tensor_copy(out=DX[:, :, 0:3], in_=dHist[:]), r=[dHist], w=[DX])
        for j in range(9):
            pb = PS[3 + j % 4]
            self.proj_fm(pb, O_DQ + j * 128, 128)
            P.op("act", lambda e, j=j, pb=pb: e.activation(out=DX[:, j, 3:3 + TBA], in_=pb[:], func=AF.Copy), r=[pb], w=[DX])
        P.op("dve", lambda e: e.tensor_copy(out=dHist[:], in_=DX[:, :, TBA:TBA + 3]), r=[DX], w=[dHist])
        wv = lambda j, t: self.PK("dncw%d" % l, col=j * 4 + t)
        for j in range(9):
            P.op("dve", lambda e, j=j: e.tensor_scalar(out=C[j][:], in0=DX[:, j, 0:TBA], scalar1=wv(j, 0), scalar2=None,
                                                       op0=ALU.mult), r=[DX, self.pk], w=[C[j]])
        for t in range(1, 4):
            for j in range(9):
                P.op("dve", lambda e, j=j, t=t: e.scalar_tensor_tensor(out=C[j][:], in0=DX[:, j, t:t + TBA], scalar=wv(j, t),
                                                                       in1=C[j][:], op0=ALU.mult, op1=ALU.add),
                     r=[DX, self.pk, C[j]], w=[C[j]])
        for j in range(9):
            P.op("act", lambda e, j=j: e.activation(out=C[j][:], in_=C[j][:], func=AF.Silu), r=[C[j]], w=[C[j]])
        P.pop_scope(barrier=False)
        P.push_scope()
        SQs = [P.sb("dn_sq%d" % j, [128, TBA], F32) for j in range(6)]
        pss = [PS[j] for j in range(6)]
        for j in range(6):
            P.op("act", lambda e, j=j: e.activation(out=SQs[j][:], in_=C[j][:], func=AF.Square), r=[C[j]], w=[SQs[j]])
        for j in range(6):
            P.op("pe", lambda e, j=j: e.matmul(pss[j][:], lhsT=self.C("blk64"), rhs=SQs[j][:], start=True, stop=True),
                 r=[self.cst, SQs[j]], w=[pss[j]])
        for j in range(6):
            P.op("act", lambda e, j=j: e.activation(out=SQs[j][:], in_=pss[j][:], func=AF.Ln, bias=self.epsc[:, 0:1], scale=1.0),
                 r=[pss[j], self.epsc], w=[SQs[j]])
        for j in range(6):
            P.op("act", lambda e, j=j: e.activation(out=SQs[j][:], in_=SQs[j][:], func=AF.Exp, scale=-0.5), r=[SQs[j]], w=[SQs[j]])
        for j in range(6):
            P.op("dve", lambda e, j=j: e.scalar_tensor_tensor(
                out=C[j][:], in0=C[j][:], scalar=(0.125 if j < 3 else 1.0), in1=SQs[j][:], op0=ALU.mult, op1=ALU.mult),
                r=[C[j], SQs[j]], w=[C[j]])
        P.pop_scope(barrier=False)
        QN, KN = C[0:3], C[3:6]
        BT = P.sb("dn_bt", [6, TBA], F32)
        GC = P.sb("dn_gc", [6, TBA], F32)
        EG = P.sb("dn_eg", [6, TBA], F32)
        BG = P.sb("dn_bg", [6, TBA], F32)
        ER = P.sb("dn_er", [6, TBA], F32)
        T1 = P.sb("dn_t1", [6, TBA], F32)
        T2 = P.sb("dn_t2", [6, TBA], F32)
        NG = P.sb("dn_ng", [6, 6, TBA], F32)
        pb = PS[3]
        self.proj_fm(pb, O_DB, 6)
        P.op("act", lambda e: e.activation(out=BT[:], in_=pb[0:6, :], func=AF.Sigmoid), r=[pb], w=[BT])
        pa = PS[4]
        self.proj_fm(pa, O_DA, 6)
        P.op("act", lambda e: e.activation(out=T2[:], in_=pa[0:6, :], func=AF.Identity,
                                           bias=self.PK("dndtb%d" % l, rows=6), scale=1.0), r=[pa, self.pk], w=[T2])
        P.op("act", lambda e: e.activation(out=T1[:], in_=T2[:], func=AF.Abs), r=[T2], w=[T1])
        P.op("act", lambda e: e.activation(out=T1[:], in_=T1[:], func=AF.Exp, scale=-1.0), r=[T1], w=[T1])
        P.op("act", lambda e: e.activation(out=T1[:], in_=T1[:], func=AF.Ln, bias=1.0, scale=1.0), r=[T1], w=[T1])
        P.op("dve", lambda e: e.tensor_scalar(out=T2[:], in0=T2[:], scalar1=0.0, scalar2=None, op0=ALU.max), r=[T2], w=[T2])
        P.op("dve", lambda e: e.tensor_tensor(out=T1[:], in0=T1[:], in1=T2[:], op=ALU.add), r=[T1, T2], w=[T1])
        P.op("dve", lambda e: e.tensor_scalar(out=T2[:], in0=T1[:], scalar1=self.dnA[:, 0:1], scalar2=None, op0=ALU.mult),
             r=[T1, self.dnA], w=[T2])
        P.op("dve", lambda e: e.tensor_tensor_scan(out=GC[:], data0=self.C("rst", rows=6)[:, 0:TBA], data1=T2[:], initial=0.0,
                                                   op0=ALU.mult, op1=ALU.add), r=[T2, self.cst], w=[GC])
        P.op("act", lambda e: e.activation(out=EG[:], in_=GC[:], func=AF.Exp), r=[GC], w=[EG])
        P.op("dve", lambda e: e.tensor_tensor(out=BG[:], in0=BT[:], in1=EG[:], op=ALU.mult), r=[BT, EG], w=[BG])
        for n in range(NCH):
            col = n * 64 + 63
            P.op("dve", lambda e, n=n, col=col: e.tensor_scalar(
                out=ER[:, n * 64:(n + 1) * 64], in0=GC[:, n * 64:(n + 1) * 64], scalar1=-1.0, scalar2=GC[:, col:col + 1],
                op0=ALU.mult, op1=ALU.add), r=[GC], w=[ER])
        P.op("act", lambda e: e.activation(out=ER[:], in_=ER[:], func=AF.Exp), r=[ER], w=[ER])
        for h in range(6):
            P.op("dve", lambda e, h=h: e.tensor_scalar(out=NG[:, h, :], in0=GC[:], scalar1=self.C("noh6", rows=6)[:, h:h + 1],
                                                       scalar2=None, op0=ALU.mult), r=[GC, self.cst], w=[NG])
        EGd = P.sb("dn_egd", [6, NCH * 6], F32)
        DEC = P.sb("dn_dec", [64, NCH * 6], F32)
        for n in range(NCH):
            col = n * 64 + 63
            P.op("dve", lambda e, n=n, col=col: e.tensor_scalar(
                out=EGd[:, n * 6:(n + 1) * 6], in0=self.C("oh6", rows=6), scalar1=EG[:, col:col + 1], scalar2=None,
                op0=ALU.mult), r=[EG, self.cst], w=[EGd])
        pdx = PS[5]
        P.op("pe", lambda e: e.matmul(pdx[0:64, 0:NCH * 6], lhsT=self.C("ones", rows=6)[:, 0:64], rhs=EGd[:],
                                      start=True, stop=True), r=[self.cst, EGd], w=[pdx])
        P.op("act", lambda e: e.activation(out=DEC[:], in_=pdx[0:64, 0:NCH * 6], func=AF.Copy), r=[pdx], w=[DEC])
        KB = [P.sb("dn_kb%d" % j, [128, TBA], F32) for j in range(3)]
        KG = [P.sb("dn_kg%d" % j, [128, TBA], F32) for j in range(3)]
        QG = [P.sb("dn_qg%d" % j, [128, TBA], F32) for j in range(3)]
        KR = [P.sb("dn_kr%d" % j, [128, TBA], F32) for j in range(3)]
        VB = C[6:9]
        nb = [0]

        def bprod(fld, dst, src, j):
            pbx = PS[3 + nb[0] % 4]
            nb[0] += 1
            P.op("pe", lambda e: e.matmul(pbx[:], lhsT=self.C("e6_%d" % j, rows=6), rhs=fld[:], start=True, stop=True),
                 r=[self.cst, fld], w=[pbx])
            P.op("dve", lambda e: e.tensor_tensor(out=dst[:], in0=src[:], in1=pbx[:], op=ALU.mult), r=[src, pbx], w=[dst])

        for j in range(3):
            bprod(BT, KB[j], KN[j], j)
            bprod(BG, KG[j], KN[j], j)
            bprod(BT, VB[j], VB[j], j)
            bprod(EG, QG[j], QN[j], j)
            bprod(ER, KR[j], KN[j], j)
        XO = {}
        for nm, X in (("kn", KN), ("kb", KB), ("qn", QN), ("qg", QG)):
            xo = P.sb("dn_xo_" + nm, [64, 3 * TBA], F32)
            XO[nm] = xo
            for j in range(3):
                px = PS[3 + nb[0] % 4]
                nb[0] += 1
                P.op("pe", lambda e, px=px, X=X, j=j: e.matmul(px[0:64, :], lhsT=self.C("selhi"), rhs=X[j][:],
                                                               start=True, stop=True), r=[self.cst, X[j]], w=[px])
                P.op("act", lambda e, px=px, xo=xo, j=j: e.activation(out=xo[:, j * TBA:(j + 1) * TBA], in_=px[0:64, :],
                                                                      func=AF.Copy), r=[px], w=[xo])
        XE = {"kn": KN, "kb": KB, "qn": QN, "qg": QG}

        def hd(nm, h, ctok):
            j = h // 2
            if h % 2 == 0:
                return XE[nm][j][0:64, ctok], XE[nm][j]
            return XO[nm][:, j * TBA + ctok.start:j * TBA + ctok.stop], XO[nm]

        NIL = int(os.environ.get("DN_NIL", "2"))
        TMa = [P.sb("dn_tma%d" % p, [64, 768], F32) for p in range(NIL)]
        KRt = [P.sb("dn_krt%d" % n, [64, 384], F32) for n in range(NCH)]
        QK = [P.sb("dn_qk%d" % n, [64, 384], F32) for n in range(NCH)]
        U = [P.sb("dn_u%d" % n, [64, 384], F32) for n in range(NCH)]
        WT = [P.sb("dn_wt%d" % n, [64, 384], F32) for n in range(NCH)]
        DT = [P.sb("dn_dt%d" % p, [64, 384], F32) for p in range(NIL)]
        Mt = [[P.sb("dn_m%d_%d" % (p, i), [64, 384], F32) for i in range(2)] for p in range(NIL)]
        Nt = [[P.sb("dn_n%d_%d" % (p, i), [64, 384], F32) for i in range(2)] for p in range(NIL)]
        W = [P.sb("dn_w%d" % p, [64, 384], F32) for p in range(NIL)]
        VNEW = P.sb("dn_vnew", [64, 384], F32)
        id64 = self.C("ident", rows=64)[:, 0:64]
        hcs = [slice(h * 64, (h + 1) * 64) for h in range(6)]
        bk = [0]

        def nbank():
            bk[0] += 1
            return PS[3 + bk[0] % 5]

        def st_transpose(n, p):
            ctok = slice(n * 64, (n + 1) * 64)
            for qi, (X, dst, dcol, dep) in enumerate(((KG, TMa[p], 0, (TMa[p], 0)), (VB, TMa[p], 384, (TMa[p], 1)),
                                                       (KR, KRt[n], 0, KRt[n]))):
                pt = nbank()
                for j in range(3):
                    P.op("pe", lambda e, pt=pt, X=X, j=j: e.transpose(out=pt[0:64, j * 128:(j + 1) * 128], in_=X[j][:, ctok],
                                                                      identity=self.C("ident")), r=[X[j], self.cst], w=[pt])
                P.op("act", lambda e, pt=pt, dst=dst, dcol=dcol: e.activation(out=dst[:, dcol:dcol + 384], in_=pt[0:64, 0:384],
                                                                              func=AF.Copy), r=[pt], w=[dep])

        def st_decay(n, p):
            ctok = slice(n * 64, (n + 1) * 64)
            pd = nbank()
            for h in range(6):
                oh_ = pd[0:64, hcs[h]]
                P.op("pe", lambda e, oh_=oh_: e.matmul(oh_, lhsT=id64, rhs=self.C("negm", rows=64), start=True, stop=False),
                     r=[self.cst], w=[pd])
                P.op("pe", lambda e, oh_=oh_, h=h: e.matmul(oh_, lhsT=self.C("selh_%d" % h, rows=6), rhs=GC[:, ctok],
                                                            start=False, stop=False), r=[self.cst, GC], w=[pd])
                P.op("pe", lambda e, oh_=oh_, h=h: e.matmul(oh_, lhsT=NG[:, h, ctok], rhs=self.C("ones", rows=6)[:, 0:64],
                                                            start=False, stop=True), r=[self.cst, NG], w=[pd])
            P.op("act", lambda e: e.activation(out=DT[p][:], in_=pd[0:64, 0:384], func=AF.Exp), r=[pd], w=[DT[p]])

        def st_kk(n, p):
            ctok = slice(n * 64, (n + 1) * 64)
            pk_ = nbank()
            pq = nbank()
            for h in range(6):
                kn_ap, kn_d = hd("kn", h, ctok)
                kb_ap, kb_d = hd("kb", h, ctok)
                qn_ap, qn_d = hd("qn", h, ctok)
                P.op("pe", lambda e, h=h, kn_ap=kn_ap, kb_ap=kb_ap: e.matmul(pk_[0:64, hcs[h]], lhsT=kn_ap, rhs=kb_ap,
                                                                             start=True, stop=True), r=[kn_d, kb_d], w=[pk_])
                P.op("pe", lambda e, h=h, kn_ap=kn_ap, qn_ap=qn_ap: e.matmul(pq[0:64, hcs[h]], lhsT=kn_ap, rhs=qn_ap,
                                                                             start=True, stop=True), r=[kn_d, qn_d], w=[pq])
            M = Mt[p][0]
            P.op("dve", lambda e: e.tensor_tensor(out=M[:], in0=pk_[0:64, 0:384], in1=DT[p][:], op=ALU.mult),
                 r=[pk_, DT[p]], w=[M])
            P.op("pool", lambda e: e.tensor_tensor(out=M[:], in0=M[:], in1=self.C("sut6", rows=64), op=ALU.mult),
                 r=[M, self.cst], w=[M])
            P.op("dve", lambda e: e.tensor_tensor(out=QK[n][:], in0=pq[0:64, 0:384], in1=DT[p][:], op=ALU.mult),
                 r=[pq, DT[p]], w=[QK[n]])

        def st_n0(n, p):
            M, N = Mt[p][0], Nt[p][0]
            pn = nbank()
            for h in range(6):
                P.op("pe", lambda e, h=h: e.transpose(out=pn[0:64, hcs[h]], in_=M[:, hcs[h]], identity=id64),
                     r=[M, self.cst], w=[pn])
            P.op("act", lambda e: e.activation(out=N[:], in_=pn[0:64, 0:384], func=AF.Copy), r=[pn], w=[N])
            P.op("dve", lambda e: e.tensor_tensor(out=W[p][:], in0=self.C("id6", rows=64), in1=M[:], op=ALU.subtract),
                 r=[M, self.cst], w=[W[p]])

        def st_level(i):
            def f(n, p):
                M, N = Mt[p][i % 2], Nt[p][i % 2]
                Mn, Nn = Mt[p][(i + 1) % 2], Nt[p][(i + 1) % 2]
                pn = nbank()
                for h in range(6):
                    P.op("pe", lambda e, h=h: e.matmul(pn[0:64, hcs[h]], lhsT=M[:, hcs[h]], rhs=N[:, hcs[h]],
                                                       start=True, stop=True), r=[M, N], w=[pn])
                P.op("act", lambda e: e.activation(out=Nn[:], in_=pn[0:64, 0:384], func=AF.Copy), r=[pn], w=[Nn])
                if i < 4:
                    pm = nbank()
                    for h in range(6):
                        P.op("pe", lambda e, h=h: e.matmul(pm[0:64, hcs[h]], lhsT=N[:, hcs[h]], rhs=M[:, hcs[h]],
                                                           start=True, stop=True), r=[M, N], w=[pm])
                    P.op("dve", lambda e: e.tensor_copy(out=Mn[:], in_=pm[0:64, 0:384]), r=[pm], w=[Mn])
            return f

        def st_wupd(i):
            def f(n, p):
                Nn = Nt[p][(i + 1) % 2]
                pw = nbank()
                for h in range(6):
                    P.op("pe", lambda e, h=h: e.matmul(pw[0:64, hcs[h]], lhsT=Nn[:, hcs[h]], rhs=W[p][:, hcs[h]],
                                                       start=True, stop=True), r=[Nn, W[p]], w=[pw])
                P.op("dve", lambda e: e.tensor_tensor(out=W[p][:], in0=pw[0:64, 0:384], in1=W[p][:], op=ALU.add),
                     r=[pw, W[p]], w=[W[p]])
            return f

        def st_uw(n, p):
            pu = nbank()
            pw_ = nbank()
            for h in range(6):
                P.op("pe", lambda e, h=h: e.matmul(pu[0:64, hcs[h]], lhsT=W[p][:, hcs[h]],
                                                   rhs=TMa[p][:, 384 + h * 64:384 + (h + 1) * 64],
                                                   start=True, stop=True), r=[W[p], (TMa[p], 1)], w=[pu])
                P.op("pe", lambda e, h=h: e.matmul(pw_[0:64, hcs[h]], lhsT=TMa[p][:, h * 64:(h + 1) * 64], rhs=W[p][:, hcs[h]],
                                                   start=True, stop=True), r=[W[p], (TMa[p], 0)], w=[pw_])
            P.op("act", lambda e: e.activation(out=U[n][:], in_=pu[0:64, 0:384], func=AF.Copy), r=[pu], w=[U[n]])
            P.op("dve", lambda e: e.tensor_copy(out=WT[n][:], in_=pw_[0:64, 0:384]), r=[pw_], w=[WT[n]])

        stages = [st_transpose, st_decay, st_kk, st_n0]
        for i in range(int(os.environ.get("DN_LEVELS", "5"))):
            stages += [st_level(i), st_wupd(i)]
        stages.append(st_uw)
        for g0 in range(0, NCH, NIL):
            for st in stages:
                for p in range(NIL):
                    st(g0 + p, p)
        OT = [PS[0], PS[1], PS[2]]
        pv = PS[3]
        pkv = PS[4]
        for n in range(NCH):
            ctok = slice(n * 64, (n + 1) * 64)
            for h in range(6):
                P.op("pe", lambda e, h=h, n=n: e.matmul(pv[0:64, hcs[h]], lhsT=WT[n][:, hcs[h]], rhs=dS[:, hcs[h]],
                                                        start=True, stop=True), r=[WT[n], dS], w=[pv])
            P.op("dve", lambda e, n=n: e.tensor_tensor(out=VNEW[:], in0=U[n][:], in1=pv[0:64, 0:384], op=ALU.subtract),
                 r=[U[n], pv], w=[VNEW])
            for h in range(6):
                oap = OT[h // 2][(h % 2) * 64:(h % 2 + 1) * 64, ctok]
                qg_ap, qg_d = hd("qg", h, ctok)
                P.op("pe", lambda e, h=h, oap=oap, qg_ap=qg_ap: e.matmul(oap, lhsT=dS[:, hcs[h]], rhs=qg_ap,
                                                                         start=True, stop=False), r=[dS, qg_d], w=[OT[h // 2]])
                P.op("pe", lambda e, h=h, oap=oap, n=n: e.matmul(oap, lhsT=VNEW[:, hcs[h]], rhs=QK[n][:, hcs[h]],
                                                                 start=False, stop=True), r=[VNEW, QK[n]], w=[OT[h // 2]])
            for h in range(6):
                P.op("pe", lambda e, h=h, n=n: e.matmul(pkv[0:64, hcs[h]], lhsT=KRt[n][:, hcs[h]], rhs=VNEW[:, hcs[h]],
                                                        start=True, stop=True), r=[KRt[n], VNEW], w=[pkv])
            S3 = dS[:].rearrange("p (h v) -> p h v", h=6)
            decB = DEC[:, n * 6:(n + 1) * 6].rearrange("p (h o) -> p h o", o=1).broadcast_to([64, 6, 64])
            P.op("dve", lambda e, S3=S3, decB=decB: e.tensor_tensor(out=S3, in0=S3, in1=decB, op=ALU.mult), r=[dS, DEC], w=[dS])
            P.op("dve", lambda e: e.tensor_tensor(out=dS[:], in0=dS[:], in1=pkv[0:64, 0:384], op=ALU.add), r=[dS, pkv], w=[dS])
        NPS = [PS[5], PS[6], PS[7]]
        NT = [(C[jj], C[3 + jj]) for jj in range(3)]
        for jj in range(3):
            osb, sq = NT[jj]
            P.op("act", lambda e, osb=osb, jj=jj: e.activation(out=osb[:], in_=OT[jj][:], func=AF.Copy), r=[OT[jj]], w=[osb])
            P.op("act", lambda e, sq=sq, jj=jj: e.activation(out=sq[:], in_=OT[jj][:], func=AF.Square), r=[OT[jj]], w=[sq])
        for jj in range(3):
            osb, sq = NT[jj]
            pss = NPS[jj]
            P.op("pe", lambda e, sq=sq, pss=pss: e.matmul(pss[:], lhsT=self.C("blk64"), rhs=sq[:], start=True, stop=True),
                 r=[self.cst, sq], w=[pss])
        for jj in range(3):
            osb, sq = NT[jj]
            pss = NPS[jj]
            P.op("act", lambda e, sq=sq, pss=pss: e.activation(out=sq[:], in_=pss[:], func=AF.Ln, bias=self.epsc[:, 0:1],
                                                               scale=1.0 / 64), r=[pss, self.epsc], w=[sq])
        for jj in range(3):
            osb, sq = NT[jj]
            P.op("act", lambda e, sq=sq: e.activation(out=sq[:], in_=sq[:], func=AF.Exp, scale=-0.5), r=[sq], w=[sq])
        for jj in range(3):
            osb, sq = NT[jj]
            P.op("dve", lambda e, osb=osb, sq=sq: e.scalar_tensor_tensor(
                out=osb[:], in0=osb[:], scalar=self.PK("dnnw%d" % l), in1=sq[:], op0=ALU.mult, op1=ALU.mult),
                r=[osb, sq, self.pk], w=[osb])
        for jj in range(3):
            osb, sq = NT[jj]
            pgz = NPS[jj]
            self.proj_fm(pgz, O_DZ + jj * 128, 128)
            P.op("act", lambda e, sq=sq, pgz=pgz: e.activation(out=sq[:], in_=pgz[:], func=AF.Silu), r=[pgz], w=[sq])
        for jj in range(3):
            osb, sq = NT[jj]
            P.op("dve", lambda e, osb=osb, sq=sq, jj=jj: e.tensor_tensor(out=self.MT[:, 5 + jj, :], in0=osb[:], in1=sq[:],
                                                                         op=ALU.mult), r=[osb, sq], w=[(self.MT, 5 + jj)])
        P.pop_scope(barrier=False)

    def passB(self, l, last):
        P = self.P
        S, SBT = self.S, self.SBT
        moe = (l % 2 == 1)
        P.push_scope()
        nbs = SBT // TB
        H2 = P.sb("B_H2", [128, NK, SBT], BF16)
        YACC = P.sb("B_YACC", [128, NK, SBT], F32)
        XT = P.sb("B_XT", [128, NK, TB], F32)
        SQ = P.sb("B_SQ", [128, NK, TB], BF16)
        rstd = P.sb("B_rstd", [128, TB], F32)
        tmp = [P.sb("B_tmp%d" % i, [128, TB], F32) for i in range(2)]
        WG = [P.sb("B_WG%d" % i, [128, NK, 512], BF16) for i in range(2)]
        WU = [P.sb("B_WU%d" % i, [128, NK, 512], BF16) for i in range(2)]
        WD = [P.sb("B_WD%d" % i, [128, 4, D], BF16) for i in range(2)]
        AT = [P.sb("B_AT%d" % i, [128, 4, TB], BF16) for i in range(2)]
        SG = [P.sb("B_SG%d" % i, [128, TB], F32) for i in range(2)]
        mod = self.mod[l]
        if moe:
            CB = P.sb("B_cstb", [8, self.ccb.n], F32)
            P.dma("sp", CB[:], self.cstb_d[0:8, :], r=[self.cstb_d], w=[CB])
            HE = [P.sb("B_HE%d" % i, [128, NK, TB], BF16) for i in range(2)]
            RW = P.sb("B_RW", [128, NK, NE], F32)
            P.dma("sp", RW[:], self.router_w[0].rearrange("(k p) n -> p k n", p=128), r=[self.router_w], w=[RW])
            GT = P.sb("B_GT", [128, SBT // 128, NE], F32)
            GF = P.sb("B_GF", [NE, SBT], F32)
            gs = [P.sb("B_gs%d" % i, [128, NE], F32) for i in range(4)]
            gm = [P.sb("B_gm%d" % i, [128, 1], F32) for i in range(4)]
            experts = list(range(NE))
            dff = D_FFE
        else:
            experts = [0]
            dff = D_FF
        fgs = []
        f0 = 0
        while f0 < dff:
            fs = min(512, dff - f0)
            fgs.append((f0, fs))
            f0 += fs
        nsb = S // SBT
        for sb in range(nsb):
            for bi in range(nbs):
                b = sb * nbs + bi
                if moe:
                    lgp = self.PSB[6]
                    first = [True]

                    def hf_cb(k, t, bi=bi, lgp=lgp, first=first):
                        for tt in range(4):
                            st = first[0]
                            first[0] = False
                            P.op("pe", lambda e, k=k, t=t, tt=tt, st=st: e.matmul(
                                lgp[:, tt * NE:(tt + 1) * NE], lhsT=t[:, tt * 128:(tt + 1) * 128], rhs=RW[:, k, :],
                                start=st, stop=(k == NK - 1), skip_group_check=True), r=[t, RW], w=[lgp])
                else:
                    hf_cb = None
                self.load_norm(XT, b, self.gv2[l], lambda k: mod[:, 24 + k:24 + k + 1],
                               lambda k, bi=bi: H2[:, k, bi * TB:(bi + 1) * TB], (H2, bi), SQ, tmp, rstd, hf_cb=hf_cb)
                if moe:
                    for tt in range(4):
                        ti = bi * 4 + tt
                        lg = lgp[:, tt * NE:(tt + 1) * NE]
                        g0, g1_, g2_, g3_ = gs
                        m1, m2, sm, _ = gm
                        P.op("dve", lambda e, lg=lg: e.tensor_copy(out=g0[:], in_=lg), r=[lgp], w=[g0])
                        P.op("dve", lambda e: e.tensor_reduce(out=m1[:], in_=g0[:], axis=AX.X, op=ALU.max), r=[g0], w=[m1])
                        P.op("dve", lambda e: e.tensor_scalar(out=g1_[:], in0=g0[:], scalar1=m1[:, 0:1], scalar2=-1e30,
                                                              op0=ALU.is_ge, op1=ALU.mult), r=[g0, m1], w=[g1_])
                        P.op("dve", lambda e: e.tensor_tensor(out=g2_[:], in0=g0[:], in1=g1_[:], op=ALU.add), r=[g0, g1_], w=[g2_])
                        P.op("dve", lambda e: e.tensor_reduce(out=m2[:], in_=g2_[:], axis=AX.X, op=ALU.max), r=[g2_], w=[m2])
                        P.op("dve", lambda e: e.tensor_scalar(out=g1_[:], in0=g0[:], scalar1=m2[:, 0:1], scalar2=None,
                                                              op0=ALU.is_ge), r=[g0, m2], w=[g1_])
                        P.op("dve", lambda e: e.tensor_scalar(out=g2_[:], in0=g0[:], scalar1=m1[:, 0:1], scalar2=None,
                                                              op0=ALU.subtract), r=[g0, m1], w=[g2_])
                        P.op("act", lambda e: e.activation(out=g2_[:], in_=g2_[:], func=AF.Exp), r=[g2_], w=[g2_])
                        P.op("dve", lambda e: e.tensor_tensor(out=g2_[:], in0=g2_[:], in1=g1_[:], op=ALU.mult), r=[g2_, g1_], w=[g2_])
                        P.op("dve", lambda e: e.tensor_reduce(out=sm[:], in_=g2_[:], axis=AX.X, op=ALU.add), r=[g2_], w=[sm])
                        P.op("dve", lambda e: e.reciprocal(out=sm[:], in_=sm[:]), r=[sm], w=[sm])
                        P.op("dve", lambda e, ti=ti: e.tensor_scalar(out=GT[:, ti, :], in0=g2_[:], scalar1=sm[:, 0:1],
                                                                     scalar2=None, op0=ALU.mult), r=[g2_, sm], w=[GT])
                        tp = self.PSB[7]
                        P.op("pe", lambda e, ti=ti, tp=tp: e.transpose(out=tp[0:NE, 0:128], in_=GT[:, ti, :],
                                                                       identity=self.C("ident")), r=[GT, self.cst], w=[tp])
                        P.op("act", lambda e, ti=ti, tp=tp: e.activation(out=GF[:, ti * 128:(ti + 1) * 128],
                                                                         in_=tp[0:NE, 0:128], func=AF.Copy), r=[tp], w=[GF])
            units = []
            for ei, ex in enumerate(experts):
                for fi, (f0, fs) in enumerate(fgs):
                    for bi in range(nbs):
                        units.append((ei, ex, fi, f0, fs, bi))
            wslot = {}
            nload = [0]

            def load_w(ex, fi, f0, fs):
                s = nload[0] % 2
                nload[0] += 1
                wslot[(ex, fi)] = s
                nfc = fs // 128
                if moe:
                    wg, wu, wd = self.moe_wg[0, ex], self.moe_wu[0, ex], self.moe_wd[0, ex]
                else:
                    wg, wu, wd = self.ffn_wg[0], self.ffn_wu[0], self.ffn_wd[0]
                P.dma("pool", WG[s][:, :, 0:fs], wg[:, f0:f0 + fs].rearrange("(k p) n -> p k n", p=128),
                      r=[self.moe_wg], w=[WG[s]])
                P.dma("pool", WU[s][:, :, 0:fs], wu[:, f0:f0 + fs].rearrange("(k p) n -> p k n", p=128),
                      r=[self.moe_wg], w=[WU[s]])
                P.dma("pool", WD[s][:, 0:nfc, :], wd[f0:f0 + fs, :].rearrange("(k p) n -> p k n", p=128),
                      r=[self.moe_wg], w=[WD[s]])

            efs = [(ex, fi, f0, fs) for ex in experts for fi, (f0, fs) in enumerate(fgs)]
            load_w(*efs[0])
            he_slot = {}
            nhe = [0]
            gcount = [0]
            ycount = [0]

            def stage1(u, ui):
                ei, ex, fi, f0, fs, bi = u
                s = wslot[(ex, fi)]
                nfc = fs // 128
                tok = slice(bi * TB, (bi + 1) * TB)
                if moe:
                    if fi == 0:
                        hs_ = nhe[0] % 2
                        nhe[0] += 1
                        he_slot[(ex, bi)] = hs_
                        gb = self.PSB[7]
                        P.op("pe", lambda e, ex=ex, tok=tok, gb=gb: e.matmul(
                            gb[:], lhsT=CB[:, self.ccb.off["sel8_%d" % ex][0]:self.ccb.off["sel8_%d" % ex][0] + 128],
                            rhs=GF[:, tok], start=True, stop=True), r=[CB, GF], w=[gb])
                        P.op("act", lambda e, gb=gb: e.activation(out=rstd[:], in_=gb[:], func=AF.Copy), r=[gb], w=[rstd])
                        for k in range(NK):
                            P.op("dve", lambda e, k=k, hs_=hs_, tok=tok: e.tensor_tensor(
                                out=HE[hs_][:, k, :], in0=H2[:, k, tok], in1=rstd[:], op=ALU.mult),
                                r=[(H2, bi), rstd], w=[HE[hs_]])
                    hu = HE[he_slot[(ex, bi)]]
                at = AT[ui % 2]
                for fc in range(nfc):
                    pg = self.PSB[gcount[0] % 2]
                    pu = self.PSB[2 + gcount[0] % 2]
                    sg = SG[gcount[0] % 2]
                    gcount[0] += 1
                    for k in range(NK):
                        P.op("pe", lambda e, k=k, fc=fc, pg=pg, s=s, tok=tok: e.matmul(
                            pg[:], lhsT=WG[s][:, k, fc * 128:(fc + 1) * 128], rhs=H2[:, k, tok],
                            start=(k == 0), stop=(k == NK - 1)), r=[WG[s], (H2, bi)], w=[pg])
                    for k in range(NK):
                        if moe:
                            P.op("pe", lambda e, k=k, fc=fc, pu=pu, s=s, hu=hu: e.matmul(
                                pu[:], lhsT=WU[s][:, k, fc * 128:(fc + 1) * 128], rhs=hu[:, k, :],
                                start=(k == 0), stop=(k == NK - 1)), r=[WU[s], hu], w=[pu])
                        else:
                            P.op("pe", lambda e, k=k, fc=fc, pu=pu, s=s, tok=tok: e.matmul(
                                pu[:], lhsT=WU[s][:, k, fc * 128:(fc + 1) * 128], rhs=H2[:, k, tok],
                                start=(k == 0), stop=(k == NK - 1)), r=[WU[s], (H2, bi)], w=[pu])
                    P.op("act", lambda e, sg=sg, pg=pg: e.activation(out=sg[:], in_=pg[:], func=AF.Silu), r=[pg], w=[sg])
                    P.op("dve", lambda e, sg=sg, pu=pu, at=at, fc=fc: e.tensor_tensor(
                        out=at[:, fc, :], in0=pu[:], in1=sg[:], op=ALU.mult), r=[pu, sg], w=[at])

            def stage2(u, ui):
                ei, ex, fi, f0, fs, bi = u
                s = wslot[(ex, fi)]
                nfc = fs // 128
                at = AT[ui % 2]
                tok = slice(bi * TB, (bi + 1) * TB)
                firstacc = (ei == 0 and fi == 0)
                for dc in range(NK):
                    py = self.PSB[4 + ycount[0] % 4]
                    ycount[0] += 1
                    for fc in range(nfc):
                        P.op("pe", lambda e, fc=fc, dc=dc, py=py, s=s, at=at: e.matmul(
                            py[:], lhsT=WD[s][:, fc, dc * 128:(dc + 1) * 128], rhs=at[:, fc, :],
                            start=(fc == 0), stop=(fc == nfc - 1)), r=[WD[s], at], w=[py])
                    if firstacc:
                        P.op("act", lambda e, dc=dc, py=py, tok=tok: e.activation(
                            out=YACC[:, dc, tok], in_=py[:], func=AF.Copy), r=[py], w=[(YACC, (bi, dc))])
                    else:
                        P.op("dve", lambda e, dc=dc, py=py, tok=tok: e.tensor_tensor(
                            out=YACC[:, dc, tok], in0=py[:], in1=YACC[:, dc, tok], op=ALU.add),
                            r=[py, (YACC, (bi, dc))], w=[(YACC, (bi, dc))])

            for ui, u in enumerate(units):
                stage1(u, ui)
                if ui > 0:
                    stage2(units[ui - 1], ui - 1)
                if u[5] == 0:
                    idx = efs.index((u[1], u[2], u[3], u[4]))
                    if idx + 1 < len(efs):
                        load_w(*efs[idx + 1])
            stage2(units[-1], len(units) - 1)
            for bi in range(nbs):
                b = sb * nbs + bi
                tok = slice(bi * TB, (bi + 1) * TB)
                P.dma("sp", XT[:], self.xT[:, b * TB:(b + 1) * TB].rearrange("(k p) n -> p k n", p=128),
                      r=[(self.xT, b)], w=[XT])
                for k in range(NK):
                    P.op("dve", lambda e, k=k, tok=tok: e.scalar_tensor_tensor(
                        out=XT[:, k, :], in0=YACC[:, k, tok], scalar=mod[:, 40 + k:40 + k + 1], in1=XT[:, k, :],
                        op0=ALU.mult, op1=ALU.add), r=[(YACC, (bi, k)), mod, XT], w=[XT])
                if not last or self.dbg:
                    P.dma("sp", self.xT[:, b * TB:(b + 1) * TB].rearrange("(k p) n -> p k n", p=128), XT[:],
                          r=[XT], w=[(self.xT, b)])
                if last:
                    self.final_block(XT, SQ, rstd, tmp, b)
        P.pop_scope()
        self.dbg_dump("dbg_xB%d" % l)

    def final_block(self, XT, SQ, rstd, tmp, b):
        P = self.P
        ps = self.PSB[7]
        P.op("act", lambda e: e.activation(out=SQ[:], in_=XT[:], func=AF.Square), r=[XT], w=[SQ])
        for k in range(NK):
            P.op("pe", lambda e, k=k: e.matmul(ps[:], lhsT=self.ones_bf[:], rhs=SQ[:, k, :],
                                               start=(k == 0), stop=(k == NK - 1)), r=[self.ones_bf, SQ], w=[ps])
        P.op("act", lambda e: e.activation(out=rstd[:], in_=ps[:], func=AF.Ln, bias=self.epsc[:, 0:1], scale=1.0 / D),
             r=[ps, self.epsc], w=[rstd])
        P.op("act", lambda e: e.activation(out=rstd[:], in_=rstd[:], func=AF.Exp, scale=-0.5), r=[rstd], w=[rstd])
        for k in range(NK):
            P.op("dve", lambda e, k=k: e.scalar_tensor_tensor(
                out=XT[:, k, :], in0=XT[:, k, :], scalar=self.PK("fnw", col=k), in1=rstd[:],
                op0=ALU.mult, op1=ALU.mult), r=[XT, self.pk, rstd], w=[XT])
        for tt in range(TB // 128):
            ot = tmp[tt % 2]
            for half in range(2):
                pb = self.PSB[(tt * 2 + half) % 4]
                for q in range(4):
                    k = half * 4 + q
                    P.op("pe", lambda e, pb=pb, q=q, k=k, tt=tt: e.transpose(
                        out=pb[:, q * 128:(q + 1) * 128], in_=XT[:, k, tt * 128:(tt + 1) * 128],
                        identity=self.C("ident")), r=[XT, self.cst], w=[pb])
                o = tmp[half]
                if half == 0:
                    P.op("act", lambda e, pb=pb, o=o: e.activation(out=o[:], in_=pb[:], func=AF.Copy), r=[pb], w=[o])
                else:
                    P.op("dve", lambda e, pb=pb, o=o: e.tensor_copy(out=o[:], in_=pb[:]), r=[pb], w=[o])
                r0 = b * TB + tt * 128
                ev = P.dma("sp", self.out[r0:r0 + 128, half * 512:(half + 1) * 512], o[:], r=[o], w=[(self.out, (r0, half))])
                self.out_events.append(ev)


class _View:
    def __init__(self, buf, bi):
        self.buf = buf
        self.bi = bi
        self.id = buf.id

    def __getitem__(self, idx):
        p, k, n = idx
        assert n == slice(None, None, None)
        return self.buf[p, k, self.bi * TB:(self.bi + 1) * TB]


_PARAM_CACHE = {}


def _dummy_inputs():
    z = lambda *s: np.zeros(s, np.float32)
    return {"ada_b": z(2, 6144), "norm1_w": z(2, 1024), "norm2_w": z(2, 1024), "gla_b_lr2": z(2, 192),
            "gla_norm_w": z(2, 64), "lru_conv_w": z(2, 4, 256), "lru_conv_b": z(2, 256), "lru_b_a": z(2, 256),
            "lru_b_x": z(2, 256), "lru_lambda": z(2, 256), "dn_conv_w": z(2, 4, 1152), "dn_a_log": z(2, 6),
            "dn_dt_bias": z(2, 6), "dn_norm_w": z(2, 64), "final_norm_w": z(1024)}


def build_params_ncols():
    if "c" not in _PARAM_CACHE:
        _PARAM_CACHE["c"] = build_params(_dummy_inputs())
    return _PARAM_CACHE["c"].n


def build_params_offsets():
    build_params_ncols()
    return _PARAM_CACHE["c"].off


def make_in_maps(inputs, S, ncores):
    consts = build_consts().build()
    pk = build_params(inputs).build()
    mats = build_mats(inputs)
    f = lambda a: np.ascontiguousarray(np.asarray(a, np.float32))
    shared = {
        "w_in": f(inputs["w_in"]), "w_out": f(inputs["w_out"]), "ada_w": f(inputs["ada_w"]),
        "ffn_w_gate": f(inputs["ffn_w_gate"]), "ffn_w_up": f(inputs["ffn_w_up"]), "ffn_w_down": f(inputs["ffn_w_down"]),
        "router_w": f(inputs["router_w"]), "moe_w_gate": f(inputs["moe_w_gate"]), "moe_w_up": f(inputs["moe_w_up"]),
        "moe_w_down": f(inputs["moe_w_down"]), "cst": consts, "cstb": build_consts_b().build(), "pk": pk, "wlr2": mats["wlr2"], "lrubd": mats["lrubd"],
    }
    maps = []
    x = np.asarray(inputs["x"], np.float32)
    c = np.asarray(inputs["c"], np.float32)
    for i in range(ncores):
        m = dict(shared)
        m["x"] = np.ascontiguousarray(x[i, :S])
        m["cfm"] = _fm(c[i], 8)
        maps.append(m)
    return maps


_BUILD = {}


def kernel(**inputs):
    S = inputs["x"].shape[1]
    B = inputs["x"].shape[0]
    if "b" not in _BUILD:
        _BUILD["b"] = Builder(S)
    bld = _BUILD["b"]
    maps = make_in_maps(inputs, S, B)
    res = run_bass_kernel_spmd(bld.nc, maps, core_ids=list(range(B)))
    return np.stack([np.asarray(r["out"]) for r in res.results], 0).astype(np.float32)
```

```python
import os
import numpy as np
import concourse.bass as bass
import concourse.mybir as mybir
from concourse.bass_utils import run_bass_kernel_spmd
from contextlib import ExitStack

F32 = mybir.dt.float32
BF16 = mybir.dt.bfloat16
AF = mybir.ActivationFunctionType
ALU = mybir.AluOpType
AX = mybir.AxisListType

D = 1024
NK = 8
D_IN = 3228
D_FF = 2816
NE = 8
D_FFE = 3584
EPS = 1e-6
O_GQ, O_GK, O_GV, O_GZ, O_GLR, O_LX, O_LG, O_DQ, O_DK, O_DV, O_DZ, O_DB, O_DA = (
    0, 192, 384, 768, 1152, 1168, 1424, 1680, 2064, 2448, 2832, 3216, 3222)
TB = 512
TBA = 256
CH = 64


class Buf:
    _n = 0

    def __init__(self, h, name, init_evs=()):
        self.h = h
        self.name = name
        Buf._n += 1
        self.id = Buf._n
        self.init_evs = list(init_evs)

    def __getitem__(self, idx):
        return self.h[idx]


class BufV(Buf):
    def __init__(self, parent, width):
        self.h = parent.h
        self.name = parent.name
        self.id = parent.id
        self.width = width
        self.init_evs = parent.init_evs

    def __getitem__(self, idx):
        full = slice(None, None, None)
        if idx == full:
            return self.h[:, 0:self.width]
        if isinstance(idx, tuple) and len(idx) == 2 and idx[1] == full:
            return self.h[idx[0], 0:self.width]
        return self.h[idx]


class _Unit:
    __slots__ = ("w", "rs")

    def __init__(self):
        self.w = None
        self.rs = []


class _Rec:
    def __init__(self):
        self.call = None

    def __getattr__(self, name):
        def f(*a, **k):
            self.call = (name, a, k)
            return self
        return f


class _Ev:
    __slots__ = ("sem", "val", "eng", "seen")

    def __init__(self, sem, val, eng, seen):
        self.sem = sem
        self.val = val
        self.eng = eng
        self.seen = seen


class Prog:
    ENGS = ("pe", "act", "dve", "pool", "sp")

    def __init__(self, nc, n_dma_sems=16, same_eng_sync=True):
        self.nc = nc
        self.es = ExitStack()
        self.scopes = []
        self.same_eng_sync = same_eng_sync
        self.sems = {}
        self.cnt = {}
        self.seen = {}
        self.streams = {e: [] for e in self.ENGS}
        for e in self.ENGS:
            self.sems[e] = self.es.enter_context(nc.semaphore("s_" + e))
            self.cnt[e] = 0
            self.seen[e] = {}
        self.dma_sems = {}
        self.dma_uses = {}
        self.dma_rr = {}
        for q in ("sp", "act", "pool"):
            self.dma_sems[q] = [self.es.enter_context(nc.semaphore("d_%s%d" % (q, i)))
                                for i in range(n_dma_sems)]
            self.dma_uses[q] = [0] * n_dma_sems
            self.dma_rr[q] = 0
        self.units = {}
        self.ninst = 0
        self.last_ev = {}
        self.freed = {}
        self.scope_bufs = []
        self.buf_units = {}

    def _stack(self):
        return self.scopes[-1] if self.scopes else self.es

    def sb(self, name, shape, dtype):
        self._uid = getattr(self, "_uid", 0) + 1
        name = "%s_u%d" % (name, self._uid)
        h = self._stack().enter_context(self.nc.sbuf_tensor(name, list(shape), dtype))
        b = Buf(h, name, init_evs=self.freed.values())
        if self.scope_bufs:
            self.scope_bufs[-1].append(b)
        return b

    def ps(self, name, shape, dtype=F32):
        h = self._stack().enter_context(self.nc.psum_tensor(name, list(shape), dtype))
        return Buf(h, name)

    def dram(self, name, shape, dtype, kind="Internal"):
        h = self.nc.dram_tensor(name, list(shape), dtype, kind=kind)
        return Buf(h, name)

    def push_scope(self):
        self.scopes.append(ExitStack())
        self.scope_bufs.append([])

    def pop_scope(self, barrier=True):
        bufs = self.scope_bufs.pop()
        if barrier:
            self.barrier()
        else:
            for b in bufs:
                for key in self.buf_units.get(b.id, ()):
                    un = self.units[key]
                    for ev in ([un.w] if un.w is not None else []) + un.rs:
                        cur = self.freed.get(ev.sem)
                        if cur is None or cur.val < ev.val:
                            self.freed[ev.sem] = ev
                for ev in b.init_evs:
                    cur = self.freed.get(ev.sem)
                    if cur is None or cur.val < ev.val:
                        self.freed[ev.sem] = ev
        self.scopes.pop().close()

    def _unit(self, u):
        buf = u if isinstance(u, Buf) else u[0]
        key = (buf.id, None) if isinstance(u, Buf) else (buf.id, u[1])
        un = self.units.get(key)
        if un is None:
            un = self.units[key] = _Unit()
            un.rs = list(buf.init_evs)
            self.buf_units.setdefault(buf.id, []).append(key)
        return un

    def _collect(self, eng, r, w):
        need = {}

        def add(ev):
            if ev is None:
                return
            if ev.eng == eng and (eng in ("pe", "sp") or not self.same_eng_sync):
                return
            cur = need.get(ev.sem)
            if cur is None or cur[0] < ev.val:
                need[ev.sem] = (ev.val, ev)

        for u in r:
            add(self._unit(u).w)
        for u in w:
            un = self._unit(u)
            add(un.w)
            for ev in un.rs:
                add(ev)
        seen = self.seen[eng]
        waits = []
        for sem, (val, ev) in need.items():
            if seen.get(sem, 0) >= val:
                continue
            waits.append((sem, val))
            seen[sem] = val
            for s2, v2 in ev.seen.items():
                if seen.get(s2, 0) < v2:
                    seen[s2] = v2
        return waits

    def _record(self, ev, r, w):
        for u in r:
            self._unit(u).rs.append(ev)
        for u in w:
            un = self._unit(u)
            un.w = ev
            un.rs = []

    def op(self, eng, fn, r=(), w=()):
        waits = self._collect(eng, r, w)
        sem = self.sems[eng]
        self.cnt[eng] += 1
        val = self.cnt[eng]
        evseen = dict(self.seen[eng])
        evseen[sem] = val
        ev = _Ev(sem, val, eng, evseen)
        rec = _Rec()
        fn(rec)
        name, a, k = rec.call

        def fn2(e, name=name, a=a, k=k):
            return getattr(e, name)(*a, **k)
        self.streams[eng].append((waits, fn2, sem, 1))
        self._record(ev, r, w)
        self.ninst += 1
        self.last_ev[eng] = ev
        return ev

    def dma(self, q, out, in_, r=(), w=(), **kw):
        waits = self._collect(q, r, w)
        pool = self.dma_sems[q]
        i = self.dma_rr[q]
        self.dma_rr[q] = (i + 1) % len(pool)
        sem = pool[i]
        m = self.dma_uses[q][i]
        seen = self.seen[q]
        if m > 0 and seen.get(sem, 0) < 16 * m:
            waits.append((sem, 16 * m))
            seen[sem] = 16 * m
        self.dma_uses[q][i] = m + 1
        val = 16 * (m + 1)
        evseen = dict(seen)
        evseen[sem] = val
        ev = _Ev(sem, val, "dma_" + q, evseen)

        def fn(e, out=out, in_=in_, kw=kw):
            return e.dma_start(out=out, in_=in_, **kw)
        self.streams[q].append((waits, fn, sem, 16))
        self._record(ev, r, w)
        self.ninst += 1
        self.last_ev[("dma", q, i)] = ev
        return ev

    def wait_event(self, eng, ev):
        seen = self.seen[eng]
        if seen.get(ev.sem, 0) >= ev.val:
            return
        seen[ev.sem] = ev.val
        for s2, v2 in ev.seen.items():
            if seen.get(s2, 0) < v2:
                seen[s2] = v2
        self.streams[eng].append(([(ev.sem, ev.val)], None, None, 0))

    def barrier(self):
        evs = list(self.last_ev.values())
        for e in self.ENGS:
            for ev in evs:
                if ev.eng == e and e in ("pe", "sp"):
                    continue
                self.wait_event(e, ev)
        self.freed = {}

    def emit(self):
        nc = self.nc
        streams = self.streams
        with nc.Block() as block:
            def run(e, lst):
                for waits, fn, sem, inc in lst:
                    for (s, v) in waits:
                        e.wait_ge(s, v)
                    if fn is not None:
                        fn(e).then_inc(sem, inc)

            @block.tensor
            def _(e):
                run(e, streams["pe"])

            @block.scalar
            def _(e):
                run(e, streams["act"])

            @block.vector
            def _(e):
                run(e, streams["dve"])

            @block.gpsimd
            def _(e):
                run(e, streams["pool"])

            @block.sync
            def _(e):
                run(e, streams["sp"])


def _fm(v, nchunk):
    return np.ascontiguousarray(np.asarray(v, np.float32).reshape(nchunk, 128).T)


class Cols:
    def __init__(self):
        self.n = 0
        self.off = {}
        self.parts = []

    def add(self, name, arr):
        arr = np.asarray(arr, np.float32)
        if arr.ndim == 1:
            arr = arr[:, None]
        if arr.shape[0] < 128:
            arr = np.concatenate([arr, np.zeros((128 - arr.shape[0], arr.shape[1]), np.float32)], 0)
        self.off[name] = (self.n, arr.shape[1])
        self.n += arr.shape[1]
        self.parts.append(arr)

    def build(self):
        return np.ascontiguousarray(np.concatenate(self.parts, axis=1))


def build_consts():
    c = Cols()
    p = np.arange(128)
    c.add("ident", np.eye(128, dtype=np.float32))
    c.add("ones", np.ones((128, 128), np.float32))
    c.add("blk64", (p[:, None] // 64 == p[None, :] // 64).astype(np.float32))
    t = np.arange(TB)
    c.add("rst", np.tile((t % CH != 0).astype(np.float32)[None, :], (128, 1)))
    s = p % 64
    cc = np.arange(64)
    m = (cc[None, :] >= s[:, None]).astype(np.float32)
    c.add("ut6", np.tile(m, (1, 6)))
    ms = (cc[None, :] > s[:, None]).astype(np.float32)
    c.add("sut6", np.tile(ms, (1, 6)))
    c.add("su2", ((p[:, None] // 64 == p[None, :] // 64) & (p[:, None] > p[None, :])).astype(np.float32))
    for j in range(3):
        e = np.zeros((128, 128), np.float32)
        for h in range(6):
            e[h, :] = (2 * j + p // 64 == h)
        c.add("e6_%d" % j, e)
    nm = np.zeros((128, 64), np.float32)
    nm[:64, :] = np.where(cc[None, :] >= cc[:, None], 0.0, -30000.0)
    c.add("negm", nm)
    i6 = np.zeros((128, 384), np.float32)
    i6[:64, :] = np.tile(np.eye(64, dtype=np.float32), (1, 6))
    c.add("id6", i6)
    sh = np.zeros((128, 64), np.float32)
    sh[64 + np.arange(64), np.arange(64)] = 1.0
    c.add("selhi", sh)
    for h in range(6):
        a = np.zeros((128, 64), np.float32)
        a[h, :] = 1.0
        c.add("selh_%d" % h, a)
    oh = np.zeros((128, 6), np.float32)
    for h in range(6):
        oh[h, h] = 1.0
    c.add("oh6", oh)
    c.add("noh6", -oh)
    return c


def build_consts_b():
    c = Cols()
    for e_ in range(8):
        s8 = np.zeros((8, 128), np.float32)
        s8[e_, :] = 1.0
        c.add("sel8_%d" % e_, s8)
    return c


def build_params(inp):
    c = Cols()
    for l in range(2):
        c.add("ada_b%d" % l, _fm(inp["ada_b"][l], 48))
        c.add("n1w%d" % l, _fm(inp["norm1_w"][l], 8))
        c.add("n2w%d" % l, _fm(inp["norm2_w"][l], 8))
        b = np.asarray(inp["gla_b_lr2"][l], np.float32)
        c.add("glab%d" % l, np.stack([b[0:96], b[96:192]], 1))
        c.add("glanw%d" % l, np.tile(np.asarray(inp["gla_norm_w"][l], np.float32), 2))
        cw = np.asarray(inp["lru_conv_w"][l], np.float32)
        c.add("lrucw%d" % l, np.stack([cw[j, ch * 128:(ch + 1) * 128] for ch in range(2) for j in range(4)], 1))
        c.add("lrucb%d" % l, _fm(inp["lru_conv_b"][l], 2))
        c.add("lruba%d" % l, _fm(inp["lru_b_a"][l], 2))
        c.add("lrubx%d" % l, _fm(inp["lru_b_x"][l], 2))
        c.add("lrulam%d" % l, _fm(inp["lru_lambda"][l], 2))
        dw = np.asarray(inp["dn_conv_w"][l], np.float32)
        c.add("dncw%d" % l, np.stack([dw[j, ch * 128:(ch + 1) * 128] for ch in range(9) for j in range(4)], 1))
        c.add("dnalog%d" % l, np.asarray(inp["dn_a_log"][l], np.float32))
        c.add("dndtb%d" % l, np.asarray(inp["dn_dt_bias"][l], np.float32))
        c.add("dnnw%d" % l, np.tile(np.asarray(inp["dn_norm_w"][l], np.float32), 2))
    c.add("fnw", _fm(inp["final_norm_w"], 8))
    return c


def build_mats(inp):
    out = {}
    lr = np.zeros((2, 32, 192), np.float32)
    for l in range(2):
        lr[l, :16] = inp["gla_w_lr2"][l]
        lr[l, 16] = inp["gla_b_lr2"][l]
    out["wlr2"] = lr
    bd = np.zeros((2, 2, 2, 128, 128), np.float32)
    for l in range(2):
        for t, nm in enumerate(("lru_w_a", "lru_w_x")):
            w = np.asarray(inp[nm][l], np.float32)
            for ch in range(2):
                for q in range(2):
                    n = ch * 2 + q
                    bd[l, t, ch, q * 64:(q + 1) * 64, q * 64:(q + 1) * 64] = w[n]
    out["lrubd"] = bd
    return out


class Builder:
    def __init__(self, S, dbg=False, use=("gla", "lru", "dn"), nlayers=2, sbt=1024):
        self.S = S
        self.dbg = dbg
        self.use = use
        self.nlayers = nlayers
        self.NB = S // TB
        self.SBT = min(sbt, S)
        self.cc = build_consts()
        nc = bass.Bass("TRN2", target_bir_lowering=False)
        self.nc = nc
        P = Prog(nc, same_eng_sync=(os.environ.get('SES', '1') == '1'))
        self.P = P
        self.dbg_outs = []
        self._declare_io()
        self._globals()
        self.pass0()
        for l in range(nlayers):
            self.adaln(l)
            self.passA(l)
            self.passB(l, last=(l == nlayers - 1))
        P.barrier()
        for ev in self.out_events:
            P.wait_event("sp", ev)
        P.emit()

    def _declare_io(self):
        P, S = self.P, self.S
        di = lambda n, s: P.dram(n, s, F32, kind="ExternalInput")
        self.x = di("x", [S, D])
        self.cfm = di("cfm", [128, NK])
        self.w_in = di("w_in", [2, D, D_IN])
        self.w_out = di("w_out", [2, D, D])
        self.ada_w = di("ada_w", [2, D, 6 * D])
        self.ffn_wg = di("ffn_w_gate", [1, D, D_FF])
        self.ffn_wu = di("ffn_w_up", [1, D, D_FF])
        self.ffn_wd = di("ffn_w_down", [1, D_FF, D])
        self.router_w = di("router_w", [1, D, NE])
        self.moe_wg = di("moe_w_gate", [1, NE, D, D_FFE])
        self.moe_wu = di("moe_w_up", [1, NE, D, D_FFE])
        self.moe_wd = di("moe_w_down", [1, NE, D_FFE, D])
        self.cst_d = di("cst", [128, self.cc.n])
        self.ccb = build_consts_b()
        self.cstb_d = di("cstb", [128, self.ccb.n])
        self.pk_n = build_params_ncols()
        self.pk_d = di("pk", [128, self.pk_n])
        self.wlr2_d = di("wlr2", [2, 32, 192])
        self.lrubd_d = di("lrubd", [2, 2, 2, 128, 128])
        self.out = P.dram("out", [S, D], F32, kind="ExternalOutput")
        self.xT = P.dram("xT_scr", [D, S], F32)
        self.out_events = []

    def dbg_dump(self, name):
        if not self.dbg:
            return
        P = self.P
        o = P.dram(name, [D, self.S], F32, kind="ExternalOutput")
        P.push_scope()
        t = P.sb("dbgt", [128, NK, TB], F32)
        for b in range(self.NB):
            P.dma("sp", t[:], self.xT[:, b * TB:(b + 1) * TB].rearrange("(k p) n -> p k n", p=128),
                  r=[(self.xT, b)], w=[t])
            ev = P.dma("sp", o[:, b * TB:(b + 1) * TB].rearrange("(k p) n -> p k n", p=128), t[:],
                       r=[t], w=[o])
            self.out_events.append(ev)
        P.pop_scope()
        self.dbg_outs.append(name)

    def C(self, name, rows=128):
        o, n = self.cc.off[name]
        return self.cst[0:rows, o:o + n]

    def PK(self, name, rows=128, col=None, ncol=None):
        o, n = self.pko[name]
        if col is not None:
            o = o + col
            n = 1 if ncol is None else ncol
        return self.pk[0:rows, o:o + n]

    def _globals(self):
        P = self.P
        self.cst = P.sb("cst_sb", [128, self.cc.n], F32)
        P.dma("sp", self.cst[:], self.cst_d[:], w=[self.cst])
        self.pk = P.sb("pk_sb", [128, self.pk_n], F32)
        P.dma("sp", self.pk[:], self.pk_d[:], w=[self.pk])
        self.pko = build_params_offsets()
        self.ones_bf = P.sb("ones_bf", [128, 128], BF16)
        P.op("dve", lambda e: e.tensor_copy(out=self.ones_bf[:], in_=self.C("ones")), r=[self.cst], w=[self.ones_bf])
        self.blk_bf = P.sb("blk_bf", [128, 128], BF16)
        P.op("dve", lambda e: e.tensor_copy(out=self.blk_bf[:], in_=self.C("blk64")), r=[self.cst], w=[self.blk_bf])
        self.PSB = [P.ps("psb%d" % i, [128, 512], F32) for i in range(8)]
        self.epsc = P.sb("epsc", [128, 1], F32)
        P.op("dve", lambda e: e.memset(self.epsc[:], EPS), w=[self.epsc])
        self.csil = P.sb("csil", [128, NK], F32)
        ct = P.sb("ctmp", [128, NK], F32)
        P.dma("sp", ct[:], self.cfm[:], w=[ct])
        P.op("act", lambda e: e.activation(out=self.csil[:], in_=ct[:], func=AF.Silu), r=[ct], w=[self.csil])
        self.mod = [P.sb("mod%d" % l, [128, 48], F32) for l in range(2)]
        self.gv1 = [P.sb("gv1_%d" % l, [128, NK], F32) for l in range(2)]
        self.gv2 = [P.sb("gv2_%d" % l, [128, NK], F32) for l in range(2)]

    def pass0(self):
        P = self.P
        P.push_scope()
        xin = [P.sb("p0_in%d" % i, [128, D], F32) for i in range(2)]
        xt = [P.sb("p0_xt%d" % i, [128, NK, TB], F32) for i in range(2)]
        n = 0
        for b in range(self.NB):
            xo = xt[b % 2]
            for tt in range(TB // 128):
                xi = xin[n % 2]
                r0 = b * TB + tt * 128
                P.dma("sp", xi[:], self.x[r0:r0 + 128, :], r=[self.x], w=[xi])
                for half in range(2):
                    pb = self.PSB[(n * 2 + half) % 4]
                    for q in range(4):
                        k = half * 4 + q
                        P.op("pe", lambda e, pb=pb, q=q, xi=xi, k=k: e.transpose(
                            out=pb[:, q * 128:(q + 1) * 128], in_=xi[:, k * 128:(k + 1) * 128],
                            identity=self.C("ident")), r=[xi, self.cst], w=[pb])
                    eng = "act" if half == 0 else "dve"
                    if eng == "act":
                        P.op("act", lambda e, pb=pb, xo=xo, half=half, tt=tt: e.activation(
                            out=xo[:, half * 4:half * 4 + 4, tt * 128:(tt + 1) * 128],
                            in_=pb[:].rearrange("p (q n) -> p q n", q=4), func=AF.Copy), r=[pb], w=[xo])
                    else:
                        P.op("dve", lambda e, pb=pb, xo=xo, half=half, tt=tt: e.tensor_copy(
                            out=xo[:, half * 4:half * 4 + 4, tt * 128:(tt + 1) * 128],
                            in_=pb[:].rearrange("p (q n) -> p q n", q=4)), r=[pb], w=[xo])
                n += 1
            P.dma("sp", self.xT[:, b * TB:(b + 1) * TB].rearrange("(k p) n -> p k n", p=128), xo[:],
                  r=[xo], w=[(self.xT, b)])
        P.pop_scope()
        self.dbg_dump("dbg_x0")

    def adaln(self, l):
        P = self.P
        P.push_scope()
        wt = [P.sb("ada_wt%d" % i, [128, NK, 1024], F32) for i in range(2)]
        pb = self.PSB[7]
        for g in range(6):
            t = wt[g % 2]
            P.dma("sp" if g % 2 == 0 else "act", t[:],
                  self.ada_w[l, :, g * 1024:(g + 1) * 1024].rearrange("(k p) n -> p k n", p=128),
                  r=[self.ada_w], w=[t])
            for oc in range(8):
                col = g * 8 + oc
                for k in range(NK):
                    P.op("pe", lambda e, t=t, k=k, oc=oc, col=col: e.matmul(
                        pb[:, col:col + 1], lhsT=t[:, k, oc * 128:(oc + 1) * 128], rhs=self.csil[:, k:k + 1],
                        start=(k == 0), stop=(k == NK - 1)), r=[t, self.csil], w=[pb])
        mod = self.mod[l]
        P.op("dve", lambda e: e.tensor_tensor(out=mod[:], in0=pb[:, 0:48], in1=self.PK("ada_b%d" % l), op=ALU.add),
             r=[pb, self.pk], w=[mod])
        for (gv, nw, c0) in ((self.gv1[l], "n1w%d" % l, 8), (self.gv2[l], "n2w%d" % l, 32)):
            P.op("dve", lambda e, gv=gv, nw=nw, c0=c0: e.scalar_tensor_tensor(
                out=gv[:], in0=mod[:, c0:c0 + 8], scalar=1.0, in1=self.PK(nw), op0=ALU.add, op1=ALU.mult),
                r=[mod, self.pk], w=[gv])
        P.pop_scope()

    def load_norm(self, XT, b, gv, shc, Hap, Hdep, SQ, tmp, rstd, hf_cb=None, ps=None, tb=TB):
        P = self.P
        ps = ps if ps is not None else self.PSB[7]
        P.dma("sp", XT[:], self.xT[:, b * tb:(b + 1) * tb].rearrange("(k p) n -> p k n", p=128),
              r=[(self.xT, b)], w=[XT])
        P.op("act", lambda e: e.activation(out=SQ[:], in_=XT[:], func=AF.Square), r=[XT], w=[SQ])
        for k in range(NK):
            P.op("pe", lambda e, k=k: e.matmul(ps[:, 0:tb], lhsT=self.ones_bf[:], rhs=SQ[:, k, :],
                                               start=(k == 0), stop=(k == NK - 1)),
                 r=[self.ones_bf, SQ], w=[ps])
        P.op("act", lambda e: e.activation(out=rstd[:], in_=ps[:, 0:tb], func=AF.Ln, bias=self.epsc[:, 0:1], scale=1.0 / D),
             r=[ps, self.epsc], w=[rstd])
        P.op("act", lambda e: e.activation(out=rstd[:], in_=rstd[:], func=AF.Exp, scale=-0.5), r=[rstd], w=[rstd])
        for k in range(NK):
            t = tmp[k % len(tmp)]
            P.op("dve", lambda e, k=k, t=t: e.scalar_tensor_tensor(
                out=t[:], in0=XT[:, k, :], scalar=gv[:, k:k + 1], in1=rstd[:], op0=ALU.mult, op1=ALU.mult),
                r=[XT, gv, rstd], w=[t])
            if hf_cb is None:
                P.op("act", lambda e, k=k, t=t: e.activation(
                    out=Hap(k), in_=t[:], func=AF.Identity, bias=shc(k), scale=1.0),
                    r=[t, self.mod[0], self.mod[1]], w=[Hdep])
            else:
                P.op("act", lambda e, k=k, t=t: e.activation(
                    out=t[:], in_=t[:], func=AF.Identity, bias=shc(k), scale=1.0),
                    r=[t, self.mod[0], self.mod[1]], w=[t])
                P.op("dve", lambda e, k=k, t=t: e.tensor_copy(out=Hap(k), in_=t[:]), r=[t], w=[Hdep])
                hf_cb(k, t)

    def passA(self, l):
        P = self.P
        P.push_scope()
        self.A_l = l
        self.PSA = [BufV(b_, TBA) for b_ in self.PSB]
        WIN = P.sb("WIN", [128, NK, D_IN], BF16)
        self.WIN = WIN
        WOUT = P.sb("WOUT", [128, NK, D], BF16)
        for k in range(NK):
            P.dma("pool", WOUT[:, k, :], self.w_out[l, k * 128:(k + 1) * 128, :], r=[self.w_out], w=[(WOUT, k)])
        for k in range(NK):
            P.dma("pool", WIN[:, k, :], self.w_in[l, k * 128:(k + 1) * 128, :], r=[self.w_in], w=[(WIN, k)],
                  max_dma_last_dim=4096)
        H = P.sb("A_H", [128, NK, TBA], BF16)
        MT = P.sb("A_MT", [128, NK, TBA], BF16)
        self.H, self.MT = H, MT
        self.tpool = [P.sb("A_t%d" % i, [128, TBA], F32) for i in range(self.NTP)]
        self.tfree = list(self.tpool)
        self.mixer_setup(l)
        mod = self.mod[l]
        for b in range(self.S // TBA):
            P.push_scope()
            XT = P.sb("A_XT", [128, NK, TBA], F32)
            SQ = P.sb("A_SQ", [128, NK, TBA], BF16)
            rstd = P.sb("A_rstd", [128, TBA], F32)
            tmp = [P.sb("A_tmp%d" % i, [128, TBA], F32) for i in range(2)]
            self.load_norm(XT, b, self.gv1[l], lambda k: mod[:, 0 + k:0 + k + 1], lambda k: H[:, k, :], H, SQ, tmp, rstd, tb=TBA)
            P.pop_scope(barrier=False)
            if "lru" in self.use:
                self.lru_block(l, b)
            else:
                self.zero_mt((3, 4))
            if "gla" in self.use:
                self.gla_block(l, b)
            else:
                self.zero_mt((0, 1, 2))
            if "dn" in self.use:
                self.dn_block(l, b)
            else:
                self.zero_mt((5, 6, 7))
            P.push_scope()
            XT = P.sb("A_XT2", [128, NK, TBA], F32)
            P.dma("sp", XT[:], self.xT[:, b * TBA:(b + 1) * TBA].rearrange("(k p) n -> p k n", p=128),
                  r=[(self.xT, b)], w=[XT])
            for dc in range(NK):
                pb = self.PSA[dc % 2]
                for j in range(NK):
                    P.op("pe", lambda e, pb=pb, j=j, dc=dc: e.matmul(
                        pb[:], lhsT=WOUT[:, j, dc * 128:(dc + 1) * 128], rhs=MT[:, j, :],
                        start=(j == 0), stop=(j == NK - 1)),
                        r=[(WOUT, j), (MT, j)], w=[pb])
                P.op("dve", lambda e, pb=pb, dc=dc: e.scalar_tensor_tensor(
                    out=XT[:, dc, :], in0=pb[:], scalar=mod[:, 16 + dc:16 + dc + 1], in1=XT[:, dc, :],
                    op0=ALU.mult, op1=ALU.add), r=[pb, mod, XT], w=[XT])
            P.dma("sp", self.xT[:, b * TBA:(b + 1) * TBA].rearrange("(k p) n -> p k n", p=128), XT[:],
                  r=[XT], w=[(self.xT, b)])
            P.pop_scope(barrier=False)
        P.pop_scope()
        self.dbg_dump("dbg_xA%d" % l)

    def zero_mt(self, js):
        P = self.P
        for j in js:
            P.op("pool", lambda e, j=j: e.memset(self.MT[:, j, :], 0.0), w=[(self.MT, j)])

    NTP = 6

    def tget(self):
        return self.tfree.pop()

    def tput(self, *ts):
        for t in ts:
            self.tfree.append(t)

    def proj_fm(self, pb, col0, ncols, prow0=0):
        P = self.P
        for k in range(NK):
            P.op("pe", lambda e, k=k: e.matmul(
                pb[prow0:prow0 + ncols, :], lhsT=self.WIN[:, k, col0:col0 + ncols], rhs=self.H[:, k, :],
                start=(k == 0), stop=(k == NK - 1)), r=[(self.WIN, k), self.H], w=[pb])

    def mixer_setup(self, l):
        P = self.P
        self.lru_bd = P.sb("lru_bd", [128, 2, 2, 128], F32)
        for t in range(2):
            for ch in range(2):
                P.dma("sp", self.lru_bd[:, t, ch, :], self.lrubd_d[l, t, ch], r=[self.lrubd_d], w=[self.lru_bd])
        self.lru_c1 = P.sb("lru_c1", [128, 2], F32)
        self.lru_c2 = P.sb("lru_c2", [128, 2], F32)
        ta = P.sb("lru_ta", [128, 2], F32)
        tb = P.sb("lru_tb", [128, 2], F32)
        lam = self.PK("lrulam%d" % l)
        P.op("act", lambda e: e.activation(out=ta[:], in_=lam, func=AF.Abs), r=[self.pk], w=[ta])
        P.op("act", lambda e: e.activation(out=ta[:], in_=ta[:], func=AF.Exp, scale=-1.0), r=[ta], w=[ta])
        P.op("act", lambda e: e.activation(out=ta[:], in_=ta[:], func=AF.Ln, bias=1.0, scale=1.0), r=[ta], w=[ta])
        P.op("dve", lambda e: e.tensor_scalar(out=tb[:], in0=lam, scalar1=-1.0, scalar2=0.0,
                                              op0=ALU.mult, op1=ALU.max), r=[self.pk], w=[tb])
        P.op("dve", lambda e: e.tensor_tensor(out=ta[:], in0=ta[:], in1=tb[:], op=ALU.add), r=[ta, tb], w=[ta])
        P.op("dve", lambda e: e.tensor_scalar(out=self.lru_c1[:], in0=ta[:], scalar1=-8.0, scalar2=None,
                                              op0=ALU.mult), r=[ta], w=[self.lru_c1])
        P.op("dve", lambda e: e.tensor_scalar(out=self.lru_c2[:], in0=ta[:], scalar1=-16.0, scalar2=None,
                                              op0=ALU.mult), r=[ta], w=[self.lru_c2])
        self.lru_x = [P.sb("lru_x%d" % ch, [128, 3 + TBA], F32) for ch in range(2)]
        self.lru_h = P.sb("lru_h", [128, 2], F32)
        for ch in range(2):
            P.op("pool", lambda e, ch=ch: e.memset(self.lru_x[ch][:], 0.0), w=[self.lru_x[ch]])
        P.op("pool", lambda e: e.memset(self.lru_h[:], 0.0), w=[self.lru_h])
        if "gla" in self.use:
            self.gla_setup(l)
        if "dn" in self.use:
            self.dn_setup(l)

    def conv4(self, out, xin, wname, wcol0, bias_ap=None, rdeps=(), wdeps=()):
        P = self.P
        w = lambda j: self.PK(wname, col=wcol0 + j)
        if bias_ap is not None:
            P.op("dve", lambda e: e.tensor_scalar(out=out, in0=xin[:, 0:TBA], scalar1=w(0), scalar2=bias_ap,
                                                  op0=ALU.mult, op1=ALU.add), r=list(rdeps) + [self.pk], w=list(wdeps))
        else:
            P.op("dve", lambda e: e.tensor_scalar(out=out, in0=xin[:, 0:TBA], scalar1=w(0), scalar2=None,
                                                  op0=ALU.mult), r=list(rdeps) + [self.pk], w=list(wdeps))
        for j in range(1, 4):
            P.op("dve", lambda e, j=j: e.scalar_tensor_tensor(out=out, in0=xin[:, j:j + TBA], scalar=w(j), in1=out,
                                                              op0=ALU.mult, op1=ALU.add),
                 r=list(rdeps) + [self.pk] + list(wdeps), w=list(wdeps))

    def lru_block(self, l, b):
        P = self.P
        PS = self.PSA
        P.push_scope()
        CH2 = range(2)
        T = {nm: [P.sb("lru_%s%d" % (nm, ch), [128, TBA], F32) for ch in CH2] for nm in ("xr", "rg", "ig", "a", "a2", "g", "t2", "hs")}
        X = self.lru_x
        for ch in CH2:
            if b > 0:
                P.op("dve", lambda e, ch=ch: e.tensor_copy(out=X[ch][:, 0:3], in_=X[ch][:, TBA:TBA + 3]), r=[X[ch]], w=[X[ch]])
            self.proj_fm(PS[ch], O_LX + ch * 128, 128)
            P.op("act", lambda e, ch=ch: e.activation(out=X[ch][:, 3:3 + TBA], in_=PS[ch][:], func=AF.Copy), r=[PS[ch]], w=[X[ch]])
            self.proj_fm(PS[6 + ch], O_LG + ch * 128, 128)
            P.op("act", lambda e, ch=ch: e.activation(out=T["g"][ch][:], in_=PS[6 + ch][:], func=AF.Copy), r=[PS[6 + ch]], w=[T["g"][ch]])
        for ch in CH2:
            xr = T["xr"][ch]
            self.conv4(xr[:], X[ch], "lrucw%d" % l, ch * 4, bias_ap=self.PK("lrucb%d" % l, col=ch), rdeps=[X[ch]], wdeps=[xr])
        for ch in CH2:
            g, t2 = T["g"][ch], T["t2"][ch]
            P.op("dve", lambda e, g=g, t2=t2: e.tensor_tensor(out=t2[:], in0=g[:], in1=g[:], op=ALU.mult), r=[g], w=[t2])
        for ch in CH2:
            t2 = T["t2"][ch]
            P.op("dve", lambda e, t2=t2: e.tensor_scalar(out=t2[:], in0=t2[:], scalar1=0.044715, scalar2=1.0,
                                                         op0=ALU.mult, op1=ALU.add), r=[t2], w=[t2])
        for ch in CH2:
            g, t2 = T["g"][ch], T["t2"][ch]
            P.op("dve", lambda e, g=g, t2=t2: e.tensor_tensor(out=t2[:], in0=t2[:], in1=g[:], op=ALU.mult), r=[t2, g], w=[t2])
        for ch in CH2:
            xr = T["xr"][ch]
            P.op("pe", lambda e, ch=ch, xr=xr: e.matmul(PS[2 + ch][:], lhsT=self.lru_bd[:, 0, ch, :], rhs=xr[:],
                                                        start=True, stop=True), r=[self.lru_bd, xr], w=[PS[2 + ch]])
            P.op("pe", lambda e, ch=ch, xr=xr: e.matmul(PS[4 + ch][:], lhsT=self.lru_bd[:, 1, ch, :], rhs=xr[:],
                                                        start=True, stop=True), r=[self.lru_bd, xr], w=[PS[4 + ch]])
        for ch in CH2:
            t2 = T["t2"][ch]
            P.op("act", lambda e, t2=t2: e.activation(out=t2[:], in_=t2[:], func=AF.Sigmoid, scale=1.5957691216), r=[t2], w=[t2])
        for ch in CH2:
            rg, ig = T["rg"][ch], T["ig"][ch]
            P.op("act", lambda e, rg=rg, ch=ch: e.activation(
                out=rg[:], in_=PS[2 + ch][:], func=AF.Sigmoid, bias=self.PK("lruba%d" % l, col=ch), scale=1.0),
                r=[PS[2 + ch], self.pk], w=[rg])
            P.op("act", lambda e, ig=ig, ch=ch: e.activation(
                out=ig[:], in_=PS[4 + ch][:], func=AF.Sigmoid, bias=self.PK("lrubx%d" % l, col=ch), scale=1.0),
                r=[PS[4 + ch], self.pk], w=[ig])
        for ch in CH2:
            g, t2 = T["g"][ch], T["t2"][ch]
            P.op("dve", lambda e, g=g, t2=t2: e.tensor_tensor(out=t2[:], in0=t2[:], in1=g[:], op=ALU.mult), r=[t2, g], w=[t2])
        for ch in CH2:
            rg, a, a2 = T["rg"][ch], T["a"][ch], T["a2"][ch]
            P.op("act", lambda e, a=a, rg=rg, ch=ch: e.activation(
                out=a[:], in_=rg[:], func=AF.Exp, scale=self.lru_c1[:, ch:ch + 1]), r=[rg, self.lru_c1], w=[a])
            P.op("act", lambda e, a2=a2, rg=rg, ch=ch: e.activation(
                out=a2[:], in_=rg[:], func=AF.Exp, scale=self.lru_c2[:, ch:ch + 1]), r=[rg, self.lru_c2], w=[a2])
        for ch in CH2:
            a2 = T["a2"][ch]
            P.op("dve", lambda e, a2=a2: e.tensor_scalar(out=a2[:], in0=a2[:], scalar1=-1.0, scalar2=1.0,
                                                         op0=ALU.mult, op1=ALU.add), r=[a2], w=[a2])
        for ch in CH2:
            a2 = T["a2"][ch]
            P.op("dve", lambda e, a2=a2: e.tensor_scalar(out=a2[:], in0=a2[:], scalar1=1e-30, scalar2=None,
                                                         op0=ALU.max), r=[a2], w=[a2])
        for ch in CH2:
            a2 = T["a2"][ch]
            P.op("act", lambda e, a2=a2: e.activation(out=a2[:], in_=a2[:], func=AF.Ln), r=[a2], w=[a2])
        for ch in CH2:
            a2 = T["a2"][ch]
            P.op("act", lambda e, a2=a2: e.activation(out=a2[:], in_=a2[:], func=AF.Exp, scale=0.5), r=[a2], w=[a2])
        for ch in CH2:
            ig, xr = T["ig"][ch], T["xr"][ch]
            P.op("dve", lambda e, ig=ig, xr=xr: e.tensor_tensor(out=ig[:], in0=ig[:], in1=xr[:], op=ALU.mult), r=[ig, xr], w=[ig])
        for ch in CH2:
            ig, a2 = T["ig"][ch], T["a2"][ch]
            P.op("dve", lambda e, ig=ig, a2=a2: e.tensor_tensor(out=ig[:], in0=ig[:], in1=a2[:], op=ALU.mult), r=[ig, a2], w=[ig])
        for ch in CH2:
            hs, a, ig = T["hs"][ch], T["a"][ch], T["ig"][ch]
            P.op("dve", lambda e, hs=hs, a=a, ig=ig, ch=ch: e.tensor_tensor_scan(
                out=hs[:], data0=a[:], data1=ig[:], initial=self.lru_h[:, ch:ch + 1], op0=ALU.mult, op1=ALU.add),
                r=[a, ig, self.lru_h], w=[hs])
        for ch in CH2:
            hs = T["hs"][ch]
            P.op("dve", lambda e, hs=hs, ch=ch: e.tensor_copy(out=self.lru_h[:, ch:ch + 1], in_=hs[:, TBA - 1:TBA]),
                 r=[hs], w=[self.lru_h])
        for ch in CH2:
            hs, t2 = T["hs"][ch], T["t2"][ch]
            P.op("dve", lambda e, t2=t2, hs=hs, ch=ch: e.tensor_tensor(out=self.MT[:, 3 + ch, :], in0=t2[:], in1=hs[:],
                                                                       op=ALU.mult), r=[t2, hs], w=[(self.MT, 3 + ch)])
        P.pop_scope(barrier=False)

    def gla_setup(self, l):
        P = self.P
        W6 = 6 * TBA
        self.WLR = P.sb("gla_wlr", [32, 192], F32)
        P.dma("sp", self.WLR[:], self.wlr2_d[l], r=[self.wlr2_d], w=[self.WLR])
        self.gS = P.sb("gla_S", [64, 384], F32)
        P.op("pool", lambda e: e.memset(self.gS[:], 0.0), w=[self.gS])

    def gla_block(self, l, b):
        P = self.P
        PS = self.PSA
        NEG = -1.0 / 16.0
        W6 = 6 * TBA
        P.push_scope()
        self.GL = P.sb("gla_gl", [32, TBA], F32)
        P.op("pool", lambda e: e.memset(self.GL[:], 1.0), w=[self.GL])
        GQ = P.sb("gla_q", [64, W6], F32)
        GK = P.sb("gla_k", [64, W6], F32)
        GEB = P.sb("gla_eb", [32, W6], F32)
        GL1 = P.sb("gla_l1", [32, W6], F32)
        GL2 = P.sb("gla_l2", [32, W6], F32)
        gS = self.gS
        for t in (GQ, GK):
            P.op("pool", lambda e, t=t: e.memset(t[:], 0.0), w=[t])
        self.gTM = [P.sb("gla_tm%d" % t, [64, 1152], F32) for t in range(TBA // CH)]
        self.proj_fm(PS[0], O_GLR, 16)
        P.op("act", lambda e: e.activation(out=self.GL[0:16, :], in_=PS[0][0:16, :], func=AF.Copy), r=[PS[0]], w=[self.GL])
        for h in range(6):
            pz = PS[1 + h % 2]
            P.op("pe", lambda e, h=h, pz=pz: e.matmul(pz[0:32, :], lhsT=self.WLR[0:17, h * 32:(h + 1) * 32],
                                                      rhs=self.GL[0:17, :], start=True, stop=True),
                 r=[self.WLR, self.GL], w=[pz])
            P.op("dve", lambda e, h=h, pz=pz: e.tensor_scalar(out=GL1[:, h * TBA:(h + 1) * TBA], in0=pz[0:32, :], scalar1=-80.0,
                                                              scalar2=None, op0=ALU.max), r=[pz], w=[GL1])
        P.op("act", lambda e: e.activation(out=GL1[:], in_=GL1[:], func=AF.Exp, scale=-1.0), r=[GL1], w=[GL1])
        P.op("act", lambda e: e.activation(out=GL1[:], in_=GL1[:], func=AF.Ln, bias=1.0, scale=1.0), r=[GL1], w=[GL1])
        for i in range(3):
            cs = slice(i * 2 * TBA, (i + 1) * 2 * TBA)
            P.op("dve", lambda e, cs=cs: e.tensor_tensor_scan(
                out=GL2[:, cs], data0=self.C("rst", rows=32)[:, 0:2 * TBA], data1=GL1[:, cs], initial=0.0,
                op0=ALU.mult, op1=ALU.add), r=[GL1, self.cst], w=[GL2])
        P.op("act", lambda e: e.activation(out=GEB[:], in_=GL2[:], func=AF.Exp, scale=NEG), r=[GL2], w=[GEB])
        P.op("act", lambda e: e.activation(out=GL1[:], in_=GL2[:], func=AF.Exp, scale=-NEG), r=[GL2], w=[GL1])
        for h in range(6):
            hs = slice(h * TBA, (h + 1) * TBA)
            pq = PS[1 + h % 2]
            self.proj_fm(pq, O_GQ + h * 32, 32)
            P.op("dve", lambda e, hs=hs, pq=pq: e.scalar_tensor_tensor(
                out=GQ[0:32, hs], in0=pq[0:32, :], scalar=0.17677669529663687, in1=GEB[:, hs], op0=ALU.mult, op1=ALU.mult),
                r=[pq, GEB], w=[GQ])
            pk_ = PS[3 + h % 2]
            self.proj_fm(pk_, O_GK + h * 32, 32)
            P.op("dve", lambda e, hs=hs, pk_=pk_: e.tensor_tensor(
                out=GK[0:32, hs], in0=pk_[0:32, :], in1=GL1[:, hs], op=ALU.mult), r=[pk_, GL1], w=[GK])
        NCH = TBA // CH
        BSETS = [(PS[0], PS[1], PS[2]), (PS[5], PS[6], PS[7])]

        def tparts(n):
            tm = self.gTM[n]
            return tm, tm[:, 0:192], tm[:, 192:384], tm[:, 384:768]

        def t_s1(n, bs):
            tm, LT, KE, VT = tparts(n)
            ctok = slice(n * 64, (n + 1) * 64)
            pz = bs[0]
            P.op("pe", lambda e: e.matmul(pz[0:64, 0:192], lhsT=self.GL[0:17, ctok], rhs=self.WLR[0:17, :],
                                          start=True, stop=True), r=[self.GL, self.WLR], w=[pz])
            P.op("dve", lambda e: e.tensor_scalar(out=LT, in0=pz[0:64, 0:192], scalar1=-80.0, scalar2=None,
                                                  op0=ALU.max), r=[pz], w=[(tm, "L")])

        def t_s2(n, bs):
            tm, LT, KE, VT = tparts(n)
            P.op("act", lambda e: e.activation(out=LT, in_=LT, func=AF.Exp, scale=-1.0), r=[(tm, "L")], w=[(tm, "L")])
            P.op("act", lambda e: e.activation(out=LT, in_=LT, func=AF.Ln, bias=1.0, scale=1.0),
                 r=[(tm, "L")], w=[(tm, "L")])

        def t_s3(n, bs):
            tm, LT, KE, VT = tparts(n)
            pz = bs[0]
            P.op("pe", lambda e: e.matmul(pz[0:64, 192:384], lhsT=self.C("su2", rows=64)[:, 0:64], rhs=LT,
                                          start=True, stop=True), r=[self.cst, (tm, "L")], w=[pz])
            P.op("act", lambda e: e.activation(out=KE, in_=pz[0:64, 192:384], func=AF.Exp, scale=NEG),
                 r=[pz], w=[(tm, "KE")])

        def t_s4(n, bs):
            tok = slice(n * 64, (n + 1) * 64)
            pk_, pv = bs[1], bs[2]
            for kc in range(NK):
                P.op("pe", lambda e, kc=kc: e.matmul(
                    pk_[0:64, 0:192], lhsT=self.H[:, kc, tok], rhs=self.WIN[:, kc, O_GK:O_GK + 192],
                    start=(kc == 0), stop=(kc == NK - 1)), r=[self.H, (self.WIN, kc)], w=[pk_])
            for kc in range(NK):
                P.op("pe", lambda e, kc=kc: e.matmul(
                    pv[0:64, 0:384], lhsT=self.H[:, kc, tok], rhs=self.WIN[:, kc, O_GV:O_GV + 384],
                    start=(kc == 0), stop=(kc == NK - 1)), r=[self.H, (self.WIN, kc)], w=[pv])

        def t_s5(n, bs):
            tm, LT, KE, VT = tparts(n)
            pk_, pv = bs[1], bs[2]
            P.op("dve", lambda e: e.tensor_tensor(out=KE, in0=pk_[0:64, 0:192], in1=KE, op=ALU.mult),
                 r=[pk_, (tm, "KE")], w=[(tm, "KE")])
            P.op("act", lambda e: e.activation(out=VT, in_=pv[0:64, 0:384], func=AF.Copy), r=[pv], w=[(tm, "V")])

        def t_s6(n, bs):
            tm, LT, KE, VT = tparts(n)
            psc = bs[0]
            for h in range(6):
                hc = slice(h * TBA + n * 64, h * TBA + n * 64 + 64)
                P.op("pe", lambda e, h=h, hc=hc: e.matmul(
                    psc[0:64, h * 64:(h + 1) * 64], lhsT=GK[:, hc], rhs=GQ[:, hc], start=True, stop=True),
                    r=[GK, GQ], w=[psc])
            P.op("dve", lambda e: e.tensor_tensor(out=tm[:, 768:1152], in0=psc[0:64, 0:384],
                                                  in1=self.C("ut6", rows=64), op=ALU.mult),
                 r=[psc, self.cst], w=[(tm, "SC")])

        for g0 in range(0, NCH, 2):
            for st in (t_s1, t_s2, t_s3, t_s4, t_s5, t_s6):
                for p in range(2):
                    st(g0 + p, BSETS[p])
        OT = [PS[0], PS[1], PS[2]]
        pkv = PS[3]
        for n in range(NCH):
            tm = self.gTM[n]
            ctok = slice(n * 64, (n + 1) * 64)
            for h in range(6):
                jj, hp = h // 2, h % 2
                hc = slice(h * TBA + n * 64, h * TBA + n * 64 + 64)
                oap = OT[jj][hp * 64:(hp + 1) * 64, ctok]
                P.op("pe", lambda e, oap=oap, tm=tm, h=h: e.matmul(
                    oap, lhsT=tm[:, 384 + h * 64:384 + (h + 1) * 64], rhs=tm[:, 768 + h * 64:768 + (h + 1) * 64],
                    start=True, stop=False), r=[(tm, "V"), (tm, "SC")], w=[OT[jj]])
                P.op("pe", lambda e, oap=oap, h=h, hc=hc: e.matmul(
                    oap, lhsT=gS[:, h * 64:(h + 1) * 64], rhs=GQ[:, hc], start=False, stop=True),
                    r=[gS, GQ], w=[OT[jj]])
            for h in range(6):
                P.op("pe", lambda e, tm=tm, h=h: e.matmul(
                    pkv[0:32, h * 64:(h + 1) * 64], lhsT=tm[:, 192 + h * 32:192 + (h + 1) * 32],
                    rhs=tm[:, 384 + h * 64:384 + (h + 1) * 64], start=True, stop=True),
                    r=[(tm, "KE"), (tm, "V")], w=[pkv])
            col = n * 64 + 63
            decB = GEB[:].rearrange("p (h t) -> p h t", h=6)[:, :, col:col + 1].broadcast_to([32, 6, 64])
            S3 = gS[0:32, :].rearrange("p (h v) -> p h v", h=6)
            P.op("dve", lambda e, S3=S3, decB=decB: e.tensor_tensor(out=S3, in0=S3, in1=decB, op=ALU.mult),
                 r=[gS, GEB], w=[gS])
            P.op("dve", lambda e: e.tensor_tensor(out=gS[0:32, :], in0=gS[0:32, :], in1=pkv[0:32, 0:384], op=ALU.add),
                 r=[gS, pkv], w=[gS])
        NPS = [PS[3], PS[4], PS[5]]
        NT = [(self.tget(), self.tget()) for _ in range(3)]
        for jj in range(3):
            osb, sq = NT[jj]
            P.op("act", lambda e, osb=osb, jj=jj: e.activation(out=osb[:], in_=OT[jj][:], func=AF.Copy), r=[OT[jj]], w=[osb])
            P.op("act", lambda e, sq=sq, jj=jj: e.activation(out=sq[:], in_=OT[jj][:], func=AF.Square), r=[OT[jj]], w=[sq])
        for jj in range(3):
            osb, sq = NT[jj]
            pss = NPS[jj]
            P.op("pe", lambda e, sq=sq, pss=pss: e.matmul(pss[:], lhsT=self.C("blk64"), rhs=sq[:], start=True, stop=True),
                 r=[self.cst, sq], w=[pss])
        for jj in range(3):
            osb, sq = NT[jj]
            pss = NPS[jj]
            P.op("act", lambda e, sq=sq, pss=pss: e.activation(out=sq[:], in_=pss[:], func=AF.Ln, bias=self.epsc[:, 0:1],
                                                               scale=1.0 / 64), r=[pss, self.epsc], w=[sq])
        for jj in range(3):
            osb, sq = NT[jj]
            P.op("act", lambda e, sq=sq: e.activation(out=sq[:], in_=sq[:], func=AF.Exp, scale=-0.5), r=[sq], w=[sq])
        for jj in range(3):
            osb, sq = NT[jj]
            P.op("dve", lambda e, osb=osb, sq=sq: e.scalar_tensor_tensor(
                out=osb[:], in0=osb[:], scalar=self.PK("glanw%d" % l), in1=sq[:], op0=ALU.mult, op1=ALU.mult),
                r=[osb, sq, self.pk], w=[osb])
        for jj in range(3):
            osb, sq = NT[jj]
            pgz = NPS[jj]
            self.proj_fm(pgz, O_GZ + jj * 128, 128)
            P.op("act", lambda e, sq=sq, pgz=pgz: e.activation(out=sq[:], in_=pgz[:], func=AF.Silu), r=[pgz], w=[sq])
        for jj in range(3):
            osb, sq = NT[jj]
            P.op("dve", lambda e, osb=osb, sq=sq, jj=jj: e.tensor_tensor(out=self.MT[:, 0 + jj, :], in0=osb[:], in1=sq[:],
                                                                         op=ALU.mult), r=[osb, sq], w=[(self.MT, 0 + jj)])
        for a_, b_ in NT:
            self.tput(a_, b_)
        P.pop_scope(barrier=False)

    def dn_setup(self, l):
        P = self.P
        self.dS = P.sb("dn_S", [64, 384], F32)
        P.op("pool", lambda e: e.memset(self.dS[:], 0.0), w=[self.dS])
        self.dHist = P.sb("dn_hist", [128, 9, 3], F32)
        P.op("pool", lambda e: e.memset(self.dHist[:], 0.0), w=[self.dHist])
        self.dnA = P.sb("dn_nA", [6, 1], F32)
        P.op("act", lambda e: e.activation(out=self.dnA[:], in_=self.PK("dnalog%d" % l, rows=6), func=AF.Exp),
             r=[self.pk], w=[self.dnA])
        P.op("dve", lambda e: e.tensor_scalar(out=self.dnA[:], in0=self.dnA[:], scalar1=-1.0, scalar2=None, op0=ALU.mult),
             r=[self.dnA], w=[self.dnA])

    def dn_block(self, l, b):
        P = self.P
        PS = self.PSA
        NCH = TBA // CH
        dS, dHist = self.dS, self.dHist
        P.push_scope()
        C = [P.sb("dn_c%d" % j, [128, TBA], F32) for j in range(9)]
        P.push_scope()
        DX = P.sb("dn_x", [128, 9, 3 + TBA], F32)
        P.op("dve", lambda e: e.tensor_copy(out=DX[:, :, 0:3], in_=dHist[:]), r=[dHist], w=[DX])
        for j in range(9):
            pb = PS[3 + j % 4]
            self.proj_fm(pb, O_DQ + j * 128, 128)
            P.op("act", lambda e, j=j, pb=pb: e.activation(out=DX[:, j, 3:3 + TBA], in_=pb[:], func=AF.Copy), r=[pb], w=[DX])
        P.op("dve", lambda e: e.tensor_copy(out=dHist[:], in_=DX[:, :, TBA:TBA + 3]), r=[DX], w=[dHist])
        wv = lambda j, t: self.PK("dncw%d" % l, col=j * 4 + t)
        for j in range(9):
            P.op("dve", lambda e, j=j: e.tensor_scalar(out=C[j][:], in0=DX[:, j, 0:TBA], scalar1=wv(j, 0), scalar2=None,
                                                       op0=ALU.mult), r=[DX, self.pk], w=[C[j]])
        for t in range(1, 4):
            for j in range(9):
                P.op("dve", lambda e, j=j, t=t: e.scalar_tensor_tensor(out=C[j][:], in0=DX[:, j, t:t + TBA], scalar=wv(j, t),
                                                                       in1=C[j][:], op0=ALU.mult, op1=ALU.add),
                     r=[DX, self.pk, C[j]], w=[C[j]])
        for j in range(9):
            P.op("act", lambda e, j=j: e.activation(out=C[j][:], in_=C[j][:], func=AF.Silu), r=[C[j]], w=[C[j]])
        P.pop_scope(barrier=False)
        P.push_scope()
        SQs = [P.sb("dn_sq%d" % j, [128, TBA], F32) for j in range(6)]
        pss = [PS[j] for j in range(6)]
        for j in range(6):
            P.op("act", lambda e, j=j: e.activation(out=SQs[j][:], in_=C[j][:], func=AF.Square), r=[C[j]], w=[SQs[j]])
        for j in range(6):
            P.op("pe", lambda e, j=j: e.matmul(pss[j][:], lhsT=self.C("blk64"), rhs=SQs[j][:], start=True, stop=True),
                 r=[self.cst, SQs[j]], w=[pss[j]])
        for j in range(6):
            P.op("act", lambda e, j=j: e.activation(out=SQs[j][:], in_=pss[j][:], func=AF.Ln, bias=self.epsc[:, 0:1], scale=1.0),
                 r=[pss[j], self.epsc], w=[SQs[j]])
        for j in range(6):
            P.op("act", lambda e, j=j: e.activation(out=SQs[j][:], in_=SQs[j][:], func=AF.Exp, scale=-0.5), r=[SQs[j]], w=[SQs[j]])
        for j in range(6):
            P.op("dve", lambda e, j=j: e.scalar_tensor_tensor(
                out=C[j][:], in0=C[j][:], scalar=(0.125 if j < 3 else 1.0), in1=SQs[j][:], op0=ALU.mult, op1=ALU.mult),
                r=[C[j], SQs[j]], w=[C[j]])
        P.pop_scope(barrier=False)
        QN, KN = C[0:3], C[3:6]
        BT = P.sb("dn_bt", [6, TBA], F32)
        GC = P.sb("dn_gc", [6, TBA], F32)
        EG = P.sb("dn_eg", [6, TBA], F32)
        BG = P.sb("dn_bg", [6, TBA], F32)
        ER = P.sb("dn_er", [6, TBA], F32)
        T1 = P.sb("dn_t1", [6, TBA], F32)
        T2 = P.sb("dn_t2", [6, TBA], F32)
        NG = P.sb("dn_ng", [6, 6, TBA], F32)
        pb = PS[3]
        self.proj_fm(pb, O_DB, 6)
        P.op("act", lambda e: e.activation(out=BT[:], in_=pb[0:6, :], func=AF.Sigmoid), r=[pb], w=[BT])
        pa = PS[4]
        self.proj_fm(pa, O_DA, 6)
        P.op("act", lambda e: e.activation(out=T2[:], in_=pa[0:6, :], func=AF.Identity,
                                           bias=self.PK("dndtb%d" % l, rows=6), scale=1.0), r=[pa, self.pk], w=[T2])
        P.op("act", lambda e: e.activation(out=T1[:], in_=T2[:], func=AF.Abs), r=[T2], w=[T1])
        P.op("act", lambda e: e.activation(out=T1[:], in_=T1[:], func=AF.Exp, scale=-1.0), r=[T1], w=[T1])
        P.op("act", lambda e: e.activation(out=T1[:], in_=T1[:], func=AF.Ln, bias=1.0, scale=1.0), r=[T1], w=[T1])
        P.op("dve", lambda e: e.tensor_scalar(out=T2[:], in0=T2[:], scalar1=0.0, scalar2=None, op0=ALU.max), r=[T2], w=[T2])
        P.op("dve", lambda e: e.tensor_tensor(out=T1[:], in0=T1[:], in1=T2[:], op=ALU.add), r=[T1, T2], w=[T1])
        P.op("dve", lambda e: e.tensor_scalar(out=T2[:], in0=T1[:], scalar1=self.dnA[:, 0:1], scalar2=None, op0=ALU.mult),
             r=[T1, self.dnA], w=[T2])
        P.op("dve", lambda e: e.tensor_tensor_scan(out=GC[:], data0=self.C("rst", rows=6)[:, 0:TBA], data1=T2[:], initial=0.0,
                                                   op0=ALU.mult, op1=ALU.add), r=[T2, self.cst], w=[GC])
        P.op("act", lambda e: e.activation(out=EG[:], in_=GC[:], func=AF.Exp), r=[GC], w=[EG])
        P.op("dve", lambda e: e.tensor_tensor(out=BG[:], in0=BT[:], in1=EG[:], op=ALU.mult), r=[BT, EG], w=[BG])
        for n in range(NCH):
            col = n * 64 + 63
            P.op("dve", lambda e, n=n, col=col: e.tensor_scalar(
                out=ER[:, n * 64:(n + 1) * 64], in0=GC[:, n * 64:(n + 1) * 64], scalar1=-1.0, scalar2=GC[:, col:col + 1],
                op0=ALU.mult, op1=ALU.add), r=[GC], w=[ER])
        P.op("act", lambda e: e.activation(out=ER[:], in_=ER[:], func=AF.Exp), r=[ER], w=[ER])
        for h in range(6):
            P.op("dve", lambda e, h=h: e.tensor_scalar(out=NG[:, h, :], in0=GC[:], scalar1=self.C("noh6", rows=6)[:, h:h + 1],
                                                       scalar2=None, op0=ALU.mult), r=[GC, self.cst], w=[NG])
        EGd = P.sb("dn_egd", [6, NCH * 6], F32)
        DEC = P.sb("dn_dec", [64, NCH * 6], F32)
        for n in range(NCH):
            col = n * 64 + 63
            P.op("dve", lambda e, n=n, col=col: e.tensor_scalar(
                out=EGd[:, n * 6:(n + 1) * 6], in0=self.C("oh6", rows=6), scalar1=EG[:, col:col + 1], scalar2=None,
                op0=ALU.mult), r=[EG, self.cst], w=[EGd])
        pdx = PS[5]
        P.op("pe", lambda e: e.matmul(pdx[0:64, 0:NCH * 6], lhsT=self.C("ones", rows=6)[:, 0:64], rhs=EGd[:],
                                      start=True, stop=True), r=[self.cst, EGd], w=[pdx])
        P.op("act", lambda e: e.activation(out=DEC[:], in_=pdx[0:64, 0:NCH * 6], func=AF.Copy), r=[pdx], w=[DEC])
        KB = [P.sb("dn_kb%d" % j, [128, TBA], F32) for j in range(3)]
        KG = [P.sb("dn_kg%d" % j, [128, TBA], F32) for j in range(3)]
        QG = [P.sb("dn_qg%d" % j, [128, TBA], F32) for j in range(3)]
        KR = [P.sb("dn_kr%d" % j, [128, TBA], F32) for j in range(3)]
        VB = C[6:9]
        nb = [0]

        def bprod(fld, dst, src, j):
            pbx = PS[3 + nb[0] % 4]
            nb[0] += 1
            P.op("pe", lambda e: e.matmul(pbx[:], lhsT=self.C("e6_%d" % j, rows=6), rhs=fld[:], start=True, stop=True),
                 r=[self.cst, fld], w=[pbx])
            P.op("dve", lambda e: e.tensor_tensor(out=dst[:], in0=src[:], in1=pbx[:], op=ALU.mult), r=[src, pbx], w=[dst])

        for j in range(3):
            bprod(BT, KB[j], KN[j], j)
            bprod(BG, KG[j], KN[j], j)
            bprod(BT, VB[j], VB[j], j)
            bprod(EG, QG[j], QN[j], j)
            bprod(ER, KR[j], KN[j], j)
        XO = {}
        for nm, X in (("kn", KN), ("kb", KB), ("qn", QN), ("qg", QG)):
            xo = P.sb("dn_xo_" + nm, [64, 3 * TBA], F32)
            XO[nm] = xo
            for j in range(3):
                px = PS[3 + nb[0] % 4]
                nb[0] += 1
                P.op("pe", lambda e, px=px, X=X, j=j: e.matmul(px[0:64, :], lhsT=self.C("selhi"), rhs=X[j][:],
                                                               start=True, stop=True), r=[self.cst, X[j]], w=[px])
                P.op("act", lambda e, px=px, xo=xo, j=j: e.activation(out=xo[:, j * TBA:(j + 1) * TBA], in_=px[0:64, :],
                                                                      func=AF.Copy), r=[px], w=[xo])
        XE = {"kn": KN, "kb": KB, "qn": QN, "qg": QG}

        def hd(nm, h, ctok):
            j = h // 2
            if h % 2 == 0:
                return XE[nm][j][0:64, ctok], XE[nm][j]
            return XO[nm][:, j * TBA + ctok.start:j * TBA + ctok.stop], XO[nm]

        NIL = int(os.environ.get("DN_NIL", "2"))
        TMa = [P.sb("dn_tma%d" % p, [64, 768], F32) for p in range(NIL)]
        KRt = [P.sb("dn_krt%d" % n, [64, 384], F32) for n in range(NCH)]
        QK = [P.sb("dn_qk%d" % n, [64, 384], F32) for n in range(NCH)]
        U = [P.sb("dn_u%d" % n, [64, 384], F32) for n in range(NCH)]
        WT = [P.sb("dn_wt%d" % n, [64, 384], F32) for n in range(NCH)]
        DT = [P.sb("dn_dt%d" % p, [64, 384], F32) for p in range(NIL)]
        Mt = [[P.sb("dn_m%d_%d" % (p, i), [64, 384], F32) for i in range(2)] for p in range(NIL)]
        Nt = [[P.sb("dn_n%d_%d" % (p, i), [64, 384], F32) for i in range(2)] for p in range(NIL)]
        W = [P.sb("dn_w%d" % p, [64, 384], F32) for p in range(NIL)]
        VNEW = P.sb("dn_vnew", [64, 384], F32)
        id64 = self.C("ident", rows=64)[:, 0:64]
        hcs = [slice(h * 64, (h + 1) * 64) for h in range(6)]
        bk = [0]

        def nbank():
            bk[0] += 1
            return PS[3 + bk[0] % 5]

        def st_transpose(n, p):
            ctok = slice(n * 64, (n + 1) * 64)
            for qi, (X, dst, dcol, dep) in enumerate(((KG, TMa[p], 0, (TMa[p], 0)), (VB, TMa[p], 384, (TMa[p], 1)),
                                                       (KR, KRt[n], 0, KRt[n]))):
                pt = nbank()
                for j in range(3):
                    P.op("pe", lambda e, pt=pt, X=X, j=j: e.transpose(out=pt[0:64, j * 128:(j + 1) * 128], in_=X[j][:, ctok],
                                                                      identity=self.C("ident")), r=[X[j], self.cst], w=[pt])
                P.op("act", lambda e, pt=pt, dst=dst, dcol=dcol: e.activation(out=dst[:, dcol:dcol + 384], in_=pt[0:64, 0:384],
                                                                              func=AF.Copy), r=[pt], w=[dep])

        def st_decay(n, p):
            ctok = slice(n * 64, (n + 1) * 64)
            pd = nbank()
            for h in range(6):
                oh_ = pd[0:64, hcs[h]]
                P.op("pe", lambda e, oh_=oh_: e.matmul(oh_, lhsT=id64, rhs=self.C("negm", rows=64), start=True, stop=False),
                     r=[self.cst], w=[pd])
                P.op("pe", lambda e, oh_=oh_, h=h: e.matmul(oh_, lhsT=self.C("selh_%d" % h, rows=6), rhs=GC[:, ctok],
                                                            start=False, stop=False), r=[self.cst, GC], w=[pd])
                P.op("pe", lambda e, oh_=oh_, h=h: e.matmul(oh_, lhsT=NG[:, h, ctok], rhs=self.C("ones", rows=6)[:, 0:64],
                                                            start=False, stop=True), r=[self.cst, NG], w=[pd])
            P.op("act", lambda e: e.activation(out=DT[p][:], in_=pd[0:64, 0:384], func=AF.Exp), r=[pd], w=[DT[p]])

        def st_kk(n, p):
            ctok = slice(n * 64, (n + 1) * 64)
            pk_ = nbank()
            pq = nbank()
            for h in range(6):
                kn_ap, kn_d = hd("kn", h, ctok)
                kb_ap, kb_d = hd("kb", h, ctok)
                qn_ap, qn_d = hd("qn", h, ctok)
                P.op("pe", lambda e, h=h, kn_ap=kn_ap, kb_ap=kb_ap: e.matmul(pk_[0:64, hcs[h]], lhsT=kn_ap, rhs=kb_ap,
                                                                             start=True, stop=True), r=[kn_d, kb_d], w=[pk_])
                P.op("pe", lambda e, h=h, kn_ap=kn_ap, qn_ap=qn_ap: e.matmul(pq[0:64, hcs[h]], lhsT=kn_ap, rhs=qn_ap,
                                                                             start=True, stop=True), r=[kn_d, qn_d], w=[pq])
            M = Mt[p][0]
            P.op("dve", lambda e: e.tensor_tensor(out=M[:], in0=pk_[0:64, 0:384], in1=DT[p][:], op=ALU.mult),
                 r=[pk_, DT[p]], w=[M])
            P.op("pool", lambda e: e.tensor_tensor(out=M[:], in0=M[:], in1=self.C("sut6", rows=64), op=ALU.mult),
                 r=[M, self.cst], w=[M])
            P.op("dve", lambda e: e.tensor_tensor(out=QK[n][:], in0=pq[0:64, 0:384], in1=DT[p][:], op=ALU.mult),
                 r=[pq, DT[p]], w=[QK[n]])

        def st_n0(n, p):
            M, N = Mt[p][0], Nt[p][0]
            pn = nbank()
            for h in range(6):
                P.op("pe", lambda e, h=h: e.transpose(out=pn[0:64, hcs[h]], in_=M[:, hcs[h]], identity=id64),
                     r=[M, self.cst], w=[pn])
            P.op("act", lambda e: e.activation(out=N[:], in_=pn[0:64, 0:384], func=AF.Copy), r=[pn], w=[N])
            P.op("dve", lambda e: e.tensor_tensor(out=W[p][:], in0=self.C("id6", rows=64), in1=M[:], op=ALU.subtract),
                 r=[M, self.cst], w=[W[p]])

        def st_level(i):
            def f(n, p):
                M, N = Mt[p][i % 2], Nt[p][i % 2]
                Mn, Nn = Mt[p][(i + 1) % 2], Nt[p][(i + 1) % 2]
                pn = nbank()
                for h in range(6):
                    P.op("pe", lambda e, h=h: e.matmul(pn[0:64, hcs[h]], lhsT=M[:, hcs[h]], rhs=N[:, hcs[h]],
                                                       start=True, stop=True), r=[M, N], w=[pn])
                P.op("act", lambda e: e.activation(out=Nn[:], in_=pn[0:64, 0:384], func=AF.Copy), r=[pn], w=[Nn])
                if i < 4:
                    pm = nbank()
                    for h in range(6):
                        P.op("pe", lambda e, h=h: e.matmul(pm[0:64, hcs[h]], lhsT=N[:, hcs[h]], rhs=M[:, hcs[h]],
                                                           start=True, stop=True), r=[M, N], w=[pm])
                    P.op("dve", lambda e: e.tensor_copy(out=Mn[:], in_=pm[0:64, 0:384]), r=[pm], w=[Mn])
            return f

        def st_wupd(i):
            def f(n, p):
                Nn = Nt[p][(i + 1) % 2]
                pw = nbank()
                for h in range(6):
                    P.op("pe", lambda e, h=h: e.matmul(pw[0:64, hcs[h]], lhsT=Nn[:, hcs[h]], rhs=W[p][:, hcs[h]],
                                                       start=True, stop=True), r=[Nn, W[p]], w=[pw])
                P.op("dve", lambda e: e.tensor_tensor(out=W[p][:], in0=pw[0:64, 0:384], in1=W[p][:], op=ALU.add),
                     r=[pw, W[p]], w=[W[p]])
            return f

        def st_uw(n, p):
            pu = nbank()
            pw_ = nbank()
            for h in range(6):
                P.op("pe", lambda e, h=h: e.matmul(pu[0:64, hcs[h]], lhsT=W[p][:, hcs[h]],
                                                   rhs=TMa[p][:, 384 + h * 64:384 + (h + 1) * 64],
                                                   start=True, stop=True), r=[W[p], (TMa[p], 1)], w=[pu])
                P.op("pe", lambda e, h=h: e.matmul(pw_[0:64, hcs[h]], lhsT=TMa[p][:, h * 64:(h + 1) * 64], rhs=W[p][:, hcs[h]],
                                                   start=True, stop=True), r=[W[p], (TMa[p], 0)], w=[pw_])
            P.op("act", lambda e: e.activation(out=U[n][:], in_=pu[0:64, 0:384], func=AF.Copy), r=[pu], w=[U[n]])
            P.op("dve", lambda e: e.tensor_copy(out=WT[n][:], in_=pw_[0:64, 0:384]), r=[pw_], w=[WT[n]])

        stages = [st_transpose, st_decay, st_kk, st_n0]
        for i in range(int(os.environ.get("DN_LEVELS", "5"))):
            stages += [st_level(i), st_wupd(i)]
        stages.append(st_uw)
        for g0 in range(0, NCH, NIL):
            for st in stages:
                for p in range(NIL):
                    st(g0 + p, p)
        OT = [PS[0], PS[1], PS[2]]
        pv = PS[3]
        pkv = PS[4]
        for n in range(NCH):
            ctok = slice(n * 64, (n + 1) * 64)
            for h in range(6):
                P.op("pe", lambda e, h=h, n=n: e.matmul(pv[0:64, hcs[h]], lhsT=WT[n][:, hcs[h]], rhs=dS[:, hcs[h]],
                                                        start=True, stop=True), r=[WT[n], dS], w=[pv])
            P.op("dve", lambda e, n=n: e.tensor_tensor(out=VNEW[:], in0=U[n][:], in1=pv[0:64, 0:384], op=ALU.subtract),
                 r=[U[n], pv], w=[VNEW])
            for h in range(6):
                oap = OT[h // 2][(h % 2) * 64:(h % 2 + 1) * 64, ctok]
                qg_ap, qg_d = hd("qg", h, ctok)
                P.op("pe", lambda e, h=h, oap=oap, qg_ap=qg_ap: e.matmul(oap, lhsT=dS[:, hcs[h]], rhs=qg_ap,
                                                                         start=True, stop=False), r=[dS, qg_d], w=[OT[h // 2]])
                P.op("pe", lambda e, h=h, oap=oap, n=n: e.matmul(oap, lhsT=VNEW[:, hcs[h]], rhs=QK[n][:, hcs[h]],
                                                                 start=False, stop=True), r=[VNEW, QK[n]], w=[OT[h // 2]])
            for h in range(6):
                P.op("pe", lambda e, h=h, n=n: e.matmul(pkv[0:64, hcs[h]], lhsT=KRt[n][:, hcs[h]], rhs=VNEW[:, hcs[h]],
                                                        start=True, stop=True), r=[KRt[n], VNEW], w=[pkv])
            S3 = dS[:].rearrange("p (h v) -> p h v", h=6)
            decB = DEC[:, n * 6:(n + 1) * 6].rearrange("p (h o) -> p h o", o=1).broadcast_to([64, 6, 64])
            P.op("dve", lambda e, S3=S3, decB=decB: e.tensor_tensor(out=S3, in0=S3, in1=decB, op=ALU.mult), r=[dS, DEC], w=[dS])
            P.op("dve", lambda e: e.tensor_tensor(out=dS[:], in0=dS[:], in1=pkv[0:64, 0:384], op=ALU.add), r=[dS, pkv], w=[dS])
        NPS = [PS[5], PS[6], PS[7]]
        NT = [(C[jj], C[3 + jj]) for jj in range(3)]
        for jj in range(3):
            osb, sq = NT[jj]
            P.op("act", lambda e, osb=osb, jj=jj: e.activation(out=osb[:], in_=OT[jj][:], func=AF.Copy), r=[OT[jj]], w=[osb])
            P.op("act", lambda e, sq=sq, jj=jj: e.activation(out=sq[:], in_=OT[jj][:], func=AF.Square), r=[OT[jj]], w=[sq])
        for jj in range(3):
            osb, sq = NT[jj]
            pss = NPS[jj]
            P.op("pe", lambda e, sq=sq, pss=pss: e.matmul(pss[:], lhsT=self.C("blk64"), rhs=sq[:], start=True, stop=True),
                 r=[self.cst, sq], w=[pss])
        for jj in range(3):
            osb, sq = NT[jj]
            pss = NPS[jj]
            P.op("act", lambda e, sq=sq, pss=pss: e.activation(out=sq[:], in_=pss[:], func=AF.Ln, bias=self.epsc[:, 0:1],
                                                               scale=1.0 / 64), r=[pss, self.epsc], w=[sq])
        for jj in range(3):
            osb, sq = NT[jj]
            P.op("act", lambda e, sq=sq: e.activation(out=sq[:], in_=sq[:], func=AF.Exp, scale=-0.5), r=[sq], w=[sq])
        for jj in range(3):
            osb, sq = NT[jj]
            P.op("dve", lambda e, osb=osb, sq=sq: e.scalar_tensor_tensor(
                out=osb[:], in0=osb[:], scalar=self.PK("dnnw%d" % l), in1=sq[:], op0=ALU.mult, op1=ALU.mult),
                r=[osb, sq, self.pk], w=[osb])
        for jj in range(3):
            osb, sq = NT[jj]
            pgz = NPS[jj]
            self.proj_fm(pgz, O_DZ + jj * 128, 128)
            P.op("act", lambda e, sq=sq, pgz=pgz: e.activation(out=sq[:], in_=pgz[:], func=AF.Silu), r=[pgz], w=[sq])
        for jj in range(3):
            osb, sq = NT[jj]
            P.op("dve", lambda e, osb=osb, sq=sq, jj=jj: e.tensor_tensor(out=self.MT[:, 5 + jj, :], in0=osb[:], in1=sq[:],
                                                                         op=ALU.mult), r=[osb, sq], w=[(self.MT, 5 + jj)])
        P.pop_scope(barrier=False)

    def passB(self, l, last):
        P = self.P
        S, SBT = self.S, self.SBT
        moe = (l % 2 == 1)
        P.push_scope()
        nbs = SBT // TB
        H2 = P.sb("B_H2", [128, NK, SBT], BF16)
        YACC = P.sb("B_YACC", [128, NK, SBT], F32)
        XT = P.sb("B_XT", [128, NK, TB], F32)
        SQ = P.sb("B_SQ", [128, NK, TB], BF16)
        rstd = P.sb("B_rstd", [128, TB], F32)
        tmp = [P.sb("B_tmp%d" % i, [128, TB], F32) for i in range(2)]
        WG = [P.sb("B_WG%d" % i, [128, NK, 512], BF16) for i in range(2)]
        WU = [P.sb("B_WU%d" % i, [128, NK, 512], BF16) for i in range(2)]
        WD = [P.sb("B_WD%d" % i, [128, 4, D], BF16) for i in range(2)]
        AT = [P.sb("B_AT%d" % i, [128, 4, TB], BF16) for i in range(2)]
        SG = [P.sb("B_SG%d" % i, [128, TB], F32) for i in range(2)]
        mod = self.mod[l]
        if moe:
            CB = P.sb("B_cstb", [8, self.ccb.n], F32)
            P.dma("sp", CB[:], self.cstb_d[0:8, :], r=[self.cstb_d], w=[CB])
            HE = [P.sb("B_HE%d" % i, [128, NK, TB], BF16) for i in range(2)]
            RW = P.sb("B_RW", [128, NK, NE], F32)
            P.dma("sp", RW[:], self.router_w[0].rearrange("(k p) n -> p k n", p=128), r=[self.router_w], w=[RW])
            GT = P.sb("B_GT", [128, SBT // 128, NE], F32)
            GF = P.sb("B_GF", [NE, SBT], F32)
            gs = [P.sb("B_gs%d" % i, [128, NE], F32) for i in range(4)]
            gm = [P.sb("B_gm%d" % i, [128, 1], F32) for i in range(4)]
            experts = list(range(NE))
            dff = D_FFE
        else:
            experts = [0]
            dff = D_FF
        fgs = []
        f0 = 0
        while f0 < dff:
            fs = min(512, dff - f0)
            fgs.append((f0, fs))
            f0 += fs
        nsb = S // SBT
        for sb in range(nsb):
            for bi in range(nbs):
                b = sb * nbs + bi
                if moe:
                    lgp = self.PSB[6]
                    first = [True]

                    def hf_cb(k, t, bi=bi, lgp=lgp, first=first):
                        for tt in range(4):
                            st = first[0]
                            first[0] = False
                            P.op("pe", lambda e, k=k, t=t, tt=tt, st=st: e.matmul(
                                lgp[:, tt * NE:(tt + 1) * NE], lhsT=t[:, tt * 128:(tt + 1) * 128], rhs=RW[:, k, :],
                                start=st, stop=(k == NK - 1), skip_group_check=True), r=[t, RW], w=[lgp])
                else:
                    hf_cb = None
                self.load_norm(XT, b, self.gv2[l], lambda k: mod[:, 24 + k:24 + k + 1],
                               lambda k, bi=bi: H2[:, k, bi * TB:(bi + 1) * TB], (H2, bi), SQ, tmp, rstd, hf_cb=hf_cb)
                if moe:
                    for tt in range(4):
                        ti = bi * 4 + tt
                        lg = lgp[:, tt * NE:(tt + 1) * NE]
                        g0, g1_, g2_, g3_ = gs
                        m1, m2, sm, _ = gm
                        P.op("dve", lambda e, lg=lg: e.tensor_copy(out=g0[:], in_=lg), r=[lgp], w=[g0])
                        P.op("dve", lambda e: e.tensor_reduce(out=m1[:], in_=g0[:], axis=AX.X, op=ALU.max), r=[g0], w=[m1])
                        P.op("dve", lambda e: e.tensor_scalar(out=g1_[:], in0=g0[:], scalar1=m1[:, 0:1], scalar2=-1e30,
                                                              op0=ALU.is_ge, op1=ALU.mult), r=[g0, m1], w=[g1_])
                        P.op("dve", lambda e: e.tensor_tensor(out=g2_[:], in0=g0[:], in1=g1_[:], op=ALU.add), r=[g0, g1_], w=[g2_])
                        P.op("dve", lambda e: e.tensor_reduce(out=m2[:], in_=g2_[:], axis=AX.X, op=ALU.max), r=[g2_], w=[m2])
                        P.op("dve", lambda e: e.tensor_scalar(out=g1_[:], in0=g0[:], scalar1=m2[:, 0:1], scalar2=None,
                                                              op0=ALU.is_ge), r=[g0, m2], w=[g1_])
                        P.op("dve", lambda e: e.tensor_scalar(out=g2_[:], in0=g0[:], scalar1=m1[:, 0:1], scalar2=None,
                                                              op0=ALU.subtract), r=[g0, m1], w=[g2_])
                        P.op("act", lambda e: e.activation(out=g2_[:], in_=g2_[:], func=AF.Exp), r=[g2_], w=[g2_])
                        P.op("dve", lambda e: e.tensor_tensor(out=g2_[:], in0=g2_[:], in1=g1_[:], op=ALU.mult), r=[g2_, g1_], w=[g2_])
                        P.op("dve", lambda e: e.tensor_reduce(out=sm[:], in_=g2_[:], axis=AX.X, op=ALU.add), r=[g2_], w=[sm])
                        P.op("dve", lambda e: e.reciprocal(out=sm[:], in_=sm[:]), r=[sm], w=[sm])
                        P.op("dve", lambda e, ti=ti: e.tensor_scalar(out=GT[:, ti, :], in0=g2_[:], scalar1=sm[:, 0:1],
                                                                     scalar2=None, op0=ALU.mult), r=[g2_, sm], w=[GT])
                        tp = self.PSB[7]
                        P.op("pe", lambda e, ti=ti, tp=tp: e.transpose(out=tp[0:NE, 0:128], in_=GT[:, ti, :],
                                                                       identity=self.C("ident")), r=[GT, self.cst], w=[tp])
                        P.op("act", lambda e, ti=ti, tp=tp: e.activation(out=GF[:, ti * 128:(ti + 1) * 128],
                                                                         in_=tp[0:NE, 0:128], func=AF.Copy), r=[tp], w=[GF])
            units = []
            for ei, ex in enumerate(experts):
                for fi, (f0, fs) in enumerate(fgs):
                    for bi in range(nbs):
                        units.append((ei, ex, fi, f0, fs, bi))
            wslot = {}
            nload = [0]

            def load_w(ex, fi, f0, fs):
                s = nload[0] % 2
                nload[0] += 1
                wslot[(ex, fi)] = s
                nfc = fs // 128
                if moe:
                    wg, wu, wd = self.moe_wg[0, ex], self.moe_wu[0, ex], self.moe_wd[0, ex]
                else:
                    wg, wu, wd = self.ffn_wg[0], self.ffn_wu[0], self.ffn_wd[0]
                P.dma("pool", WG[s][:, :, 0:fs], wg[:, f0:f0 + fs].rearrange("(k p) n -> p k n", p=128),
                      r=[self.moe_wg], w=[WG[s]])
                P.dma("pool", WU[s][:, :, 0:fs], wu[:, f0:f0 + fs].rearrange("(k p) n -> p k n", p=128),
                      r=[self.moe_wg], w=[WU[s]])
                P.dma("pool", WD[s][:, 0:nfc, :], wd[f0:f0 + fs, :].rearrange("(k p) n -> p k n", p=128),
                      r=[self.moe_wg], w=[WD[s]])

            efs = [(ex, fi, f0, fs) for ex in experts for fi, (f0, fs) in enumerate(fgs)]
            load_w(*efs[0])
            he_slot = {}
            nhe = [0]
            gcount = [0]
            ycount = [0]

            he_pre = moe and nbs == 2

            def make_he(ex, bi):
                tok = slice(bi * TB, (bi + 1) * TB)
                hs_ = nhe[0] % 2
                nhe[0] += 1
                he_slot[(ex, bi)] = hs_
                gb = self.PSB[7]
                P.op("pe", lambda e: e.matmul(
                    gb[:], lhsT=CB[:, self.ccb.off["sel8_%d" % ex][0]:self.ccb.off["sel8_%d" % ex][0] + 128],
                    rhs=GF[:, tok], start=True, stop=True), r=[CB, GF], w=[gb])
                P.op("act", lambda e: e.activation(out=rstd[:], in_=gb[:], func=AF.Copy), r=[gb], w=[rstd])
                for k in range(NK):
                    P.op("dve", lambda e, k=k: e.tensor_tensor(
                        out=HE[hs_][:, k, :], in0=H2[:, k, tok], in1=rstd[:], op=ALU.mult),
                        r=[(H2, bi), rstd], w=[HE[hs_]])

            if he_pre:
                for bi_ in range(nbs):
                    make_he(experts[0], bi_)

            def stage1(u, ui):
                ei, ex, fi, f0, fs, bi = u
                s = wslot[(ex, fi)]
                nfc = fs // 128
                tok = slice(bi * TB, (bi + 1) * TB)
                if moe:
                    if fi == 0 and not he_pre:
                        make_he(ex, bi)
                    hu = HE[he_slot[(ex, bi)]]
                at = AT[ui % 2]
                for fc in range(nfc):
                    pg = self.PSB[gcount[0] % 2]
                    pu = self.PSB[2 + gcount[0] % 2]
                    sg = SG[gcount[0] % 2]
                    gcount[0] += 1
                    for k in range(NK):
                        P.op("pe", lambda e, k=k, fc=fc, pg=pg, s=s, tok=tok: e.matmul(
                            pg[:], lhsT=WG[s][:, k, fc * 128:(fc + 1) * 128], rhs=H2[:, k, tok],
                            start=(k == 0), stop=(k == NK - 1)), r=[WG[s], (H2, bi)], w=[pg])
                    for k in range(NK):
                        if moe:
                            P.op("pe", lambda e, k=k, fc=fc, pu=pu, s=s, hu=hu: e.matmul(
                                pu[:], lhsT=WU[s][:, k, fc * 128:(fc + 1) * 128], rhs=hu[:, k, :],
                                start=(k == 0), stop=(k == NK - 1)), r=[WU[s], hu], w=[pu])
                        else:
                            P.op("pe", lambda e, k=k, fc=fc, pu=pu, s=s, tok=tok: e.matmul(
                                pu[:], lhsT=WU[s][:, k, fc * 128:(fc + 1) * 128], rhs=H2[:, k, tok],
                                start=(k == 0), stop=(k == NK - 1)), r=[WU[s], (H2, bi)], w=[pu])
                    P.op("act", lambda e, sg=sg, pg=pg: e.activation(out=sg[:], in_=pg[:], func=AF.Silu), r=[pg], w=[sg])
                    P.op("dve", lambda e, sg=sg, pu=pu, at=at, fc=fc: e.tensor_tensor(
                        out=at[:, fc, :], in0=pu[:], in1=sg[:], op=ALU.mult), r=[pu, sg], w=[at])

            def stage2(u, ui):
                ei, ex, fi, f0, fs, bi = u
                s = wslot[(ex, fi)]
                nfc = fs // 128
                at = AT[ui % 2]
                tok = slice(bi * TB, (bi + 1) * TB)
                firstacc = (ei == 0 and fi == 0)
                for dc in range(NK):
                    py = self.PSB[4 + ycount[0] % 4]
                    ycount[0] += 1
                    for fc in range(nfc):
                        P.op("pe", lambda e, fc=fc, dc=dc, py=py, s=s, at=at: e.matmul(
                            py[:], lhsT=WD[s][:, fc, dc * 128:(dc + 1) * 128], rhs=at[:, fc, :],
                            start=(fc == 0), stop=(fc == nfc - 1)), r=[WD[s], at], w=[py])
                    if firstacc:
                        P.op("act", lambda e, dc=dc, py=py, tok=tok: e.activation(
                            out=YACC[:, dc, tok], in_=py[:], func=AF.Copy), r=[py], w=[(YACC, (bi, dc))])
                    else:
                        P.op("dve", lambda e, dc=dc, py=py, tok=tok: e.tensor_tensor(
                            out=YACC[:, dc, tok], in0=py[:], in1=YACC[:, dc, tok], op=ALU.add),
                            r=[py, (YACC, (bi, dc))], w=[(YACC, (bi, dc))])

            for ui, u in enumerate(units):
                stage1(u, ui)
                if he_pre and u[2] == len(fgs) - 1 and u[0] + 1 < len(experts):
                    make_he(experts[u[0] + 1], u[5])
                if ui > 0:
                    stage2(units[ui - 1], ui - 1)
                if u[5] == 0:
                    idx = efs.index((u[1], u[2], u[3], u[4]))
                    if idx + 1 < len(efs):
                        load_w(*efs[idx + 1])
            stage2(units[-1], len(units) - 1)
            for bi in range(nbs):
                b = sb * nbs + bi
                tok = slice(bi * TB, (bi + 1) * TB)
                P.dma("sp", XT[:], self.xT[:, b * TB:(b + 1) * TB].rearrange("(k p) n -> p k n", p=128),
                      r=[(self.xT, b)], w=[XT])
                for k in range(NK):
                    P.op("dve", lambda e, k=k, tok=tok: e.scalar_tensor_tensor(
                        out=XT[:, k, :], in0=YACC[:, k, tok], scalar=mod[:, 40 + k:40 + k + 1], in1=XT[:, k, :],
                        op0=ALU.mult, op1=ALU.add), r=[(YACC, (bi, k)), mod, XT], w=[XT])
                if not last or self.dbg:
                    P.dma("sp", self.xT[:, b * TB:(b + 1) * TB].rearrange("(k p) n -> p k n", p=128), XT[:],
                          r=[XT], w=[(self.xT, b)])
                if last:
                    self.final_block(XT, SQ, rstd, tmp, b)
        P.pop_scope()
        self.dbg_dump("dbg_xB%d" % l)

    def final_block(self, XT, SQ, rstd, tmp, b):
        P = self.P
        ps = self.PSB[7]
        P.op("act", lambda e: e.activation(out=SQ[:], in_=XT[:], func=AF.Square), r=[XT], w=[SQ])
        for k in range(NK):
            P.op("pe", lambda e, k=k: e.matmul(ps[:], lhsT=self.ones_bf[:], rhs=SQ[:, k, :],
                                               start=(k == 0), stop=(k == NK - 1)), r=[self.ones_bf, SQ], w=[ps])
        P.op("act", lambda e: e.activation(out=rstd[:], in_=ps[:], func=AF.Ln, bias=self.epsc[:, 0:1], scale=1.0 / D),
             r=[ps, self.epsc], w=[rstd])
        P.op("act", lambda e: e.activation(out=rstd[:], in_=rstd[:], func=AF.Exp, scale=-0.5), r=[rstd], w=[rstd])
        for k in range(NK):
            P.op("dve", lambda e, k=k: e.scalar_tensor_tensor(
                out=XT[:, k, :], in0=XT[:, k, :], scalar=self.PK("fnw", col=k), in1=rstd[:],
                op0=ALU.mult, op1=ALU.mult), r=[XT, self.pk, rstd], w=[XT])
        for tt in range(TB // 128):
            ot = tmp[tt % 2]
            for half in range(2):
                pb = self.PSB[(tt * 2 + half) % 4]
                for q in range(4):
                    k = half * 4 + q
                    P.op("pe", lambda e, pb=pb, q=q, k=k, tt=tt: e.transpose(
                        out=pb[:, q * 128:(q + 1) * 128], in_=XT[:, k, tt * 128:(tt + 1) * 128],
                        identity=self.C("ident")), r=[XT, self.cst], w=[pb])
                o = tmp[half]
                if half == 0:
                    P.op("act", lambda e, pb=pb, o=o: e.activation(out=o[:], in_=pb[:], func=AF.Copy), r=[pb], w=[o])
                else:
                    P.op("dve", lambda e, pb=pb, o=o: e.tensor_copy(out=o[:], in_=pb[:]), r=[pb], w=[o])
                r0 = b * TB + tt * 128
                ev = P.dma("sp", self.out[r0:r0 + 128, half * 512:(half + 1) * 512], o[:], r=[o], w=[(self.out, (r0, half))])
                self.out_events.append(ev)


class _View:
    def __init__(self, buf, bi):
        self.buf = buf
        self.bi = bi
        self.id = buf.id

    def __getitem__(self, idx):
        p, k, n = idx
        assert n == slice(None, None, None)
        return self.buf[p, k, self.bi * TB:(self.bi + 1) * TB]


_PARAM_CACHE = {}


def _dummy_inputs():
    z = lambda *s: np.zeros(s, np.float32)
    return {"ada_b": z(2, 6144), "norm1_w": z(2, 1024), "norm2_w": z(2, 1024), "gla_b_lr2": z(2, 192),
            "gla_norm_w": z(2, 64), "lru_conv_w": z(2, 4, 256), "lru_conv_b": z(2, 256), "lru_b_a": z(2, 256),
            "lru_b_x": z(2, 256), "lru_lambda": z(2, 256), "dn_conv_w": z(2, 4, 1152), "dn_a_log": z(2, 6),
            "dn_dt_bias": z(2, 6), "dn_norm_w": z(2, 64), "final_norm_w": z(1024)}


def build_params_ncols():
    if "c" not in _PARAM_CACHE:
        _PARAM_CACHE["c"] = build_params(_dummy_inputs())
    return _PARAM_CACHE["c"].n


def build_params_offsets():
    build_params_ncols()
    return _PARAM_CACHE["c"].off


def make_in_maps(inputs, S, ncores):
    consts = build_consts().build()
    pk = build_params(inputs).build()
    mats = build_mats(inputs)
    f = lambda a: np.ascontiguousarray(np.asarray(a, np.float32))
    shared = {
        "w_in": f(inputs["w_in"]), "w_out": f(inputs["w_out"]), "ada_w": f(inputs["ada_w"]),
        "ffn_w_gate": f(inputs["ffn_w_gate"]), "ffn_w_up": f(inputs["ffn_w_up"]), "ffn_w_down": f(inputs["ffn_w_down"]),
        "router_w": f(inputs["router_w"]), "moe_w_gate": f(inputs["moe_w_gate"]), "moe_w_up": f(inputs["moe_w_up"]),
        "moe_w_down": f(inputs["moe_w_down"]), "cst": consts, "cstb": build_consts_b().build(), "pk": pk, "wlr2": mats["wlr2"], "lrubd": mats["lrubd"],
    }
    maps = []
    x = np.asarray(inputs["x"], np.float32)
    c = np.asarray(inputs["c"], np.float32)
    for i in range(ncores):
        m = dict(shared)
        m["x"] = np.ascontiguousarray(x[i, :S])
        m["cfm"] = _fm(c[i], 8)
        maps.append(m)
    return maps


_BUILD = {}


def kernel(**inputs):
    S = inputs["x"].shape[1]
    B = inputs["x"].shape[0]
    if "b" not in _BUILD:
        _BUILD["b"] = Builder(S)
    bld = _BUILD["b"]
    maps = make_in_maps(inputs, S, B)
    res = run_bass_kernel_spmd(bld.nc, maps, core_ids=list(range(B)))
    return np.stack([np.asarray(r["out"]) for r in res.results], 0).astype(np.float32)
```
